# Optimizing a Trainium2 kernel written in Bass

```python
import jax, jax.numpy as jnp
from jax import lax
import numpy as np

D_MODEL = 1024
BATCH = 4
SEQ = 8192
DEPTH = 1

ATT_HEADS = 8
ATT_HEAD_DIM = 64
IDX_HEADS = 4
IDX_DIM = 64
IDX_TOPK_MAX = 256
Q_BLOCK = 128
ROPE_THETA = 10000.0
MLSTM_HEADS = 8
MLSTM_QK_DIM = 64
MLSTM_V_DIM = 128
MLSTM_CHUNK = 128
CONV_WIDTH = 4
N_GROUPS = 4
EXPERTS_PER_GROUP = 4
N_EXPERTS = N_GROUPS * EXPERTS_PER_GROUP
TOP_K_IN_GROUP = 2
EXPERT_DIM = 512
DEEPNORM_ALPHA = (2.0 * DEPTH) ** 0.25
DEEPNORM_BETA = (8.0 * DEPTH) ** -0.25
LN_EPS = 1e-5
GN_EPS = 1e-6

ATT_W = ATT_HEADS * ATT_HEAD_DIM
MLSTM_QK_W = MLSTM_HEADS * MLSTM_QK_DIM
MLSTM_V_W = MLSTM_HEADS * MLSTM_V_DIM
IN_SPLITS = (ATT_W, ATT_W, ATT_W, IDX_HEADS * IDX_DIM, IDX_DIM, IDX_HEADS,
             MLSTM_QK_W, MLSTM_QK_W, MLSTM_V_W, MLSTM_HEADS, MLSTM_HEADS, MLSTM_V_W,
             D_MODEL, D_MODEL)
IN_WIDTH = sum(IN_SPLITS)

kernel_name = 'hybrid_dsa_mlstm_hmoe_block'


def _layer_norm(x, gain, bias):
    xf = x.astype(jnp.float32)
    mu = xf.mean(-1, keepdims=True)
    var = jnp.square(xf - mu).mean(-1, keepdims=True)
    return (xf - mu) * lax.rsqrt(var + LN_EPS) * gain + bias


def _rope_tables(seq, dim):
    inv = ROPE_THETA ** (-jnp.arange(0, dim, 2, dtype=jnp.float32) / dim)
    ang = jnp.arange(seq, dtype=jnp.float32)[:, None] * inv[None, :]
    return jnp.cos(ang), jnp.sin(ang)


def _rope(x, cos, sin):
    x1, x2 = jnp.split(x, 2, axis=-1)
    c = cos[None, :, None, :]
    s = sin[None, :, None, :]
    return jnp.concatenate([x1 * c - x2 * s, x1 * s + x2 * c], axis=-1)


def _dsa_sparse_attention(q, k, v, q_idx, k_idx, w_idx):
    B, S, H, Dh = q.shape
    topk = min(IDX_TOPK_MAX, S // 4)
    nb = S // Q_BLOCK
    f32 = jnp.float32

    def to_blocks(a):
        return jnp.moveaxis(a.reshape((B, nb, Q_BLOCK) + a.shape[2:]), 1, 0)

    k_idx_f = k_idx.astype(f32)
    b_ix = jnp.arange(B)[:, None, None]
    key_pos = jnp.arange(S)

    def block_fn(args):
        qb, qib, wb, start = args
        t = start + jnp.arange(Q_BLOCK)
        rel = jax.nn.relu(jnp.einsum('bqhd,bsd->bqhs', qib.astype(f32), k_idx_f))
        score = jnp.einsum('bqhs,bqh->bqs', rel, wb.astype(f32))
        causal = key_pos[None, :] <= t[:, None]
        score = jnp.where(causal[None], score, -jnp.inf)
        _, sel = lax.top_k(score, topk)
        valid = sel <= t[None, :, None]
        k_sel = k[b_ix, sel]
        v_sel = v[b_ix, sel]
        logits = jnp.einsum('bqhd,bqkhd->bqhk', qb, k_sel).astype(f32) * (Dh ** -0.5)
        logits = jnp.where(valid[:, :, None, :], logits, -jnp.inf)
        p = jax.nn.softmax(logits, axis=-1).astype(v.dtype)
        return jnp.einsum('bqhk,bqkhd->bqhd', p, v_sel)

    starts = jnp.arange(nb, dtype=jnp.int32) * Q_BLOCK
    out = lax.map(block_fn, (to_blocks(q), to_blocks(q_idx), to_blocks(w_idx), starts))
    return jnp.moveaxis(out, 0, 1).reshape(B, S, H * Dh)


def _causal_depthwise_conv(x, w):
    C = x.shape[-1]
    return lax.conv_general_dilated(x, w[:, None, :].astype(x.dtype), window_strides=(1,),
                                    padding=[(CONV_WIDTH - 1, 0)],
                                    dimension_numbers=('NWC', 'WIO', 'NWC'),
                                    feature_group_count=C)


def _mlstm_chunkwise(q, k, v, i_pre, f_pre):
    B, S, H, Dk = q.shape
    Dv = v.shape[-1]
    L = MLSTM_CHUNK
    nc = S // L
    f32 = jnp.float32

    def chunks(a):
        a = a.astype(f32).reshape((B, nc, L, H) + a.shape[3:])
        return jnp.moveaxis(jnp.moveaxis(a, 1, 0), 3, 2)

    qc = chunks(q.astype(f32) * (Dk ** -0.5))
    kc = chunks(k)
    vc = chunks(v)
    ic = chunks(i_pre)
    lfc = chunks(jax.nn.log_sigmoid(f_pre.astype(f32)))
    causal = jnp.tril(jnp.ones((L, L), dtype=bool))

    def step(carry, xs):
        C, n, m = carry
        qb, kb, vb, ib, lfb = xs
        b = jnp.cumsum(lfb, axis=-1)
        dmat = jnp.where(causal, b[..., :, None] - b[..., None, :] + ib[..., None, :], -jnp.inf)
        inter = b + m[..., None]
        m_t = jnp.maximum(inter, dmat.max(-1))
        decay_in = jnp.exp(dmat - m_t[..., None])
        w_inter = jnp.exp(inter - m_t)
        s = jnp.einsum('bhjd,bhsd->bhjs', qb, kb) * decay_in
        num = (jnp.einsum('bhjs,bhsv->bhjv', s, vb)
               + w_inter[..., None] * jnp.einsum('bhjd,bhdv->bhjv', qb, C))
        den = s.sum(-1) + w_inter * jnp.einsum('bhjd,bhd->bhj', qb, n)
        h = num / jnp.maximum(jnp.abs(den), jnp.exp(-m_t))[..., None]
        b_last = b[..., -1]
        g = b_last[..., None] - b + ib
        m_new = jnp.maximum(b_last + m, g.max(-1))
        carry_decay = jnp.exp(b_last + m - m_new)
        wk = jnp.exp(g - m_new[..., None])[..., None] * kb
        C_new = carry_decay[..., None, None] * C + jnp.einsum('bhsd,bhsv->bhdv', wk, vb)
        n_new = carry_decay[..., None] * n + wk.sum(-2)
        return (C_new, n_new, m_new), h

    init = (jnp.zeros((B, H, Dk, Dv), f32), jnp.zeros((B, H, Dk), f32), jnp.zeros((B, H), f32))
    _, h = lax.scan(step, init, (qc, kc, vc, ic, lfc))
    return jnp.transpose(h, (1, 0, 3, 2, 4)).reshape(B, S, H, Dv)


def _hierarchical_moe(x, w_router_group, b_router_group, w_router_expert, b_router_expert,
                      w_exp_gate, w_exp_up, w_exp_down):
    B, S, D = x.shape
    f32 = jnp.float32
    t = x.reshape(B * S, D)
    g_logits = (t @ w_router_group + b_router_group).astype(f32)
    g_prob = jax.nn.softmax(g_logits, axis=-1)
    g_w, g_sel = lax.top_k(g_prob, 1)
    e_logits = (t @ w_router_expert + b_router_expert).astype(f32)
    e_logits = e_logits.reshape(-1, N_GROUPS, EXPERTS_PER_GROUP)
    e_logits = jnp.take_along_axis(e_logits, g_sel[:, :, None], axis=1)[:, 0]
    e_val, e_idx = lax.top_k(e_logits, TOP_K_IN_GROUP)
    e_w = jax.nn.softmax(e_val, axis=-1) * g_w
    expert_id = g_sel * EXPERTS_PER_GROUP + e_idx
    combine = jnp.einsum('tk,tke->te', e_w, jax.nn.one_hot(expert_id, N_EXPERTS, dtype=f32))
    out = jnp.zeros((B * S, D), f32)
    for e in range(N_EXPERTS):
        hdn = jax.nn.silu(t @ w_exp_gate[e]) * (t @ w_exp_up[e])
        out = out + combine[:, e:e + 1] * (hdn @ w_exp_down[e])
    return out.reshape(B, S, D)


def setup_inputs(seed: int = 0) -> dict:
    key = jax.random.key(seed)
    ks = jax.random.split(key, 20)
    f32 = jnp.float32

    def dense(k, shape, fan_in, scale=1.0):
        return jax.random.normal(k, shape, f32) * (scale * fan_in ** -0.5)

    x = jax.random.normal(ks[0], (BATCH, SEQ, D_MODEL), f32)
    w_in = dense(ks[1], (D_MODEL, IN_WIDTH), D_MODEL)
    f_off = sum(IN_SPLITS[:10])
    b_in = 0.02 * jax.random.normal(ks[2], (IN_WIDTH,), f32)
    b_in = b_in.at[f_off:f_off + MLSTM_HEADS].add(jnp.linspace(3.0, 6.0, MLSTM_HEADS, dtype=f32))
    conv_m = dense(ks[3], (CONV_WIDTH, 2 * MLSTM_QK_W), CONV_WIDTH)
    gn_m_gain = 1.0 + 0.02 * jax.random.normal(ks[4], (MLSTM_V_W,), f32)
    w_branch_attn = dense(ks[5], (ATT_W, D_MODEL), ATT_W)
    w_branch_mlstm = dense(ks[6], (MLSTM_V_W, D_MODEL), MLSTM_V_W)
    w_out = dense(ks[7], (D_MODEL, D_MODEL), D_MODEL, DEEPNORM_BETA)
    ln1_gain = 1.0 + 0.02 * jax.random.normal(ks[8], (D_MODEL,), f32)
    ln1_bias = 0.02 * jax.random.normal(ks[9], (D_MODEL,), f32)
    w_router_group = dense(ks[10], (D_MODEL, N_GROUPS), D_MODEL)
    b_router_group = 0.01 * jax.random.normal(ks[11], (N_GROUPS,), f32)
    w_router_expert = dense(ks[12], (D_MODEL, N_EXPERTS), D_MODEL)
    b_router_expert = 0.01 * jax.random.normal(ks[13], (N_EXPERTS,), f32)
    w_exp_gate = dense(ks[14], (N_EXPERTS, D_MODEL, EXPERT_DIM), D_MODEL)
    w_exp_up = dense(ks[15], (N_EXPERTS, D_MODEL, EXPERT_DIM), D_MODEL)
    w_exp_down = dense(ks[16], (N_EXPERTS, EXPERT_DIM, D_MODEL), EXPERT_DIM, DEEPNORM_BETA)
    ln2_gain = 1.0 + 0.02 * jax.random.normal(ks[17], (D_MODEL,), f32)
    ln2_bias = 0.02 * jax.random.normal(ks[18], (D_MODEL,), f32)
    return {'x': x, 'w_in': w_in, 'b_in': b_in, 'conv_m': conv_m, 'gn_m_gain': gn_m_gain,
            'w_branch_attn': w_branch_attn, 'w_branch_mlstm': w_branch_mlstm, 'w_out': w_out,
            'ln1_gain': ln1_gain, 'ln1_bias': ln1_bias,
            'w_router_group': w_router_group, 'b_router_group': b_router_group,
            'w_router_expert': w_router_expert, 'b_router_expert': b_router_expert,
            'w_exp_gate': w_exp_gate, 'w_exp_up': w_exp_up, 'w_exp_down': w_exp_down,
            'ln2_gain': ln2_gain, 'ln2_bias': ln2_bias}


def reference(x, w_in, b_in, conv_m, gn_m_gain, w_branch_attn, w_branch_mlstm, w_out,
              ln1_gain, ln1_bias, w_router_group, b_router_group, w_router_expert,
              b_router_expert, w_exp_gate, w_exp_up, w_exp_down, ln2_gain, ln2_bias):
    B, S, _ = x.shape
    splits = np.cumsum(IN_SPLITS)[:-1].tolist()
    for _layer in range(DEPTH):
        proj = jnp.einsum('bsd,de->bse', x, w_in) + b_in
        (a_q, a_k, a_v, i_q, i_k, i_w, m_q, m_k, m_v, m_i, m_f, m_o,
         g_attn, g_mlstm) = jnp.split(proj, splits, axis=-1)

        cos_a, sin_a = _rope_tables(S, ATT_HEAD_DIM)
        cos_i, sin_i = _rope_tables(S, IDX_DIM)
        q = _rope(a_q.reshape(B, S, ATT_HEADS, ATT_HEAD_DIM), cos_a, sin_a)
        k = _rope(a_k.reshape(B, S, ATT_HEADS, ATT_HEAD_DIM), cos_a, sin_a)
        v = a_v.reshape(B, S, ATT_HEADS, ATT_HEAD_DIM)
        qi = _rope(i_q.reshape(B, S, IDX_HEADS, IDX_DIM), cos_i, sin_i)
        ki = _rope(i_k.reshape(B, S, 1, IDX_DIM), cos_i, sin_i)[:, :, 0]
        wi = i_w * (IDX_HEADS ** -0.5 * IDX_DIM ** -0.5)
        y_attn = _dsa_sparse_attention(q, k, v, qi, ki, wi)

        qk_m = jax.nn.silu(_causal_depthwise_conv(jnp.concatenate([m_q, m_k], axis=-1), conv_m))
        mq, mk = jnp.split(qk_m, 2, axis=-1)
        h = _mlstm_chunkwise(mq.reshape(B, S, MLSTM_HEADS, MLSTM_QK_DIM),
                             mk.reshape(B, S, MLSTM_HEADS, MLSTM_QK_DIM),
                             m_v.reshape(B, S, MLSTM_HEADS, MLSTM_V_DIM), m_i, m_f)
        mu = h.mean(-1, keepdims=True)
        var = jnp.square(h - mu).mean(-1, keepdims=True)
        h = ((h - mu) * lax.rsqrt(var + GN_EPS)).reshape(B, S, MLSTM_V_W) * gn_m_gain
        y_mlstm = jax.nn.sigmoid(m_o) * h

        merged = (jax.nn.sigmoid(g_attn) * (y_attn @ w_branch_attn)
                  + jax.nn.sigmoid(g_mlstm) * (y_mlstm @ w_branch_mlstm))
        x = _layer_norm(DEEPNORM_ALPHA * x + merged @ w_out, ln1_gain, ln1_bias)

        moe = _hierarchical_moe(x, w_router_group, b_router_group, w_router_expert,
                                b_router_expert, w_exp_gate, w_exp_up, w_exp_down)
        x = _layer_norm(DEEPNORM_ALPHA * x + moe, ln2_gain, ln2_bias)
    return x
```

```python
from contextlib import ExitStack

import numpy as np
import concourse.bass as bass
import concourse.mybir as mybir
from concourse.bass_utils import run_bass_kernel_spmd

F32 = mybir.dt.float32
BF16 = mybir.dt.bfloat16
U32 = mybir.dt.uint32
U8 = mybir.dt.uint8
F8 = mybir.dt.float8e5
AF = mybir.ActivationFunctionType
ALU = mybir.AluOpType
AX = mybir.AxisListType

D = 1024
S = 8192
NPAIR = 32
NOWN = 4096
BIG = 30000.0
NEG = -1.0e30
import os
NIT = int(os.environ.get('K_NIT', '13'))
TOPK = 256
NBLK = int(os.environ.get('K_NBLK', '32'))

EPOCH = 16000
NEPOCH = 8
NSLOT = 8
ENGS = ("pe", "act", "dve", "pool", "sp")
DMAQ = ("sp", "pool")


class Res:
    __slots__ = ("name", "w", "r", "psum")

    def __init__(self, name="", psum=False):
        self.name = name
        self.w = None
        self.r = {}
        self.psum = psum


class Tl:
    def __init__(self, t, name, psum=False):
        self.t = t
        self.res = Res(name, psum)

    def __getitem__(self, k):
        return self.t[k]


class Prog:
    def __init__(self, nc, es):
        self.nc = nc
        self.q = {e: [] for e in ENGS}
        self.tr = {e: [] for e in ENGS}
        self.cnt = {e: 0 for e in ENGS}
        self.dcnt = {q: 0 for q in DMAQ}
        self.known = {e: {} for e in ENGS}
        self.esem = {e: [es.enter_context(nc.semaphore(f"s_{e}{k}")) for k in range(NEPOCH)] for e in ENGS}
        self.dsem = {q: [es.enter_context(nc.semaphore(f"d_{q}{k}")) for k in range(NSLOT)] for q in DMAQ}

    def _wait(self, eng, sp):
        if sp[0] == "E":
            _, e2, seq = sp
            if e2 == eng and eng == "pe":
                return
            key = ("E", e2)
            if self.known[eng].get(key, 0) >= seq:
                return
            self.known[eng][key] = seq
            sem = self.esem[e2][(seq - 1) // EPOCH]
            val = (seq - 1) % EPOCH + 1
        else:
            _, q, idx = sp
            slot = idx % NSLOT
            need = idx // NSLOT + 1
            key = ("D", q, slot)
            if self.known[eng].get(key, 0) >= need:
                return
            self.known[eng][key] = need
            sem = self.dsem[q][slot]
            val = 16 * need
        self.q[eng].append(lambda E, sem=sem, val=val: E.wait_ge(sem, val))
        self.tr[eng].append(('wait', id(sem), val))

    def _deps(self, eng, reads, writes):
        for r in reads:
            if r.w is not None:
                self._wait(eng, r.w)
            if r.psum:
                for key, v in r.r.items():
                    if key[0] == "E" and key[1] != eng:
                        self._wait(eng, ("E", key[1], v))
        for w in writes:
            if w.w is not None:
                self._wait(eng, w.w)
            for key, v in w.r.items():
                self._wait(eng, ("E", key[1], v) if key[0] == "E" else ("D", key[1], v))

    def _mark(self, me, reads, writes):
        key = ("E", me[1]) if me[0] == "E" else ("D", me[1], me[2] % NSLOT)
        for r in reads:
            if r.r.get(key, -1) < me[2]:
                r.r[key] = me[2]
        for w in writes:
            w.w = me
            w.r = {}

    def op(self, eng, fn, reads=(), writes=(), inc=True):
        self._deps(eng, reads, writes)
        if not inc:
            assert eng == "pe"
            self.q[eng].append(lambda E, fn=fn: fn(E))
            self._mark(("E", eng, self.cnt[eng] + 1), reads, writes)
            return
        self.cnt[eng] += 1
        seq = self.cnt[eng]
        assert seq <= EPOCH * NEPOCH, f"too many instructions on {eng}"
        sem = self.esem[eng][(seq - 1) // EPOCH]
        self.q[eng].append(lambda E, fn=fn, sem=sem: fn(E).then_inc(sem, 1))
        self.tr[eng].append(('inc', id(sem), 1))
        self._mark(("E", eng, seq), reads, writes)

    def dma(self, q, out, in_, reads=(), writes=()):
        self._deps(q, reads, writes)
        idx = self.dcnt[q]
        self.dcnt[q] += 1
        if idx >= NSLOT:
            self._wait(q, ("D", q, idx - NSLOT))
        sem = self.dsem[q][idx % NSLOT]
        self.q[q].append(lambda E, out=out, in_=in_, sem=sem: E.dma_start(out=out, in_=in_).then_inc(sem, 16))
        self.tr[q].append(('inc', id(sem), 16))
        self._mark(("D", q, idx), reads, writes)

    def barrier(self):
        for e in ENGS:
            for e2 in ENGS:
                if self.cnt[e2] > 0:
                    self._wait(e, ("E", e2, self.cnt[e2]))
            for q in DMAQ:
                n = self.dcnt[q]
                for idx in range(max(0, n - NSLOT), n):
                    self._wait(e, ("D", q, idx))

    def finish(self):
        for q in DMAQ:
            n = self.dcnt[q]
            for idx in range(max(0, n - NSLOT), n):
                self._wait("sp", ("D", q, idx))
        for e in ENGS:
            if e != "sp" and self.cnt[e] > 0:
                self._wait("sp", ("E", e, self.cnt[e]))

    def emit(self, block):
        def mk(name):
            def f(E):
                for c in self.q[name]:
                    c(E)
            return f
        block.tensor(mk("pe"))
        block.scalar(mk("act"))
        block.vector(mk("dve"))
        block.gpsimd(mk("pool"))
        block.sync(mk("sp"))


def _sb(nc, ph, name, shape, dtype):
    return Tl(ph.enter_context(nc.sbuf_tensor("sb_" + name, list(shape), dtype)), name)


def _ps(nc, ph, name, shape, dtype):
    return Tl(ph.enter_context(nc.psum_tensor("pp_" + name, list(shape), dtype)), name, psum=True)


def _layer_norm(P, z, outt, junk, g_bc, b_bc, st, eps):
    n = float(D)
    P.op("act", lambda E: E.activation(out=junk[:], in_=z[:], func=AF.Identity, accum_out=st[:, 0:1]),
         reads=[z.res], writes=[junk.res, st.res])
    P.op("act", lambda E: E.activation(out=junk[:], in_=z[:], func=AF.Square, accum_out=st[:, 1:2]),
         reads=[z.res], writes=[junk.res, st.res])
    P.op("dve", lambda E: E.tensor_scalar(out=st[:, 2:3], in0=st[:, 0:1], scalar1=-1.0 / n, scalar2=None, op0=ALU.mult),
         reads=[st.res], writes=[st.res])
    P.op("dve", lambda E: E.tensor_tensor(out=st[:, 3:4], in0=st[:, 2:3], in1=st[:, 2:3], op=ALU.mult),
         reads=[st.res], writes=[st.res])
    P.op("dve", lambda E: E.scalar_tensor_tensor(out=st[:, 4:5], in0=st[:, 1:2], scalar=1.0 / n, in1=st[:, 3:4],
                                                  op0=ALU.mult, op1=ALU.subtract),
         reads=[st.res], writes=[st.res])
    P.op("dve", lambda E: E.tensor_scalar(out=st[:, 4:5], in0=st[:, 4:5], scalar1=eps, scalar2=None, op0=ALU.add),
         reads=[st.res], writes=[st.res])
    P.op("act", lambda E: E.activation(out=st[:, 5:6], in_=st[:, 4:5], func=AF.Sqrt),
         reads=[st.res], writes=[st.res])
    P.op("dve", lambda E: E.reciprocal(out=st[:, 6:7], in_=st[:, 5:6]), reads=[st.res], writes=[st.res])
    P.op("dve", lambda E: E.tensor_scalar(out=outt[:], in0=z[:], scalar1=st[:, 2:3], scalar2=st[:, 6:7],
                                          op0=ALU.add, op1=ALU.mult),
         reads=[z.res, st.res], writes=[outt.res])
    P.op("dve", lambda E: E.tensor_tensor(out=outt[:], in0=outt[:], in1=g_bc[:], op=ALU.mult),
         reads=[outt.res, g_bc.res], writes=[outt.res])
    P.op("dve", lambda E: E.tensor_tensor(out=outt[:], in0=outt[:], in1=b_bc[:], op=ALU.add),
         reads=[outt.res, b_bc.res], writes=[outt.res])


def _wview(ap, p=128):
    return ap.rearrange("(c p) n -> p c n", p=p)


def _phase_mlstm(P, nc, Dm, ymT_s):
    with ExitStack() as ph:
        sb = lambda n, shp, dt: _sb(nc, ph, "m_" + n, shp, dt)
        wmq, wmk = sb("wmq", [128, 8, 512], BF16), sb("wmk", [128, 8, 512], BF16)
        wmv, wmo = sb("wmv", [128, 8, D], BF16), sb("wmo", [128, 8, D], BF16)
        wmif = sb("wmif", [128, 8, 16], BF16)
        for t, n in ((wmq, "wmq"), (wmk, "wmk"), (wmv, "wmv"), (wmo, "wmo"), (wmif, "wmif")):
            P.dma("pool", t[:], _wview(Dm[n]), writes=[t.res])
        bmq, bmk = sb("bmq", [128, 4], F32), sb("bmk", [128, 4], F32)
        cwq, cwk = sb("cwq", [128, 16], F32), sb("cwk", [128, 16], F32)
        bmv, bmo, gnb = sb("bmv", [128, D], F32), sb("bmo", [128, D], F32), sb("gnb", [128, D], F32)
        bif = sb("bif", [128, 16], F32)
        tri, ones, identf = sb("tri", [128, 128], F32), sb("ones", [128, 128], F32), sb("identf", [128, 128], F32)
        identb = sb("identb", [128, 128], BF16)
        cmk, cmkT = sb("cmk", [128, 1024], F32), sb("cmkT", [128, 1024], F32)
        vflag, ineg = sb("vflag", [128, 1], F32), sb("ineg", [128, 1], F32)
        for t, n in ((bmq, "bmq"), (bmk, "bmk"), (cwq, "cwq"), (cwk, "cwk"), (bmv, "bmv_bc"), (bmo, "bmo_bc"),
                     (gnb, "gn_bc"), (bif, "bif_bc"), (tri, "tri"), (ones, "ones"), (identf, "ident"),
                     (cmk, "cmask_rep"), (cmkT, "cmaskT_rep"), (vflag, "vflag"), (ineg, "ineg")):
            P.dma("sp", t[:], Dm[n][:, :], writes=[t.res])
        P.dma("pool", identb[:], Dm["ident"][:, :], writes=[identb.res])
        xg = sb("xg", [128, 8, 512], BF16)
        cb = [sb(f"cb{j}", [128, 515], F32) for j in range(8)]
        ctmp = [sb(f"ctmp{j}", [128, 512], F32) for j in range(2)]
        mqT, mkT = sb("mqT", [128, 4, 512], BF16), sb("mkT", [128, 4, 512], BF16)
        vaug = [sb(f"vaug{j}", [128, 8, 129], BF16) for j in range(4)]
        gt = sb("gt", [128, 16], F32)
        ef = sb("ef", [128, 8], F32)
        lf = sb("lf", [128, 8], F32)
        bt = sb("bt", [128, 16], F32)
        av = sb("av", [128, 8], F32)
        col = sb("col", [128, 12, 8], F32)
        diag = sb("diag", [128, 8, 128], F32)
        Am = sb("Am", [128, 8, 128], F32)
        DT = sb("DT", [128, 8, 128], F32)
        PT = sb("PT", [128, 1024], BF16)
        og = sb("og", [128, D], F32)
        n1s = sb("n1s", [128, 129], F32)
        nums = sb("nums", [128, 129], F32)
        sc1 = sb("sc1", [128, 4], F32)
        hbuf = sb("hbuf", [128, 8, 128], F32)
        sqt = sb("sqt", [128, 8, 128], F32)
        ybf = sb("ybf", [128, D], BF16)
        yT = sb("yT", [128, 8, 128], BF16)
        wk = sb("wk", [128, 8, 64], BF16)
        Cst = sb("Cst", [128, 4, 129], F32)
        Cbf = sb("Cbf", [128, 4, 129], BF16)
        mprev = sb("mprev", [128, 8], F32)
        pj = [_ps(nc, ph, f"m_pj{j}", [128, 512], F32) for j in range(2)]
        pbc = _ps(nc, ph, "m_pbc", [128, 1024], F32)
        psS = _ps(nc, ph, "m_psS", [128, 1024], F32)
        pT = _ps(nc, ph, "m_pT", [128, 1024], BF16)
        CMAX, INTER, MT, RR, WINT, EMT, AMX, MNB, MNEW, CD, ES, TMP = range(12)
        diag_r = [Res(f"diag{h}") for h in range(8)]
        DT_r = [Res(f"DT{h}") for h in range(8)]
        hbuf_r = [Res(f"hbuf{h}") for h in range(8)]
        wk_r = [Res(f"wk{h}") for h in range(8)]
        Cst_r = [Res(f"Cst{h}") for h in range(8)]
        for j in range(8):
            P.op("pool", lambda E, j=j: E.memset(cb[j][:, 0:3], 0.0), writes=[cb[j].res])
        for j in range(4):
            P.op("pool", lambda E, j=j: E.memset(vaug[j][:, :, 128:129], 1.0), writes=[vaug[j].res])
        P.op("pool", lambda E: E.memset(Cst[:], 0.0), writes=Cst_r)
        P.op("pool", lambda E: E.memset(Cbf[:], 0.0), writes=[Cbf.res])
        P.op("pool", lambda E: E.memset(mprev[:], 0.0), writes=[mprev.res])
        xTa = Dm["xT"].rearrange("(c p) t -> p c t", p=128)
        hcol = lambda h: (h % 2) * 4 + h // 2

        def c8(idx):
            return col[:, idx, :]

        for g in range(S // 512):
            sl = slice(g * 512, (g + 1) * 512)
            P.dma("pool", xg[:], xTa[:, :, sl], writes=[xg.res])
            for c in range(8):
                isq = c < 4
                W, Bt, CW, OUT = (wmq, bmq, cwq, mqT) if isq else (wmk, bmk, cwk, mkT)
                cc = c % 4
                A = pj[c % 2]
                CB = cb[c]
                T = ctmp[c % 2]
                for k in range(8):
                    P.op("pe", lambda E, A=A, W=W, k=k, cc=cc: E.matmul(
                        A[:], W[:, k, cc * 128:(cc + 1) * 128], xg[:, k, :], start=(k == 0), stop=(k == 7)),
                        reads=[W.res, xg.res], writes=[A.res], inc=(k == 7))
                P.op("dve", lambda E, CB=CB, A=A, Bt=Bt, cc=cc: E.tensor_scalar(
                    out=CB[:, 3:515], in0=A[:], scalar1=Bt[:, cc:cc + 1], scalar2=None, op0=ALU.add),
                    reads=[A.res, Bt.res], writes=[CB.res])
                if g == 0:
                    P.op("dve", lambda E, CB=CB: E.tensor_scalar(
                        out=CB[:, 3:131], in0=CB[:, 3:131], scalar1=vflag[:, 0:1], scalar2=None, op0=ALU.mult),
                        reads=[CB.res, vflag.res], writes=[CB.res])
                P.op("dve", lambda E, T=T, CB=CB, CW=CW, cc=cc: E.tensor_scalar(
                    out=T[:], in0=CB[:, 0:512], scalar1=CW[:, cc * 4:cc * 4 + 1], scalar2=None, op0=ALU.mult),
                    reads=[CB.res, CW.res], writes=[T.res])
                for j in range(1, 4):
                    P.op("dve", lambda E, T=T, CB=CB, CW=CW, cc=cc, j=j: E.scalar_tensor_tensor(
                        out=T[:], in0=CB[:, j:j + 512], scalar=CW[:, cc * 4 + j:cc * 4 + j + 1], in1=T[:],
                        op0=ALU.mult, op1=ALU.add),
                        reads=[CB.res, CW.res, T.res], writes=[T.res])
                P.op("pool", lambda E, CB=CB: E.tensor_copy(out=CB[:, 0:3], in_=CB[:, 512:515]),
                     reads=[CB.res], writes=[CB.res])
                if isq:
                    P.op("act", lambda E, T=T: E.activation(out=T[:], in_=T[:], func=AF.Silu),
                         reads=[T.res], writes=[T.res])
                    P.op("pool", lambda E, T=T, cc=cc: E.tensor_scalar(
                        out=mqT[:, cc, :], in0=T[:], scalar1=0.125, scalar2=None, op0=ALU.mult),
                        reads=[T.res], writes=[mqT.res])
                else:
                    P.op("act", lambda E, T=T, cc=cc: E.activation(out=mkT[:, cc, :], in_=T[:], func=AF.Silu),
                         reads=[T.res], writes=[mkT.res])
            for blk in range(4):
                bsl = slice(blk * 128, (blk + 1) * 128)
                VA = vaug[blk]
                for half in range(2):
                    A = pj[half]
                    for k in range(8):
                        P.op("pe", lambda E, A=A, k=k, bsl=bsl, half=half: E.matmul(
                            A[:], xg[:, k, bsl], wmv[:, k, half * 512:(half + 1) * 512], start=(k == 0), stop=(k == 7)),
                            reads=[xg.res, wmv.res], writes=[A.res], inc=(k == 7))
                    P.op("dve", lambda E, A=A, VA=VA, half=half: E.tensor_tensor(
                        out=VA[:, half * 4:(half + 1) * 4, 0:128], in0=A[:].rearrange("p (h d) -> p h d", h=4),
                        in1=bmv[:, half * 512:(half + 1) * 512].rearrange("p (h d) -> p h d", h=4), op=ALU.add),
                        reads=[A.res, bmv.res], writes=[VA.res])
            for blk in range(4):
                pb = g * 4 + blk
                own = (pb % 2 == 1)
                bsl = slice(blk * 128, (blk + 1) * 128)
                VA = vaug[blk]
                for k in range(8):
                    P.op("pe", lambda E, k=k, bsl=bsl: E.matmul(
                        pj[0][:, 0:16], xg[:, k, bsl], wmif[:, k, :], start=(k == 0), stop=(k == 7)),
                        reads=[xg.res, wmif.res], writes=[pj[0].res])
                P.op("dve", lambda E: E.tensor_tensor(out=gt[:], in0=pj[0][:, 0:16], in1=bif[:], op=ALU.add),
                     reads=[pj[0].res, bif.res], writes=[gt.res])
                P.op("act", lambda E: E.activation(out=ef[:], in_=gt[:, 8:16], func=AF.Exp, scale=-1.0),
                     reads=[gt.res], writes=[ef.res])
                P.op("act", lambda E: E.activation(out=ef[:], in_=ef[:], func=AF.Ln, bias=1.0),
                     reads=[ef.res], writes=[ef.res])
                P.op("dve", lambda E: E.tensor_scalar(out=lf[:], in0=ef[:], scalar1=-1.0, scalar2=None, op0=ALU.mult),
                     reads=[ef.res], writes=[lf.res])
                if pb == 0:
                    P.op("dve", lambda E: E.tensor_scalar(out=lf[:], in0=lf[:], scalar1=vflag[:, 0:1], scalar2=None, op0=ALU.mult),
                         reads=[lf.res, vflag.res], writes=[lf.res])
                    P.op("dve", lambda E: E.tensor_scalar(out=gt[:, 0:8], in0=gt[:, 0:8], scalar1=vflag[:, 0:1],
                                                          scalar2=ineg[:, 0:1], op0=ALU.mult, op1=ALU.add),
                         reads=[gt.res, vflag.res, ineg.res], writes=[gt.res])
                if own:
                    for half in range(2):
                        A = pj[1]
                        hs = slice(half * 512, (half + 1) * 512)
                        for k in range(8):
                            P.op("pe", lambda E, A=A, k=k, bsl=bsl, hs=hs: E.matmul(
                                A[:], xg[:, k, bsl], wmo[:, k, hs], start=(k == 0), stop=(k == 7)),
                                reads=[xg.res, wmo.res], writes=[A.res], inc=(k == 7))
                        P.op("dve", lambda E, A=A, hs=hs: E.tensor_tensor(out=og[:, hs], in0=A[:], in1=bmo[:, hs], op=ALU.add),
                             reads=[A.res, bmo.res], writes=[og.res])
                    P.op("act", lambda E: E.activation(out=og[:], in_=og[:], func=AF.Exp, scale=-1.0), reads=[og.res], writes=[og.res])
                    P.op("dve", lambda E: E.tensor_scalar(out=og[:], in0=og[:], scalar1=1.0, scalar2=None, op0=ALU.add),
                         reads=[og.res], writes=[og.res])
                    P.op("dve", lambda E: E.reciprocal(out=og[:], in_=og[:]), reads=[og.res], writes=[og.res])
                P.op("pe", lambda E: E.matmul(pj[1][:, 0:8], tri[:], lf[:], start=True, stop=True),
                     reads=[tri.res, lf.res], writes=[pj[1].res])
                P.op("pe", lambda E: E.matmul(pj[1][:, 8:16], ones[:], lf[:], start=True, stop=True),
                     reads=[ones.res, lf.res], writes=[pj[1].res])
                P.op("dve", lambda E: E.tensor_copy(out=bt[:], in_=pj[1][:, 0:16]), reads=[pj[1].res], writes=[bt.res])
                P.op("dve", lambda E: E.tensor_tensor(out=av[:], in0=gt[:, 0:8], in1=bt[:, 0:8], op=ALU.subtract),
                     reads=[gt.res, bt.res], writes=[av.res])
                for h in range(8):
                    P.op("dve", lambda E, h=h: E.tensor_scalar(
                        out=diag[:, h, :], in0=identf[:], scalar1=av[:, h:h + 1], scalar2=None, op0=ALU.mult),
                        reads=[identf.res, av.res], writes=[diag_r[h]])
                for half in range(2):
                    P.op("pe", lambda E, half=half: E.matmul(
                        pbc[:, half * 512:(half + 1) * 512], ones[:],
                        diag[:, half * 4:(half + 1) * 4, :].rearrange("p h s -> p (h s)"), start=True, stop=True),
                        reads=[ones.res] + diag_r[half * 4:(half + 1) * 4], writes=[pbc.res])
                P.op("dve", lambda E: E.tensor_reduce(out=c8(AMX), in_=pbc[:].rearrange("p (h s) -> p h s", h=8), axis=AX.X, op=ALU.max),
                     reads=[pbc.res], writes=[col.res])
                P.op("dve", lambda E: E.tensor_tensor(out=c8(MNB), in0=c8(AMX), in1=mprev[:], op=ALU.max),
                     reads=[col.res, mprev.res], writes=[col.res])
                if own:
                    P.op("dve", lambda E: E.tensor_tensor(out=Am[:].rearrange("p h s -> p (h s)"), in0=pbc[:], in1=cmk[:], op=ALU.add),
                         reads=[pbc.res, cmk.res], writes=[Am.res])
                    P.op("dve", lambda E: E.tensor_reduce(out=c8(CMAX), in_=Am[:], axis=AX.X, op=ALU.max),
                         reads=[Am.res], writes=[col.res])
                    P.op("dve", lambda E: E.tensor_tensor(out=c8(INTER), in0=bt[:, 0:8], in1=mprev[:], op=ALU.add),
                         reads=[bt.res, mprev.res], writes=[col.res])
                    P.op("dve", lambda E: E.tensor_tensor(out=c8(MT), in0=bt[:, 0:8], in1=c8(CMAX), op=ALU.add),
                         reads=[bt.res, col.res], writes=[col.res])
                    P.op("dve", lambda E: E.tensor_tensor(out=c8(MT), in0=c8(MT), in1=c8(INTER), op=ALU.max),
                         reads=[col.res], writes=[col.res])
                    P.op("dve", lambda E: E.tensor_tensor(out=c8(RR), in0=bt[:, 0:8], in1=c8(MT), op=ALU.subtract),
                         reads=[bt.res, col.res], writes=[col.res])
                    P.op("dve", lambda E: E.tensor_tensor(out=c8(TMP), in0=c8(INTER), in1=c8(MT), op=ALU.subtract),
                         reads=[col.res], writes=[col.res])
                    P.op("act", lambda E: E.activation(out=c8(WINT), in_=c8(TMP), func=AF.Exp), reads=[col.res], writes=[col.res])
                    P.op("act", lambda E: E.activation(out=c8(EMT), in_=c8(MT), func=AF.Exp, scale=-1.0), reads=[col.res], writes=[col.res])
                    for h in range(8):
                        P.op("dve", lambda E, h=h: E.tensor_scalar(
                            out=diag[:, h, :], in0=identf[:], scalar1=col[:, RR, h:h + 1], scalar2=None, op0=ALU.mult),
                            reads=[identf.res, col.res], writes=[diag_r[h]])
                    for half in range(2):
                        P.op("pe", lambda E, half=half: E.matmul(
                            pbc[:, half * 512:(half + 1) * 512], ones[:],
                            diag[:, half * 4:(half + 1) * 4, :].rearrange("p h s -> p (h s)"), start=True, stop=True),
                            reads=[ones.res] + diag_r[half * 4:(half + 1) * 4], writes=[pbc.res])
                    P.op("dve", lambda E: E.tensor_tensor(out=Am[:].rearrange("p h s -> p (h s)"), in0=pbc[:], in1=cmkT[:], op=ALU.add),
                         reads=[pbc.res, cmkT.res], writes=[Am.res])
                    for h in range(8):
                        P.op("act", lambda E, h=h: E.activation(
                            out=DT[:, hcol(h), :], in_=Am[:, h, :], func=AF.Exp, bias=av[:, h:h + 1]),
                            reads=[Am.res, av.res], writes=[DT_r[h]])
                    for h in range(8):
                        hp = slice((h % 2) * 64, (h % 2) * 64 + 64)
                        c0 = hcol(h) * 128
                        P.op("pe", lambda E, h=h, hp=hp, c0=c0, bsl=bsl: E.matmul(
                            psS[:, c0:c0 + 128], mkT[hp, h // 2, bsl], mqT[hp, h // 2, bsl], start=True, stop=True),
                            reads=[mkT.res, mqT.res], writes=[psS.res])
                    P.op("dve", lambda E: E.tensor_tensor(out=PT[:], in0=psS[:], in1=DT[:].rearrange("p h s -> p (h s)"), op=ALU.mult),
                         reads=[psS.res] + DT_r, writes=[PT.res])
                    for h in range(8):
                        hp = slice((h % 2) * 64, (h % 2) * 64 + 64)
                        c0 = hcol(h) * 128
                        P.op("pe", lambda E, c0=c0, VA=VA, h=h: E.matmul(
                            pj[0][:, 0:129], PT[:, c0:c0 + 128], VA[:, h, :], start=True, stop=True),
                            reads=[PT.res, VA.res], writes=[pj[0].res])
                        P.op("pe", lambda E, hp=hp, h=h, bsl=bsl: E.matmul(
                            pj[1][:, 0:129], mqT[hp, h // 2, bsl], Cbf[hp, h // 2, :], start=True, stop=True),
                            reads=[mqT.res, Cbf.res], writes=[pj[1].res])
                        P.op("dve", lambda E: E.tensor_copy(out=n1s[:], in_=pj[0][:, 0:129]),
                             reads=[pj[0].res], writes=[n1s.res])
                        P.op("dve", lambda E, h=h: E.scalar_tensor_tensor(
                            out=nums[:], in0=pj[1][:, 0:129], scalar=col[:, WINT, h:h + 1], in1=n1s[:],
                            op0=ALU.mult, op1=ALU.add),
                            reads=[pj[1].res, col.res, n1s.res], writes=[nums.res])
                        P.op("dve", lambda E, h=h: E.tensor_tensor(out=sc1[:, 0:1], in0=nums[:, 128:129], in1=col[:, EMT, h:h + 1], op=ALU.max),
                             reads=[nums.res, col.res], writes=[sc1.res])
                        P.op("dve", lambda E: E.scalar_tensor_tensor(out=sc1[:, 1:2], in0=nums[:, 128:129], scalar=-1.0, in1=sc1[:, 0:1],
                                                                      op0=ALU.mult, op1=ALU.max),
                             reads=[nums.res, sc1.res], writes=[sc1.res])
                        P.op("dve", lambda E: E.reciprocal(out=sc1[:, 2:3], in_=sc1[:, 1:2]), reads=[sc1.res], writes=[sc1.res])
                        P.op("dve", lambda E, h=h: E.tensor_scalar(
                            out=hbuf[:, h, :], in0=nums[:, 0:128], scalar1=sc1[:, 2:3], scalar2=None, op0=ALU.mult),
                            reads=[nums.res, sc1.res], writes=[hbuf_r[h]])
                    P.op("dve", lambda E: E.tensor_reduce(out=c8(TMP), in_=hbuf[:], axis=AX.X, op=ALU.add),
                         reads=hbuf_r, writes=[col.res])
                    P.op("dve", lambda E: E.tensor_tensor(out=sqt[:], in0=hbuf[:], in1=hbuf[:], op=ALU.mult), reads=hbuf_r, writes=[sqt.res])
                    P.op("dve", lambda E: E.tensor_reduce(out=c8(CMAX), in_=sqt[:], axis=AX.X, op=ALU.add),
                         reads=[sqt.res], writes=[col.res])
                    P.op("dve", lambda E: E.tensor_scalar(out=c8(TMP), in0=c8(TMP), scalar1=-1.0 / 128.0, scalar2=None, op0=ALU.mult),
                         reads=[col.res], writes=[col.res])
                    P.op("dve", lambda E: E.tensor_tensor(out=c8(INTER), in0=c8(TMP), in1=c8(TMP), op=ALU.mult),
                         reads=[col.res], writes=[col.res])
                    P.op("dve", lambda E: E.scalar_tensor_tensor(out=c8(CMAX), in0=c8(CMAX), scalar=1.0 / 128.0, in1=c8(INTER),
                                                                  op0=ALU.mult, op1=ALU.subtract),
                         reads=[col.res], writes=[col.res])
                    P.op("dve", lambda E: E.tensor_scalar(out=c8(CMAX), in0=c8(CMAX), scalar1=GN_EPS, scalar2=None, op0=ALU.add),
                         reads=[col.res], writes=[col.res])
                    P.op("act", lambda E: E.activation(out=c8(CMAX), in_=c8(CMAX), func=AF.Ln), reads=[col.res], writes=[col.res])
                    P.op("act", lambda E: E.activation(out=c8(CMAX), in_=c8(CMAX), func=AF.Exp, scale=-0.5), reads=[col.res], writes=[col.res])
                    for h in range(8):
                        P.op("dve", lambda E, h=h: E.tensor_scalar(
                            out=hbuf[:, h, :], in0=hbuf[:, h, :], scalar1=col[:, TMP, h:h + 1], scalar2=col[:, CMAX, h:h + 1],
                            op0=ALU.add, op1=ALU.mult),
                            reads=[hbuf_r[h], col.res], writes=[hbuf_r[h]])
                    P.op("dve", lambda E: E.tensor_tensor(out=hbuf[:].rearrange("p h s -> p (h s)"),
                                                          in0=hbuf[:].rearrange("p h s -> p (h s)"), in1=gnb[:], op=ALU.mult),
                         reads=hbuf_r + [gnb.res], writes=hbuf_r)
                    P.op("dve", lambda E: E.tensor_tensor(out=ybf[:], in0=hbuf[:].rearrange("p h s -> p (h s)"), in1=og[:], op=ALU.mult),
                         reads=hbuf_r + [og.res], writes=[ybf.res])
                    for c in range(8):
                        P.op("pe", lambda E, c=c: E.transpose(out=pT[:, c * 128:(c + 1) * 128], in_=ybf[:, c * 128:(c + 1) * 128], identity=identb[:]),
                             reads=[ybf.res, identb.res], writes=[pT.res])
                    P.op("act", lambda E: E.activation(out=yT[:], in_=pT[:].rearrange("p (c t) -> p c t", c=8), func=AF.Identity),
                         reads=[pT.res], writes=[yT.res])
                    i = pb // 2
                    P.dma("sp", ymT_s[:, :, i * 128:(i + 1) * 128], yT[:], reads=[yT.res])
                P.op("dve", lambda E: E.tensor_tensor(out=c8(MNEW), in0=bt[:, 8:16], in1=c8(MNB), op=ALU.add),
                     reads=[bt.res, col.res], writes=[col.res])
                P.op("dve", lambda E: E.tensor_tensor(out=c8(CD), in0=bt[:, 8:16], in1=mprev[:], op=ALU.add),
                     reads=[bt.res, mprev.res], writes=[col.res])
                P.op("dve", lambda E: E.tensor_tensor(out=c8(CD), in0=c8(CD), in1=c8(MNEW), op=ALU.subtract),
                     reads=[col.res], writes=[col.res])
                P.op("act", lambda E: E.activation(out=c8(CD), in_=c8(CD), func=AF.Exp), reads=[col.res], writes=[col.res])
                P.op("dve", lambda E: E.tensor_tensor(out=c8(ES), in0=av[:], in1=bt[:, 8:16], op=ALU.add),
                     reads=[av.res, bt.res], writes=[col.res])
                P.op("dve", lambda E: E.tensor_tensor(out=c8(ES), in0=c8(ES), in1=c8(MNEW), op=ALU.subtract),
                     reads=[col.res], writes=[col.res])
                P.op("act", lambda E: E.activation(out=c8(ES), in_=c8(ES), func=AF.Exp), reads=[col.res], writes=[col.res])
                for c in range(4):
                    P.op("pe", lambda E, c=c, bsl=bsl: E.transpose(out=pT[:, c * 128:(c + 1) * 128], in_=mkT[:, c, bsl], identity=identb[:]),
                         reads=[mkT.res, identb.res], writes=[pT.res])
                for h in range(8):
                    P.op("dve", lambda E, h=h: E.tensor_scalar(
                        out=wk[:, h, :], in0=pT[:, h * 64:(h + 1) * 64], scalar1=col[:, ES, h:h + 1], scalar2=None, op0=ALU.mult),
                        reads=[pT.res, col.res], writes=[wk_r[h]])
                for h in range(8):
                    hp = slice((h % 2) * 64, (h % 2) * 64 + 64)
                    off = ((h // 2) // 2) * 512 + ((h // 2) % 2) * 129
                    P.op("pe", lambda E, h=h, hp=hp, off=off, VA=VA: E.matmul(
                        pbc[hp, off:off + 129], wk[:, h, :], VA[:, h, :], start=True, stop=True),
                        reads=[wk_r[h], VA.res], writes=[pbc.res])
                for h in range(8):
                    hp = slice((h % 2) * 64, (h % 2) * 64 + 64)
                    off = ((h // 2) // 2) * 512 + ((h // 2) % 2) * 129
                    P.op("dve", lambda E, h=h, hp=hp, off=off: E.scalar_tensor_tensor(
                        out=Cst[hp, h // 2, :], in0=Cst[hp, h // 2, :], scalar=col[hp, CD, h:h + 1], in1=pbc[hp, off:off + 129],
                        op0=ALU.mult, op1=ALU.add),
                        reads=[Cst_r[h], col.res, pbc.res], writes=[Cst_r[h]])
                P.op("act", lambda E: E.activation(out=Cbf[:], in_=Cst[:], func=AF.Identity), reads=Cst_r, writes=[Cbf.res])
                P.op("dve", lambda E: E.tensor_copy(out=mprev[:], in_=c8(MNEW)), reads=[col.res], writes=[mprev.res])
    P.barrier()


IN_SPECS = {
    "xT": ([D, S], F32), "xT_own": ([D, NOWN], F32), "x_own": ([NOWN, D], F32),
    "cosk": ([128, S], F32), "sink": ([128, S], F32),
    "cosq": ([128, NOWN], F32), "sinq": ([128, NOWN], F32),
    "cosi": ([128, NOWN], F32), "sini": ([128, NOWN], F32),
    "ident": ([128, 128], F32), "irep": ([128, 512], F32),
    "negm_diag": ([128, 128], F32), "negm_b0": ([128, 128], F32),
    "wq": ([D, 512], F32), "wq_s": ([D, 512], F32), "wk": ([D, 512], F32), "wk_s": ([D, 512], F32),
    "wv": ([D, 512], F32), "wiq": ([D, 256], F32), "wiq_s": ([D, 256], F32),
    "wik2": ([D, 128], F32), "wik2_s": ([D, 128], F32), "wiw": ([D, 4], F32),
    "bq": ([128, 4], F32), "bq_s": ([128, 4], F32), "bk": ([128, 4], F32), "bk_s": ([128, 4], F32),
    "biq": ([128, 2], F32), "biq_s": ([128, 2], F32), "bik2": ([128, 1], F32), "bik2_s": ([128, 1], F32),
    "bv_bc": ([128, 512], F32), "biw_bc": ([128, 4], F32),
    "wga": ([D, D], F32), "wgm": ([D, D], F32), "bga": ([128, 8], F32), "bgm": ([128, 8], F32),
    "wba": ([512, D], F32), "wbm": ([D, D], F32), "wout": ([D, D], F32),
    "ln1g_bc": ([128, D], F32), "ln1b_bc": ([128, D], F32), "ln2g_bc": ([128, D], F32), "ln2b_bc": ([128, D], F32),
    "wr": ([D, 20], F32), "br_bc": ([128, 20], F32),
    "weg": ([16, D, 512], F32), "weu": ([16, D, 512], F32), "wed": ([16, 512, D], F32),
    "wmq": ([D, 512], F32), "wmk": ([D, 512], F32), "bmq": ([128, 4], F32), "bmk": ([128, 4], F32),
    "cwq": ([128, 16], F32), "cwk": ([128, 16], F32),
    "wmv": ([D, D], F32), "bmv_bc": ([128, D], F32), "wmif": ([D, 16], F32), "bif_bc": ([128, 16], F32),
    "wmo": ([D, D], F32), "bmo_bc": ([128, D], F32), "gn_bc": ([128, D], F32),
    "tri": ([128, 128], F32), "ones": ([128, 128], F32),
    "cmask_rep": ([128, 1024], F32), "cmaskT_rep": ([128, 1024], F32),
    "vflag": ([128, 1], F32), "ineg": ([128, 1], F32),
    "pow2": ([128, 32], F32),
}
GN_EPS = 1e-6
C1SUB = os.environ.get("K_C1SUB", "")
ALPHA = float(2.0 ** 0.25)
LN_EPS = 1e-5


def build_nc(debug=(), stop=None):
    nc = bass.Bass("TRN2", target_bir_lowering=False)
    Dm = {}
    skip = set()
    if stop in ("q", "kv", "A1", "A2", "A3", "C1", "M", "A"):
        skip |= {"weg", "weu", "wed"}
    for name, (shape, dt) in IN_SPECS.items():
        if name in skip:
            continue
        Dm[name] = nc.dram_tensor(name, shape, dt, kind="ExternalInput").ap()
    nc._declared = set(Dm)

    def scratch(name, shape, dt):
        kind = "ExternalOutput" if name in debug else "Internal"
        return nc.dram_tensor(name, shape, dt, kind=kind).ap()

    QT_s = scratch("QT_s", [128, 4, NOWN], BF16)
    qiT_s = scratch("qiT_s", [128, 2, NOWN], BF16)
    yat_s = scratch("yat_s", [NOWN, 512], BF16)
    ymT_s = nc.dram_tensor("ymT_s", [128, 8, NOWN], BF16, kind=("ExternalInput" if "ymT_in" in debug else ("ExternalOutput" if "ymT_s" in debug else "Internal"))).ap()
    x1_s = scratch("x1_s", [NOWN, D], F32)
    x1T_s = scratch("x1T_s", [128, 8, NOWN], BF16)
    out = nc.dram_tensor("out", [NOWN, D], F32, kind="ExternalOutput").ap()

    es = ExitStack()
    with es:
        P = Prog(nc, es)
        top = ExitStack()
        es.enter_context(top)
        Wabs = _sb(nc, top, "Wabs", [128, NPAIR, 4], F32)
        Wsgn = _sb(nc, top, "Wsgn", [128, NPAIR, 4], F32)

        with ExitStack() as ph:
          if stop != 'M':
            wq = _sb(nc, ph, "wq", [128, 8, 512], BF16)
            wqs = _sb(nc, ph, "wqs", [128, 8, 512], BF16)
            wiq = _sb(nc, ph, "wiq", [128, 8, 256], BF16)
            wiqs = _sb(nc, ph, "wiqs", [128, 8, 256], BF16)
            wiw = _sb(nc, ph, "wiw", [128, 8, 4], BF16)
            bq = _sb(nc, ph, "bq", [128, 4], F32)
            bqs = _sb(nc, ph, "bqs", [128, 4], F32)
            biq = _sb(nc, ph, "biq", [128, 2], F32)
            biqs = _sb(nc, ph, "biqs", [128, 2], F32)
            biw = _sb(nc, ph, "biw", [128, 4], F32)
            for t, n in ((wq, "wq"), (wqs, "wq_s"), (wiq, "wiq"), (wiqs, "wiq_s"), (wiw, "wiw")):
                P.dma("pool", t[:], _wview(Dm[n]), writes=[t.res])
            for t, n in ((bq, "bq"), (bqs, "bq_s"), (biq, "biq"), (biqs, "biq_s"), (biw, "biw_bc")):
                P.dma("sp", t[:], Dm[n][:, :], writes=[t.res])
            xo = [_sb(nc, ph, f"xo{j}", [128, 8, 512], BF16) for j in range(2)]
            tabs = [[_sb(nc, ph, f"tab{j}_{n}", [128, 512], F32) for n in range(4)] for j in range(2)]
            t1 = [_sb(nc, ph, f"t1_{j}", [128, 512], F32) for j in range(2)]
            t2 = [_sb(nc, ph, f"t2_{j}", [128, 512], F32) for j in range(2)]
            qo = [_sb(nc, ph, f"qo{j}", [128, 4, 512], BF16) for j in range(2)]
            qio = [_sb(nc, ph, f"qio{j}", [128, 2, 512], BF16) for j in range(2)]
            wtmp = _sb(nc, ph, "wtmp", [128, 4], F32)
            psA = [_ps(nc, ph, f"psA{j}", [128, 512], F32) for j in range(3)]
            psB = [_ps(nc, ph, f"psB{j}", [128, 512], F32) for j in range(3)]
            psW = _ps(nc, ph, "psW", [128, 512], F32)
            xTo = Dm["xT_own"].rearrange("(c p) t -> p c t", p=128)
            rot = 0
            for g in range(NOWN // 512):
                sl = slice(g * 512, (g + 1) * 512)
                X = xo[g % 2]
                TB = tabs[g % 2]
                P.dma("pool", X[:], xTo[:, :, sl], writes=[X.res])
                for n, nm in enumerate(("cosq", "sinq", "cosi", "sini")):
                    P.dma("sp", TB[n][:], Dm[nm][:, sl], writes=[TB[n].res])
                QO, QIO = qo[g % 2], qio[g % 2]
                for c in range(6):
                    isq = c < 4
                    W, Ws, Bt, Bs = (wq, wqs, bq, bqs) if isq else (wiq, wiqs, biq, biqs)
                    cc = c if isq else c - 4
                    ct, st = (TB[0], TB[1]) if isq else (TB[2], TB[3])
                    A, B = psA[rot % 3], psB[rot % 3]
                    T1, T2 = t1[rot % 2], t2[rot % 2]
                    rot += 1
                    for k in range(8):
                        P.op("pe", lambda E, A=A, W=W, X=X, k=k, cc=cc: E.matmul(
                            A[:], W[:, k, cc * 128:(cc + 1) * 128], X[:, k, :], start=(k == 0), stop=(k == 7)),
                            reads=[W.res, X.res], writes=[A.res], inc=(k == 7))
                    for k in range(8):
                        P.op("pe", lambda E, B=B, Ws=Ws, X=X, k=k, cc=cc: E.matmul(
                            B[:], Ws[:, k, cc * 128:(cc + 1) * 128], X[:, k, :], start=(k == 0), stop=(k == 7)),
                            reads=[Ws.res, X.res], writes=[B.res], inc=(k == 7))
                    P.op("dve", lambda E, T1=T1, A=A, Bt=Bt, cc=cc, ct=ct: E.scalar_tensor_tensor(
                        out=T1[:], in0=A[:], scalar=Bt[:, cc:cc + 1], in1=ct[:], op0=ALU.add, op1=ALU.mult),
                        reads=[A.res, Bt.res, ct.res], writes=[T1.res])
                    P.op("dve", lambda E, T2=T2, B=B, Bs=Bs, cc=cc, st=st: E.scalar_tensor_tensor(
                        out=T2[:], in0=B[:], scalar=Bs[:, cc:cc + 1], in1=st[:], op0=ALU.add, op1=ALU.mult),
                        reads=[B.res, Bs.res, st.res], writes=[T2.res])
                    O = QO if isq else QIO
                    P.op("pool", lambda E, O=O, cc=cc, T1=T1, T2=T2: E.tensor_tensor(
                        out=O[:, cc, :], in0=T1[:], in1=T2[:], op=ALU.add),
                        reads=[T1.res, T2.res], writes=[O.res])
                P.dma("sp", QT_s[:, :, sl], QO[:], reads=[QO.res])
                P.dma("sp", qiT_s[:, :, sl], QIO[:], reads=[QIO.res])
                for blk in range(4):
                    i = g * 4 + blk
                    for k in range(8):
                        P.op("pe", lambda E, X=X, k=k, blk=blk: E.matmul(
                            psW[:, 0:4], X[:, k, blk * 128:(blk + 1) * 128], wiw[:, k, :],
                            start=(k == 0), stop=(k == 7)),
                            reads=[X.res, wiw.res], writes=[psW.res], inc=(k == 7))
                    P.op("dve", lambda E: E.tensor_tensor(out=wtmp[:], in0=psW[:, 0:4], in1=biw[:], op=ALU.add),
                         reads=[psW.res, biw.res], writes=[wtmp.res])
                    P.op("act", lambda E, i=i: E.activation(
                        out=Wabs[:, i, :], in_=wtmp[:], func=AF.Abs, scale=1.0 / 16.0),
                        reads=[wtmp.res], writes=[Wabs.res])
                    P.op("dve", lambda E, i=i: E.tensor_scalar(
                        out=Wsgn[:, i, :], in0=wtmp[:], scalar1=0.0, scalar2=2.0,
                        op0=ALU.is_ge, op1=ALU.mult),
                        reads=[wtmp.res], writes=[Wsgn.res])
                    P.op("dve", lambda E, i=i: E.tensor_scalar(
                        out=Wsgn[:, i, :], in0=Wsgn[:, i, :], scalar1=-1.0, scalar2=None, op0=ALU.add),
                        reads=[Wsgn.res], writes=[Wsgn.res])

        P.barrier()
        with ExitStack() as kv:
          if stop not in ('q', 'M'):
            KT = _sb(nc, kv, "KT", [128, 4, S], BF16)
            V = _sb(nc, kv, "V", [128, 64, 8, 65], BF16)
            kiT = _sb(nc, kv, "kiT", [128, S], BF16)
            KTr = [Res(f"KT{g}") for g in range(16)]
            Vr = [Res(f"V{g}") for g in range(16)]
            kir = [Res(f"ki{g}") for g in range(16)]
            P.op("pool", lambda E: E.memset(V[:, :, :, 64:65], 1.0), writes=Vr)
            with ExitStack() as ph:
                wk = _sb(nc, ph, "wk", [128, 8, 512], BF16)
                wks = _sb(nc, ph, "wks", [128, 8, 512], BF16)
                wv = _sb(nc, ph, "wv", [128, 8, 512], BF16)
                wik = _sb(nc, ph, "wik", [128, 8, 128], BF16)
                wiks = _sb(nc, ph, "wiks", [128, 8, 128], BF16)
                bk = _sb(nc, ph, "bk", [128, 4], F32)
                bks = _sb(nc, ph, "bks", [128, 4], F32)
                bik = _sb(nc, ph, "bik", [128, 1], F32)
                biks = _sb(nc, ph, "biks", [128, 1], F32)
                bv = _sb(nc, ph, "bv", [128, 512], F32)
                for t, n in ((wk, "wk"), (wks, "wk_s"), (wv, "wv"), (wik, "wik2"), (wiks, "wik2_s")):
                    P.dma("pool", t[:], _wview(Dm[n]), writes=[t.res])
                for t, n in ((bk, "bk"), (bks, "bk_s"), (bik, "bik2"), (biks, "bik2_s"), (bv, "bv_bc")):
                    P.dma("sp", t[:], Dm[n][:, :], writes=[t.res])
                xg = _sb(nc, ph, "xg", [128, 8, 512], BF16)
                ck = _sb(nc, ph, "ck", [128, 512], F32)
                sk = _sb(nc, ph, "sk", [128, 512], F32)
                t1 = [_sb(nc, ph, f"k_t1_{j}", [128, 512], F32) for j in range(2)]
                t2 = [_sb(nc, ph, f"k_t2_{j}", [128, 512], F32) for j in range(2)]
                psA = [_ps(nc, ph, f"kpsA{j}", [128, 512], F32) for j in range(3)]
                psB = [_ps(nc, ph, f"kpsB{j}", [128, 512], F32) for j in range(3)]
                psV = [_ps(nc, ph, f"kpsV{j}", [128, 512], F32) for j in range(2)]
                xTa = Dm["xT"].rearrange("(c p) t -> p c t", p=128)
                rot = 0
                for g in range(S // 512):
                    sl = slice(g * 512, (g + 1) * 512)
                    P.dma("pool", xg[:], xTa[:, :, sl], writes=[xg.res])
                    P.dma("sp", ck[:], Dm["cosk"][:, sl], writes=[ck.res])
                    P.dma("sp", sk[:], Dm["sink"][:, sl], writes=[sk.res])
                    for c in range(5):
                        isk = c < 4
                        W, Ws, Bt, Bs = (wk, wks, bk, bks) if isk else (wik, wiks, bik, biks)
                        cc = c if isk else 0
                        A, B = psA[rot % 3], psB[rot % 3]
                        T1, T2 = t1[rot % 2], t2[rot % 2]
                        rot += 1
                        for k in range(8):
                            P.op("pe", lambda E, A=A, W=W, k=k, cc=cc: E.matmul(
                                A[:], W[:, k, cc * 128:(cc + 1) * 128], xg[:, k, :], start=(k == 0), stop=(k == 7)),
                                reads=[W.res, xg.res], writes=[A.res], inc=(k == 7))
                        for k in range(8):
                            P.op("pe", lambda E, B=B, Ws=Ws, k=k, cc=cc: E.matmul(
                                B[:], Ws[:, k, cc * 128:(cc + 1) * 128], xg[:, k, :], start=(k == 0), stop=(k == 7)),
                                reads=[Ws.res, xg.res], writes=[B.res], inc=(k == 7))
                        P.op("dve", lambda E, T1=T1, A=A, Bt=Bt, cc=cc: E.scalar_tensor_tensor(
                            out=T1[:], in0=A[:], scalar=Bt[:, cc:cc + 1], in1=ck[:], op0=ALU.add, op1=ALU.mult),
                            reads=[A.res, Bt.res, ck.res], writes=[T1.res])
                        P.op("dve", lambda E, T2=T2, B=B, Bs=Bs, cc=cc: E.scalar_tensor_tensor(
                            out=T2[:], in0=B[:], scalar=Bs[:, cc:cc + 1], in1=sk[:], op0=ALU.add, op1=ALU.mult),
                            reads=[B.res, Bs.res, sk.res], writes=[T2.res])
                        if isk:
                            P.op("pool", lambda E, cc=cc, T1=T1, T2=T2, sl=sl: E.tensor_tensor(
                                out=KT[:, cc, sl], in0=T1[:], in1=T2[:], op=ALU.add),
                                reads=[T1.res, T2.res], writes=[KTr[g]])
                        else:
                            P.op("pool", lambda E, T1=T1, T2=T2, sl=sl: E.tensor_tensor(
                                out=kiT[:, sl], in0=T1[:], in1=T2[:], op=ALU.add),
                                reads=[T1.res, T2.res], writes=[kir[g]])
                    for blk in range(4):
                        pb = g * 4 + blk
                        PV = psV[blk % 2]
                        for k in range(8):
                            P.op("pe", lambda E, PV=PV, k=k, blk=blk: E.matmul(
                                PV[:], xg[:, k, blk * 128:(blk + 1) * 128], wv[:, k, :],
                                start=(k == 0), stop=(k == 7)),
                                reads=[xg.res, wv.res], writes=[PV.res], inc=(k == 7))
                        P.op("dve", lambda E, PV=PV, pb=pb: E.tensor_tensor(
                            out=V[:, pb, :, 0:64], in0=PV[:].rearrange("p (h d) -> p h d", h=8),
                            in1=bv[:].rearrange("p (h d) -> p h d", h=8), op=ALU.add),
                            reads=[PV.res, bv.res], writes=[Vr[g]])

            P.barrier()
            with ExitStack() as ph:
                score = _sb(nc, ph, "score", [128, S], F32)
                Mneg = _sb(nc, ph, "Mneg", [128, S], F8)
                junkt = _sb(nc, ph, "junkt", [128, S], U8)
                irep = _sb(nc, ph, "irep", [128, 512], BF16)
                nmd = _sb(nc, ph, "nmd", [128, 128], BF16)
                nm0 = _sb(nc, ph, "nm0", [128, 128], BF16)
                P.dma("pool", irep[:], Dm["irep"][:, :], writes=[irep.res])
                P.dma("pool", nmd[:], Dm["negm_diag"][:, :], writes=[nmd.res])
                P.dma("pool", nm0[:], Dm["negm_b0"][:, :], writes=[nm0.res])
                QTb = _sb(nc, ph, "QTb", [128, 4, 128], BF16)
                qiTbs = [_sb(nc, ph, f"qiTb{j}", [128, 2, 128], BF16) for j in range(2)]
                rt = [_sb(nc, ph, f"rt{j}", [128, 512], F32) for j in range(2)]
                PT = [_sb(nc, ph, f"PT{j}", [128, 1024], BF16) for j in range(2)]
                ytm = _sb(nc, ph, "ytm", [128, 512], BF16)
                lo = _sb(nc, ph, "lo", [128, 1], F32)
                hi = _sb(nc, ph, "hi", [128, 1], F32)
                mid = _sb(nc, ph, "mid", [128, 1], F32)
                cnt = _sb(nc, ph, "cnt", [128, 1], F32)
                uu = _sb(nc, ph, "uu", [128, 1], F32)
                pw2 = _sb(nc, ph, "pw2", [128, 32], F32)
                hv = _sb(nc, ph, "hv", [128, 32], F32)
                tw = _sb(nc, ph, "tw", [128, 32], F32)
                P.dma("sp", pw2[:], Dm["pow2"][:, :], writes=[pw2.res])
                rz = _sb(nc, ph, "rz", [128, 8], F32)
                psL = [_ps(nc, ph, f"psL{j}", [128, 1024], F32) for j in range(2)]
                psO = [_ps(nc, ph, f"psO{j}", [128, 512], F32) for j in range(2)]
                psI = [_ps(nc, ph, f"psI{j}", [128, 512], F32) for j in range(2)]
                NA = 0 if stop == 'kv' else (3 if stop in ('A1', 'A2', 'A3') else NBLK)

                def a_index(i):
                    nkb = 2 * i + 2
                    Sc = nkb * 128
                    osl = slice(i * 128, (i + 1) * 128)
                    qiTb = qiTbs[i % 2]
                    P.dma("sp", qiTb[:], qiT_s[:, :, osl], writes=[qiTb.res])
                    for kc in range((Sc + 511) // 512):
                        n = min(512, Sc - kc * 512)
                        ksl = slice(kc * 512, kc * 512 + n)
                        for h in range(4):
                            T = psI[h % 2]
                            pr = slice((h % 2) * 64, (h % 2) * 64 + 64)
                            P.op("pe", lambda E, T=T, n=n, pr=pr, h=h, ksl=ksl, qiTb=qiTb: E.matmul(
                                T[:, 0:n], qiTb[pr, h // 2, :], kiT[pr, ksl], start=True, stop=True),
                                reads=[qiTb.res, kir[kc]], writes=[T.res])
                            if h == 0:
                                P.op("act", lambda E, T=T, n=n, ksl=ksl, i=i: E.activation(
                                    out=score[:, ksl], in_=T[:, 0:n], func=AF.Relu, scale=Wabs[:, i, 0:1]),
                                    reads=[T.res, Wabs.res], writes=[score.res])
                                P.op("dve", lambda E, ksl=ksl, i=i: E.tensor_scalar(
                                    out=score[:, ksl], in0=score[:, ksl], scalar1=Wsgn[:, i, 0:1], scalar2=None,
                                    op0=ALU.mult),
                                    reads=[score.res, Wsgn.res], writes=[score.res])
                            else:
                                R = rt[h % 2]
                                P.op("act", lambda E, T=T, n=n, R=R, i=i, h=h: E.activation(
                                    out=R[:, 0:n], in_=T[:, 0:n], func=AF.Relu, scale=Wabs[:, i, h:h + 1]),
                                    reads=[T.res, Wabs.res], writes=[R.res])
                                P.op("dve", lambda E, R=R, n=n, ksl=ksl, i=i, h=h: E.scalar_tensor_tensor(
                                    out=score[:, ksl], in0=R[:, 0:n], scalar=Wsgn[:, i, h:h + 1], in1=score[:, ksl],
                                    op0=ALU.mult, op1=ALU.add),
                                    reads=[R.res, Wsgn.res, score.res], writes=[score.res])
                    P.op("dve", lambda E, Sc=Sc: E.tensor_reduce(out=hi[:], in_=score[:, 0:Sc], axis=AX.X, op=ALU.max),
                         reads=[score.res], writes=[hi.res])
                    P.op("dve", lambda E, Sc=Sc: E.tensor_reduce(out=lo[:], in_=score[:, 0:Sc], axis=AX.X, op=ALU.min),
                         reads=[score.res], writes=[lo.res])
                    P.op("dve", lambda E: E.tensor_tensor(out=score[:, 0:128], in0=score[:, 0:128], in1=nm0[:], op=ALU.add),
                         reads=[score.res, nm0.res], writes=[score.res])
                    P.op("dve", lambda E, Sc=Sc: E.tensor_tensor(
                        out=score[:, Sc - 128:Sc], in0=score[:, Sc - 128:Sc], in1=nmd[:], op=ALU.add),
                        reads=[score.res, nmd.res], writes=[score.res])
                    P.op("dve", lambda E: E.tensor_tensor(out=uu[:], in0=hi[:], in1=lo[:], op=ALU.subtract),
                         reads=[hi.res, lo.res], writes=[uu.res])
                    P.op("dve", lambda E: E.tensor_scalar(out=hv[:], in0=pw2[:], scalar1=uu[:, 0:1], scalar2=None, op0=ALU.mult),
                         reads=[pw2.res, uu.res], writes=[hv.res])
                    P.op("dve", lambda E: E.tensor_scalar(out=tw[:], in0=pw2[:], scalar1=uu[:, 0:1], scalar2=2.0,
                                                          op0=ALU.mult, op1=ALU.mult),
                         reads=[pw2.res, uu.res], writes=[tw.res])
                    P.op("dve", lambda E: E.tensor_tensor(out=mid[:], in0=lo[:], in1=hv[:, 0:1], op=ALU.add),
                         reads=[lo.res, hv.res], writes=[mid.res])
                    for it in range(NIT):
                        last = it == NIT - 1
                        j = it if last else it + 1
                        SA = hv if last else tw
                        P.op("dve", lambda E, Sc=Sc: E.tensor_scalar(
                            out=junkt[:, 0:Sc], in0=score[:, 0:Sc], scalar1=mid[:, 0:1], scalar2=None,
                            op0=ALU.is_ge, op1=ALU.add, accum_out=cnt[:]),
                            reads=[score.res, mid.res], writes=[junkt.res, cnt.res])
                        P.op("dve", lambda E, SA=SA, j=j: E.tensor_scalar(
                            out=uu[:], in0=cnt[:], scalar1=TOPK - 0.5, scalar2=SA[:, j:j + 1], op0=ALU.is_ge, op1=ALU.mult),
                            reads=[cnt.res, SA.res], writes=[uu.res])
                        P.op("dve", lambda E, j=j: E.scalar_tensor_tensor(
                            out=mid[:], in0=uu[:], scalar=hv[:, j:j + 1], in1=mid[:], op0=ALU.subtract, op1=ALU.add),
                            reads=[uu.res, hv.res, mid.res], writes=[mid.res])

                def a_mask(i):
                    Sc = (2 * i + 2) * 128
                    P.op("dve", lambda E, Sc=Sc: E.tensor_scalar(
                        out=Mneg[:, 0:Sc], in0=score[:, 0:Sc], scalar1=mid[:, 0:1], scalar2=-BIG,
                        op0=ALU.is_lt, op1=ALU.mult, saturate=False),
                        reads=[score.res, mid.res], writes=[Mneg.res])

                def a_attend(i):
                    nkb = 2 * i + 2
                    osl = slice(i * 128, (i + 1) * 128)
                    P.dma("sp", QTb[:], QT_s[:, :, osl], writes=[QTb.res])
                    for kb in range(nkb):
                        L = psL[kb % 2]
                        Pt = PT[kb % 2]
                        bsl = slice(kb * 128, (kb + 1) * 128)
                        g = kb // 4
                        for half in range(2):
                            P.op("pe", lambda E, L=L, half=half, bsl=bsl: E.matmul(
                                L[:, half * 512:(half + 1) * 512], Mneg[:, bsl], irep[:], start=True, stop=False),
                                reads=[Mneg.res, irep.res], writes=[L.res], inc=False)
                        for hh in range(4):
                            for half in range(2):
                                h = 2 * hh + half
                                pr = slice(half * 64, half * 64 + 64)
                                c0 = half * 512 + hh * 128
                                P.op("pe", lambda E, L=L, h=h, pr=pr, bsl=bsl, hh=hh, c0=c0: E.matmul(
                                    L[:, c0:c0 + 128], KT[pr, h // 2, bsl], QTb[pr, h // 2, :],
                                    start=False, stop=(hh == 3)),
                                    reads=[KTr[g], QTb.res], writes=[L.res], inc=(hh == 3 and half == 1))
                        P.op("act", lambda E, L=L, Pt=Pt: E.activation(out=Pt[:], in_=L[:], func=AF.Exp),
                             reads=[L.res], writes=[Pt.res])
                        for h in range(8):
                            O = psO[h // 4]
                            c0 = (h % 4) * 65
                            pc = (h % 2) * 512 + (h // 2) * 128
                            P.op("pe", lambda E, O=O, c0=c0, Pt=Pt, h=h, kb=kb, nkb=nkb, pc=pc: E.matmul(
                                O[:, c0:c0 + 65], Pt[:, pc:pc + 128], V[:, kb, h, :],
                                start=(kb == 0 and h % 4 == 0), stop=(kb == nkb - 1), skip_group_check=True),
                                reads=[Pt.res, Vr[g]], writes=[O.res], inc=(h == 7))
                    for b2 in range(2):
                        O = psO[b2]
                        P.op("dve", lambda E, O=O, b2=b2: E.reciprocal(
                            out=rz[:, b2 * 4:(b2 + 1) * 4],
                            in_=O[:, 0:260].rearrange("p (h d) -> p h d", d=65)[:, :, 64]),
                            reads=[O.res], writes=[rz.res])
                    for h in range(8):
                        O = psO[h // 4]
                        c0 = (h % 4) * 65
                        P.op("dve", lambda E, O=O, c0=c0, h=h: E.tensor_scalar(
                            out=ytm[:, h * 64:(h + 1) * 64], in0=O[:, c0:c0 + 64], scalar1=rz[:, h:h + 1],
                            scalar2=None, op0=ALU.mult),
                            reads=[O.res, rz.res], writes=[ytm.res])
                    P.dma("sp", yat_s[osl, :], ytm[:], reads=[ytm.res])

                if NA > 0:
                    a_index(0)
                    a_mask(0)
                for i in range(NA):
                    if i + 1 < NA:
                        a_index(i + 1)
                    a_attend(i)
                    if i + 1 < NA:
                        a_mask(i + 1)

        P.barrier()
        comb = _sb(nc, top, "comb", [128, NPAIR, 16], F32)
        if stop not in ("q", "kv", "A1", "A2", "A3", "A") and "ymT_in" not in debug:
            _phase_mlstm(P, nc, Dm, ymT_s)
        if stop not in ("q", "kv", "A1", "A2", "A3", "M", "A"):
          with ExitStack() as ph:
            wga = _sb(nc, ph, "wga", [128, 8, D], BF16)
            wgm = _sb(nc, ph, "wgm", [128, 8, D], BF16)
            wba = _sb(nc, ph, "wba", [128, 4, D], BF16)
            wbm = _sb(nc, ph, "wbm", [128, 8, D], BF16)
            wout = _sb(nc, ph, "wout", [128, 8, D], BF16)
            for t, n in ((wga, "wga"), (wgm, "wgm"), (wba, "wba"), (wbm, "wbm"), (wout, "wout")):
                P.dma("pool", t[:], _wview(Dm[n]), writes=[t.res])
            bga = _sb(nc, ph, "bga", [128, 8], F32)
            bgm = _sb(nc, ph, "bgm", [128, 8], F32)
            g1 = _sb(nc, ph, "g1", [128, D], F32)
            b1 = _sb(nc, ph, "b1", [128, D], F32)
            wr = _sb(nc, ph, "wr", [128, 8, 20], F32)
            brb = _sb(nc, ph, "brb", [128, 20], F32)
            identf = _sb(nc, ph, "identf", [128, 128], F32)
            identb = _sb(nc, ph, "identb", [128, 128], BF16)
            ones4 = _sb(nc, ph, "ones4", [128, 4], F32)
            for t, n in ((bga, "bga"), (bgm, "bgm"), (g1, "ln1g_bc"), (b1, "ln1b_bc"), (brb, "br_bc"), (identf, "ident")):
                P.dma("sp", t[:], Dm[n][:, :], writes=[t.res])
            P.dma("sp", wr[:], _wview(Dm["wr"]), writes=[wr.res])
            P.dma("pool", identb[:], Dm["ident"][:, :], writes=[identb.res])
            P.op("pool", lambda E: E.memset(ones4[:], 1.0), writes=[ones4.res])
            xo = _sb(nc, ph, "c_xo", [128, 8, 512], BF16)
            ymT = _sb(nc, ph, "c_ymT", [128, 8, 512], BF16)
            yaT = _sb(nc, ph, "c_yaT", [128, 4, 512], BF16)
            yab = [_sb(nc, ph, f"c_yab{j}", [128, 512], BF16) for j in range(2)]
            mT = _sb(nc, ph, "c_mT", [128, 8, 512], BF16)
            sa = [_sb(nc, ph, f"c_sa{j}", [128, 512], F32) for j in range(2)]
            sm = [_sb(nc, ph, f"c_sm{j}", [128, 512], F32) for j in range(2)]
            m1 = [_sb(nc, ph, f"c_m1{j}", [128, 512], F32) for j in range(2)]
            m2 = [_sb(nc, ph, f"c_m2{j}", [128, 512], F32) for j in range(2)]
            xb_2 = [_sb(nc, ph, f"c_xb{j}", [128, D], F32) for j in range(2)]
            z_2 = [_sb(nc, ph, f"c_z{j}", [128, D], F32) for j in range(2)]
            x1_2 = [_sb(nc, ph, f"c_x1{j}", [128, D], F32) for j in range(2)]
            junk = _sb(nc, ph, "c_junk", [128, D], F32)
            x1Tf_2 = [_sb(nc, ph, f"c_x1Tf{j}", [128, 8, 128], F32) for j in range(2)]
            x1Tb_2 = [_sb(nc, ph, f"c_x1Tb{j}", [128, 8, 128], BF16) for j in range(2)]
            st_2 = [_sb(nc, ph, f"c_st{j}", [128, 8], F32) for j in range(2)]
            rl_2 = [_sb(nc, ph, f"c_rl{j}", [128, 20], F32) for j in range(2)]
            rs_2 = [_sb(nc, ph, f"c_rs{j}", [128, 16], F32) for j in range(2)]
            gm_2 = [_sb(nc, ph, f"c_gm{j}", [128, 16], F32) for j in range(2)]
            elm_2 = [_sb(nc, ph, f"c_elm{j}", [128, 16], F32) for j in range(2)]
            oh1_2 = [_sb(nc, ph, f"c_oh1{j}", [128, 16], F32) for j in range(2)]
            oh2_2 = [_sb(nc, ph, f"c_oh2{j}", [128, 16], F32) for j in range(2)]
            goh_2 = [_sb(nc, ph, f"c_goh{j}", [128, 4], F32) for j in range(2)]
            gex_2 = [_sb(nc, ph, f"c_gex{j}", [128, 4], F32) for j in range(2)]
            psA = _ps(nc, ph, "c_psA", [128, 512], F32)
            psB = _ps(nc, ph, "c_psB", [128, 512], F32)
            psC = _ps(nc, ph, "c_psC", [128, 512], F32)
            psD = _ps(nc, ph, "c_psD", [128, 512], F32)
            psO = _ps(nc, ph, "c_psO", [128, 512], F32)
            psT = _ps(nc, ph, "c_psT", [128, 1024], BF16)
            psX = _ps(nc, ph, "c_psX", [128, 512], F32)
            psR = _ps(nc, ph, "c_psR", [128, 512], F32)
            xTo = Dm["xT_own"].rearrange("(c p) t -> p c t", p=128)
            BIGM = 1.0e4
            def c1_block(g, blk, xb, z, x1, x1Tf, x1Tb, st, rl, rs, gm, elm, oh1, oh2, goh, gex):
                i = g * 4 + blk
                tsl = slice(blk * 128, (blk + 1) * 128)
                rows = slice(i * 128, (i + 1) * 128)
                P.dma("sp", xb[:], Dm["x_own"][rows, :], writes=[xb.res])
                for half in range(2):
                    hs = slice(half * 512, (half + 1) * 512)
                    for c in range(8):
                        P.op("pe", lambda E, c=c, tsl=tsl, hs=hs: E.matmul(psO[:], mT[:, c, tsl], wout[:, c, hs], start=(c == 0), stop=(c == 7)),
                             reads=[mT.res, wout.res], writes=[psO.res], inc=(c == 7))
                    P.op("dve", lambda E, hs=hs: E.scalar_tensor_tensor(
                        out=z[:, hs], in0=xb[:, hs], scalar=ALPHA, in1=psO[:], op0=ALU.mult, op1=ALU.add),
                        reads=[xb.res, psO.res], writes=[z.res])
                _layer_norm(P, z, x1, junk, g1, b1, st, LN_EPS)
                P.dma("sp", x1_s[rows, :], x1[:], reads=[x1.res])
                if C1SUB == 'c':
                    return
                for rnd in range(2):
                    for c4 in range(4):
                        c = rnd * 4 + c4
                        P.op("pe", lambda E, c=c, c4=c4: E.transpose(
                            out=psX[:, c4 * 128:(c4 + 1) * 128], in_=x1[:, c * 128:(c + 1) * 128], identity=identf[:]),
                            reads=[x1.res, identf.res], writes=[psX.res])
                    P.op("act", lambda E, rnd=rnd: E.activation(
                        out=x1Tf[:, rnd * 4:(rnd + 1) * 4, :], in_=psX[:].rearrange("p (c t) -> p c t", c=4), func=AF.Identity),
                        reads=[psX.res], writes=[x1Tf.res])
                    P.op("dve", lambda E, rnd=rnd: E.tensor_copy(
                        out=x1Tb[:, rnd * 4:(rnd + 1) * 4, :], in_=psX[:].rearrange("p (c t) -> p c t", c=4)),
                        reads=[psX.res], writes=[x1Tb.res])
                P.dma("sp", x1T_s[:, :, rows], x1Tb[:], reads=[x1Tb.res])
                if C1SUB == 'd':
                    return
                for c in range(8):
                    P.op("pe", lambda E, c=c: E.matmul(psR[:, 0:20], x1Tf[:, c, :], wr[:, c, :], start=(c == 0), stop=(c == 7)),
                         reads=[x1Tf.res, wr.res], writes=[psR.res], inc=(c == 7))
                P.op("dve", lambda E: E.tensor_tensor(out=rl[:], in0=psR[:, 0:20], in1=brb[:], op=ALU.add),
                     reads=[psR.res, brb.res], writes=[rl.res])
                P.op("dve", lambda E: E.tensor_reduce(out=rs[:, 0:1], in_=rl[:, 0:4], axis=AX.X, op=ALU.max),
                     reads=[rl.res], writes=[rs.res])
                P.op("dve", lambda E: E.tensor_scalar(out=rs[:, 1:2], in0=rs[:, 0:1], scalar1=-1.0, scalar2=None, op0=ALU.mult),
                     reads=[rs.res], writes=[rs.res])
                P.op("act", lambda E: E.activation(out=gex[:], in_=rl[:, 0:4], func=AF.Exp, bias=rs[:, 1:2], accum_out=rs[:, 2:3]),
                     reads=[rl.res, rs.res], writes=[gex.res, rs.res])
                P.op("dve", lambda E: E.reciprocal(out=rs[:, 3:4], in_=rs[:, 2:3]), reads=[rs.res], writes=[rs.res])
                P.op("dve", lambda E: E.tensor_scalar(out=goh[:], in0=rl[:, 0:4], scalar1=rs[:, 0:1], scalar2=None, op0=ALU.is_ge),
                     reads=[rl.res, rs.res], writes=[goh.res])
                for j in range(4):
                    P.op("dve", lambda E, j=j: E.tensor_scalar(
                        out=gm[:, j * 4:(j + 1) * 4], in0=ones4[:], scalar1=goh[:, j:j + 1], scalar2=None, op0=ALU.mult),
                        reads=[ones4.res, goh.res], writes=[gm.res])
                P.op("dve", lambda E: E.tensor_scalar(out=gm[:], in0=gm[:], scalar1=-1.0, scalar2=BIGM, op0=ALU.add, op1=ALU.mult),
                     reads=[gm.res], writes=[gm.res])
                P.op("dve", lambda E: E.tensor_tensor(out=elm[:], in0=gm[:], in1=rl[:, 4:20], op=ALU.add),
                     reads=[gm.res, rl.res], writes=[elm.res])
                P.op("dve", lambda E: E.tensor_reduce(out=rs[:, 4:5], in_=elm[:], axis=AX.X, op=ALU.max),
                     reads=[elm.res], writes=[rs.res])
                P.op("dve", lambda E: E.tensor_scalar(out=oh1[:], in0=elm[:], scalar1=rs[:, 4:5], scalar2=None, op0=ALU.is_ge),
                     reads=[elm.res, rs.res], writes=[oh1.res])
                P.op("dve", lambda E: E.scalar_tensor_tensor(out=elm[:], in0=oh1[:], scalar=-BIGM, in1=elm[:], op0=ALU.mult, op1=ALU.add),
                     reads=[oh1.res, elm.res], writes=[elm.res])
                P.op("dve", lambda E: E.tensor_reduce(out=rs[:, 5:6], in_=elm[:], axis=AX.X, op=ALU.max),
                     reads=[elm.res], writes=[rs.res])
                P.op("dve", lambda E: E.tensor_scalar(out=oh2[:], in0=elm[:], scalar1=rs[:, 5:6], scalar2=None, op0=ALU.is_ge),
                     reads=[elm.res, rs.res], writes=[oh2.res])
                P.op("dve", lambda E: E.tensor_tensor(out=rs[:, 6:7], in0=rs[:, 5:6], in1=rs[:, 4:5], op=ALU.subtract),
                     reads=[rs.res], writes=[rs.res])
                P.op("act", lambda E: E.activation(out=rs[:, 7:8], in_=rs[:, 6:7], func=AF.Exp),
                     reads=[rs.res], writes=[rs.res])
                P.op("dve", lambda E: E.tensor_scalar(out=rs[:, 8:9], in0=rs[:, 7:8], scalar1=1.0, scalar2=None, op0=ALU.add),
                     reads=[rs.res], writes=[rs.res])
                P.op("dve", lambda E: E.reciprocal(out=rs[:, 8:9], in_=rs[:, 8:9]), reads=[rs.res], writes=[rs.res])
                P.op("dve", lambda E: E.tensor_tensor(out=rs[:, 9:10], in0=rs[:, 7:8], in1=rs[:, 8:9], op=ALU.mult),
                     reads=[rs.res], writes=[rs.res])
                P.op("dve", lambda E: E.tensor_scalar(out=oh1[:], in0=oh1[:], scalar1=rs[:, 8:9], scalar2=None, op0=ALU.mult),
                     reads=[oh1.res, rs.res], writes=[oh1.res])
                P.op("dve", lambda E: E.scalar_tensor_tensor(out=oh2[:], in0=oh2[:], scalar=rs[:, 9:10], in1=oh1[:], op0=ALU.mult, op1=ALU.add),
                     reads=[oh2.res, rs.res, oh1.res], writes=[oh2.res])
                P.op("dve", lambda E, i=i: E.tensor_scalar(out=comb[:, i, :], in0=oh2[:], scalar1=rs[:, 3:4], scalar2=None, op0=ALU.mult),
                     reads=[oh2.res, rs.res], writes=[comb.res])
            for g in range(NBLK // 4):
                sl = slice(g * 512, (g + 1) * 512)
                P.dma("pool", xo[:], xTo[:, :, sl], writes=[xo.res])
                P.dma("sp", ymT[:], ymT_s[:, :, sl], writes=[ymT.res])
                for blk in range(4):
                    Yb = yab[blk % 2]
                    P.dma("sp", Yb[:], yat_s[g * 512 + blk * 128:g * 512 + (blk + 1) * 128, :], writes=[Yb.res])
                    for c in range(4):
                        P.op("pe", lambda E, Yb=Yb, c=c: E.transpose(
                            out=psT[:, c * 128:(c + 1) * 128], in_=Yb[:, c * 128:(c + 1) * 128], identity=identb[:]),
                            reads=[Yb.res, identb.res], writes=[psT.res])
                    P.op("act", lambda E, blk=blk: E.activation(
                        out=yaT[:, :, blk * 128:(blk + 1) * 128], in_=psT[:, 0:512].rearrange("p (c t) -> p c t", c=4),
                        func=AF.Identity),
                        reads=[psT.res], writes=[yaT.res])
                if C1SUB == 'a':
                    continue
                for mc in range(8):
                    msl = slice(mc * 128, (mc + 1) * 128)
                    SA, SM, M1, M2 = sa[mc % 2], sm[mc % 2], m1[mc % 2], m2[mc % 2]
                    for c in range(4):
                        P.op("pe", lambda E, c=c, msl=msl: E.matmul(psA[:], wba[:, c, msl], yaT[:, c, :], start=(c == 0), stop=(c == 3)),
                             reads=[wba.res, yaT.res], writes=[psA.res], inc=(c == 3))
                    for c in range(8):
                        P.op("pe", lambda E, c=c, msl=msl: E.matmul(psB[:], wbm[:, c, msl], ymT[:, c, :], start=(c == 0), stop=(c == 7)),
                             reads=[wbm.res, ymT.res], writes=[psB.res], inc=(c == 7))
                    for c in range(8):
                        P.op("pe", lambda E, c=c, msl=msl: E.matmul(psC[:], wga[:, c, msl], xo[:, c, :], start=(c == 0), stop=(c == 7)),
                             reads=[wga.res, xo.res], writes=[psC.res], inc=(c == 7))
                    for c in range(8):
                        P.op("pe", lambda E, c=c, msl=msl: E.matmul(psD[:], wgm[:, c, msl], xo[:, c, :], start=(c == 0), stop=(c == 7)),
                             reads=[wgm.res, xo.res], writes=[psD.res], inc=(c == 7))
                    P.op("act", lambda E, SA=SA, mc=mc: E.activation(out=SA[:], in_=psC[:], func=AF.Sigmoid, bias=bga[:, mc:mc + 1]),
                         reads=[psC.res, bga.res], writes=[SA.res])
                    P.op("act", lambda E, SM=SM, mc=mc: E.activation(out=SM[:], in_=psD[:], func=AF.Sigmoid, bias=bgm[:, mc:mc + 1]),
                         reads=[psD.res, bgm.res], writes=[SM.res])
                    P.op("dve", lambda E, M1=M1, SA=SA: E.tensor_tensor(out=M1[:], in0=psA[:], in1=SA[:], op=ALU.mult),
                         reads=[psA.res, SA.res], writes=[M1.res])
                    P.op("dve", lambda E, M2=M2, SM=SM: E.tensor_tensor(out=M2[:], in0=psB[:], in1=SM[:], op=ALU.mult),
                         reads=[psB.res, SM.res], writes=[M2.res])
                    P.op("pool", lambda E, M1=M1, M2=M2, mc=mc: E.tensor_tensor(out=mT[:, mc, :], in0=M1[:], in1=M2[:], op=ALU.add),
                         reads=[M1.res, M2.res], writes=[mT.res])
                if C1SUB == 'b':
                    continue
                for blk in range(4):
                    j2 = (g * 4 + blk) % 2
                    c1_block(g, blk, xb_2[j2], z_2[j2], x1_2[j2], x1Tf_2[j2], x1Tb_2[j2], st_2[j2], rl_2[j2], rs_2[j2], gm_2[j2], elm_2[j2], oh1_2[j2], oh2_2[j2], goh_2[j2], gex_2[j2])
          P.barrier()

        if stop not in ("q", "kv", "A1", "A2", "A3", "C1", "M", "A"):
          with ExitStack() as ph:
            NBH = min(NBLK, 16)
            NH = NBH * 128
            acc = _sb(nc, ph, "e_acc", [128, NBH, D], F32)
            x1T = _sb(nc, ph, "e_x1T", [128, 8, NH], BF16)
            wg = [_sb(nc, ph, f"e_wg{j}", [128, 8, 512], BF16) for j in range(2)]
            wu = [_sb(nc, ph, f"e_wu{j}", [128, 8, 512], BF16) for j in range(2)]
            wd = [_sb(nc, ph, f"e_wd{j}", [128, 4, D], BF16) for j in range(2)]
            sg = [_sb(nc, ph, f"e_sg{j}", [128, 512], F32) for j in range(2)]
            hdn = [_sb(nc, ph, f"e_hdn{j}", [128, 4, 512], BF16) for j in range(2)]
            g2 = _sb(nc, ph, "e_g2", [128, D], F32)
            b2 = _sb(nc, ph, "e_b2", [128, D], F32)
            junk = _sb(nc, ph, "e_junk", [128, D], F32)
            ot = [_sb(nc, ph, f"e_ot{j}", [128, D], F32) for j in range(2)]
            zt = _sb(nc, ph, "e_zt", [128, D], F32)
            st = _sb(nc, ph, "e_st", [128, 8], F32)
            P.dma("sp", g2[:], Dm["ln2g_bc"][:, :], writes=[g2.res])
            P.dma("sp", b2[:], Dm["ln2b_bc"][:, :], writes=[b2.res])
            psG = [_ps(nc, ph, f"e_psG{j}", [128, 512], F32) for j in range(2)]
            psU = [_ps(nc, ph, f"e_psU{j}", [128, 512], F32) for j in range(2)]
            psDn = [_ps(nc, ph, f"e_psDn{j}", [128, 512], F32) for j in range(4)]
            accr = [Res(f"acc{b}") for b in range(NBH)]
            for hf in range(2 if NBLK == 32 else 1):
                tok = slice(hf * NH, (hf + 1) * NH)
                P.dma("sp", x1T[:], x1T_s[:, :, tok], writes=[x1T.res])
                for b in range(NBH):
                    rows = slice(hf * NH + b * 128, hf * NH + (b + 1) * 128)
                    P.dma("sp", acc[:, b, :], x1_s[rows, :], writes=[accr[b]])
                    P.op("act", lambda E, b=b: E.activation(out=acc[:, b, :], in_=acc[:, b, :], func=AF.Identity, scale=ALPHA),
                         reads=[accr[b]], writes=[accr[b]])
                for e in range(16):
                    WG, WU, WD = wg[e % 2], wu[e % 2], wd[e % 2]
                    P.dma("pool", WG[:], _wview(Dm["weg"][e]), writes=[WG.res])
                    P.dma("pool", WU[:], _wview(Dm["weu"][e]), writes=[WU.res])
                    P.dma("pool", WD[:], _wview(Dm["wed"][e]), writes=[WD.res])
                    for tg in range(NH // 512):
                        tsl = slice(tg * 512, (tg + 1) * 512)
                        H = hdn[tg % 2]
                        for fc in range(4):
                            fsl = slice(fc * 128, (fc + 1) * 128)
                            G, U, SG = psG[fc % 2], psU[fc % 2], sg[fc % 2]
                            for k in range(8):
                                P.op("pe", lambda E, G=G, WG=WG, k=k, fsl=fsl, tsl=tsl: E.matmul(
                                    G[:], WG[:, k, fsl], x1T[:, k, tsl], start=(k == 0), stop=(k == 7)),
                                    reads=[WG.res, x1T.res], writes=[G.res], inc=(k == 7))
                            for k in range(8):
                                P.op("pe", lambda E, U=U, WU=WU, k=k, fsl=fsl, tsl=tsl: E.matmul(
                                    U[:], WU[:, k, fsl], x1T[:, k, tsl], start=(k == 0), stop=(k == 7)),
                                    reads=[WU.res, x1T.res], writes=[U.res], inc=(k == 7))
                            P.op("act", lambda E, SG=SG, G=G: E.activation(out=SG[:], in_=G[:], func=AF.Silu),
                                 reads=[G.res], writes=[SG.res])
                            P.op("dve", lambda E, H=H, fc=fc, SG=SG, U=U: E.tensor_tensor(out=H[:, fc, :], in0=U[:], in1=SG[:], op=ALU.mult),
                                 reads=[U.res, SG.res], writes=[H.res])
                        for blk in range(4):
                            b = tg * 4 + blk
                            ib = hf * NBH + b
                            bsl = slice(blk * 128, (blk + 1) * 128)
                            for half in range(2):
                                hs = slice(half * 512, (half + 1) * 512)
                                Dp = psDn[(blk * 2 + half) % 4]
                                for fc in range(4):
                                    P.op("pe", lambda E, Dp=Dp, H=H, fc=fc, bsl=bsl, WD=WD, hs=hs: E.matmul(
                                        Dp[:], H[:, fc, bsl], WD[:, fc, hs], start=(fc == 0), stop=(fc == 3)),
                                        reads=[H.res, WD.res], writes=[Dp.res], inc=(fc == 3))
                                P.op("dve", lambda E, Dp=Dp, b=b, hs=hs, ib=ib, e=e: E.scalar_tensor_tensor(
                                    out=acc[:, b, hs], in0=Dp[:], scalar=comb[:, ib, e:e + 1], in1=acc[:, b, hs],
                                    op0=ALU.mult, op1=ALU.add),
                                    reads=[Dp.res, comb.res, accr[b]], writes=[accr[b]])
                for b in range(NBH):
                    rows = slice(hf * NH + b * 128, hf * NH + (b + 1) * 128)
                    O = ot[b % 2]
                    zb = Tl(acc.t[:, b, :], f"accv{b}")
                    zb.res = accr[b]
                    _layer_norm(P, zb, O, junk, g2, b2, st, LN_EPS)
                    P.dma("sp", out[rows, :], O[:], reads=[O.res])
          P.barrier()
        P.finish()
        nc._prog = P
        with nc.Block() as block:
            P.emit(block)
    return nc


IN_SPLITS = (512, 512, 512, 256, 64, 4, 512, 512, 1024, 8, 8, 1024, 1024, 1024)
OFF = np.concatenate([[0], np.cumsum(IN_SPLITS)]).astype(int)


def _swap_perm(nheads):
    idx = []
    for h in range(nheads):
        idx += list(range(h * 64 + 32, h * 64 + 64)) + list(range(h * 64, h * 64 + 32))
    return np.array(idx)


def _fm_bias(b):
    return np.ascontiguousarray(b.reshape(-1, 128).T)


def _rope_tabs(pos, scale=1.0):
    inv = (np.float32(10000.0) ** (-np.arange(0, 64, 2, dtype=np.float32) / np.float32(64))).astype(np.float32)
    ang = pos.astype(np.float32)[None, :] * inv[:, None]
    c = np.cos(ang).astype(np.float32)
    s = np.sin(ang).astype(np.float32)
    cos = np.concatenate([c, c, c, c], 0)
    sin = np.concatenate([-s, s, -s, s], 0)
    return np.ascontiguousarray(cos * np.float32(scale)), np.ascontiguousarray(sin * np.float32(scale))


def _shared_inputs(w_in, b_in, inputs):
    sh = {}
    f = lambda k: np.asarray(inputs[k], np.float32)
    bc = lambda v, n: np.broadcast_to(v[None, :], (128, n))
    sh["wba"], sh["wbm"], sh["wout"] = f("w_branch_attn"), f("w_branch_mlstm"), f("w_out")
    sh["ln1g_bc"], sh["ln1b_bc"] = bc(f("ln1_gain"), D), bc(f("ln1_bias"), D)
    sh["ln2g_bc"], sh["ln2b_bc"] = bc(f("ln2_gain"), D), bc(f("ln2_bias"), D)
    sh["wr"] = np.concatenate([f("w_router_group"), f("w_router_expert")], 1)
    sh["br_bc"] = bc(np.concatenate([f("b_router_group"), f("b_router_expert")]), 20)
    sh["weg"], sh["weu"], sh["wed"] = f("w_exp_gate"), f("w_exp_up"), f("w_exp_down")
    sw8, sw4, sw1 = _swap_perm(8), _swap_perm(4), _swap_perm(1)
    aq, ak, av = w_in[:, OFF[0]:OFF[1]], w_in[:, OFF[1]:OFF[2]], w_in[:, OFF[2]:OFF[3]]
    iq, ik, iw = w_in[:, OFF[3]:OFF[4]], w_in[:, OFF[4]:OFF[5]], w_in[:, OFF[5]:OFF[6]]
    baq, bak, bav = b_in[OFF[0]:OFF[1]], b_in[OFF[1]:OFF[2]], b_in[OFF[2]:OFF[3]]
    biq, bik, biw = b_in[OFF[3]:OFF[4]], b_in[OFF[4]:OFF[5]], b_in[OFF[5]:OFF[6]]
    sh["wq"], sh["wq_s"] = aq, aq[:, sw8]
    sh["wk"], sh["wk_s"] = ak, ak[:, sw8]
    sh["wv"] = av
    sh["wiq"], sh["wiq_s"] = iq, iq[:, sw4]
    sh["wik2"] = np.concatenate([ik, ik], 1)
    sh["wik2_s"] = np.concatenate([ik[:, sw1], ik[:, sw1]], 1)
    sh["wiw"] = iw
    sh["bq"], sh["bq_s"] = _fm_bias(baq), _fm_bias(baq[sw8])
    sh["bk"], sh["bk_s"] = _fm_bias(bak), _fm_bias(bak[sw8])
    sh["biq"], sh["biq_s"] = _fm_bias(biq), _fm_bias(biq[sw4])
    sh["bik2"] = _fm_bias(np.concatenate([bik, bik]))
    sh["bik2_s"] = _fm_bias(np.concatenate([bik[sw1], bik[sw1]]))
    sh["bv_bc"] = np.broadcast_to(bav[None, :], (128, 512))
    sh["biw_bc"] = np.broadcast_to(biw[None, :], (128, 4))
    sh["wga"], sh["wgm"] = w_in[:, OFF[12]:OFF[13]], w_in[:, OFF[13]:OFF[14]]
    sh["bga"], sh["bgm"] = _fm_bias(b_in[OFF[12]:OFF[13]]), _fm_bias(b_in[OFF[13]:OFF[14]])
    sh["wmq"], sh["wmk"] = w_in[:, OFF[6]:OFF[7]], w_in[:, OFF[7]:OFF[8]]
    sh["bmq"], sh["bmk"] = _fm_bias(b_in[OFF[6]:OFF[7]]), _fm_bias(b_in[OFF[7]:OFF[8]])
    cm = f("conv_m")
    sh["cwq"] = cm[:, 0:512].T.reshape(4, 128, 4).transpose(1, 0, 2).reshape(128, 16)
    sh["cwk"] = cm[:, 512:1024].T.reshape(4, 128, 4).transpose(1, 0, 2).reshape(128, 16)
    sh["wmv"], sh["bmv_bc"] = w_in[:, OFF[8]:OFF[9]], bc(b_in[OFF[8]:OFF[9]], D)
    sh["wmif"], sh["bif_bc"] = w_in[:, OFF[9]:OFF[11]], bc(b_in[OFF[9]:OFF[11]], 16)
    sh["wmo"], sh["bmo_bc"] = w_in[:, OFF[11]:OFF[12]], bc(b_in[OFF[11]:OFF[12]], D)
    sh["gn_bc"] = bc(f("gn_m_gain"), D)
    qq = np.arange(128)
    sh["tri"] = (qq[:, None] <= qq[None, :]).astype(np.float32)
    sh["ones"] = np.ones((128, 128), np.float32)
    sh["pow2"] = np.broadcast_to((2.0 ** -(np.arange(32) + 1.0)).astype(np.float32)[None, :], (128, 32))
    cmask = np.where(qq[None, :] <= qq[:, None], 0.0, NEG).astype(np.float32)
    sh["cmask_rep"] = np.tile(cmask, (1, 8))
    sh["cmaskT_rep"] = np.tile(cmask.T, (1, 8))
    eye = np.eye(128, dtype=np.float32)
    sh["ident"] = eye
    sh["irep"] = np.concatenate([eye] * 4, 1)
    q = np.arange(128)
    sh["negm_diag"] = np.where(q[None, :] <= q[:, None], 0.0, NEG).astype(np.float32)
    return {k: np.ascontiguousarray(v, dtype=np.float32) for k, v in sh.items()}


def _core_inputs(x, c):
    b, p = c // 2, c % 2
    xb = x[b]
    if p == 1:
        xprog = xb
        pos = np.arange(S)
    else:
        xprog = np.concatenate([np.zeros((128, D), np.float32), xb[:S - 128]], 0)
        pos = np.concatenate([np.zeros(128), np.arange(S - 128)])
    own = xprog.reshape(64, 128, D)[1::2].reshape(NOWN, D)
    pos_own = pos.reshape(64, 128)[1::2].reshape(NOWN)
    m = {"xT": np.ascontiguousarray(xprog.T), "xT_own": np.ascontiguousarray(own.T),
         "x_own": np.ascontiguousarray(own)}
    m["cosk"], m["sink"] = _rope_tabs(pos)
    m["cosq"], m["sinq"] = _rope_tabs(pos_own, 0.125)
    m["cosi"], m["sini"] = _rope_tabs(pos_own)
    m["negm_b0"] = np.full((128, 128), NEG if p == 0 else 0.0, np.float32)
    m["vflag"] = np.full((128, 1), 0.0 if p == 0 else 1.0, np.float32)
    m["ineg"] = np.full((128, 1), NEG if p == 0 else 0.0, np.float32)
    return m


def make_in_maps(inputs):
    x = np.asarray(inputs["x"], np.float32)
    sh = _shared_inputs(np.asarray(inputs["w_in"], np.float32), np.asarray(inputs["b_in"], np.float32), inputs)
    maps = []
    for c in range(8):
        m = dict(sh)
        m.update(_core_inputs(x, c))
        maps.append({k: m[k] for k in IN_SPECS})
    return maps


def kernel(**inputs):
    nc = build_nc()
    in_maps = make_in_maps(inputs)
    res = run_bass_kernel_spmd(nc, in_maps, core_ids=list(range(8)))
    B = 4
    out = np.zeros((B, S, D), np.float32)
    for c in range(8):
        b, p = c // 2, c % 2
        o = np.asarray(res.results[c]["out"]).reshape(NPAIR, 128, D)
        out[b].reshape(64, 128, D)[p::2] = o
    return out
```

```python
from contextlib import ExitStack

import numpy as np
import concourse.bass as bass
import concourse.mybir as mybir
from concourse.bass_utils import run_bass_kernel_spmd

F32 = mybir.dt.float32
BF16 = mybir.dt.bfloat16
U32 = mybir.dt.uint32
U8 = mybir.dt.uint8
F8 = mybir.dt.float8e5
AF = mybir.ActivationFunctionType
ALU = mybir.AluOpType
AX = mybir.AxisListType

D = 1024
S = 8192
NPAIR = 32
NOWN = 4096
BIG = 30000.0
NEG = -1.0e30
import os
NIT = int(os.environ.get('K_NIT', '13'))
TOPK = 256
NBLK = int(os.environ.get('K_NBLK', '32'))

EPOCH = 16000
NEPOCH = 8
NSLOT = 8
ENGS = ("pe", "act", "dve", "pool", "sp")
DMAQ = ("sp", "pool")


class Res:
    __slots__ = ("name", "w", "r", "psum")

    def __init__(self, name="", psum=False):
        self.name = name
        self.w = None
        self.r = {}
        self.psum = psum


class Tl:
    def __init__(self, t, name, psum=False):
        self.t = t
        self.res = Res(name, psum)

    def __getitem__(self, k):
        return self.t[k]


class Prog:
    def __init__(self, nc, es):
        self.nc = nc
        self.q = {e: [] for e in ENGS}
        self.tr = {e: [] for e in ENGS}
        self.cnt = {e: 0 for e in ENGS}
        self.dcnt = {q: 0 for q in DMAQ}
        self.known = {e: {} for e in ENGS}
        self.esem = {e: [es.enter_context(nc.semaphore(f"s_{e}{k}")) for k in range(NEPOCH)] for e in ENGS}
        self.dsem = {q: [es.enter_context(nc.semaphore(f"d_{q}{k}")) for k in range(NSLOT)] for q in DMAQ}

    def _wait(self, eng, sp):
        if sp[0] == "E":
            _, e2, seq = sp
            if e2 == eng and eng == "pe":
                return
            key = ("E", e2)
            if self.known[eng].get(key, 0) >= seq:
                return
            self.known[eng][key] = seq
            sem = self.esem[e2][(seq - 1) // EPOCH]
            val = (seq - 1) % EPOCH + 1
        else:
            _, q, idx = sp
            slot = idx % NSLOT
            need = idx // NSLOT + 1
            key = ("D", q, slot)
            if self.known[eng].get(key, 0) >= need:
                return
            self.known[eng][key] = need
            sem = self.dsem[q][slot]
            val = 16 * need
        self.q[eng].append(lambda E, sem=sem, val=val: E.wait_ge(sem, val))
        self.tr[eng].append(('wait', id(sem), val))

    def _deps(self, eng, reads, writes):
        for r in reads:
            if r.w is not None:
                self._wait(eng, r.w)
            if r.psum:
                for key, v in r.r.items():
                    if key[0] == "E" and key[1] != eng:
                        self._wait(eng, ("E", key[1], v))
        for w in writes:
            if w.w is not None:
                self._wait(eng, w.w)
            for key, v in w.r.items():
                self._wait(eng, ("E", key[1], v) if key[0] == "E" else ("D", key[1], v))

    def _mark(self, me, reads, writes):
        key = ("E", me[1]) if me[0] == "E" else ("D", me[1], me[2] % NSLOT)
        for r in reads:
            if r.r.get(key, -1) < me[2]:
                r.r[key] = me[2]
        for w in writes:
            w.w = me
            w.r = {}

    def op(self, eng, fn, reads=(), writes=(), inc=True):
        self._deps(eng, reads, writes)
        if not inc:
            assert eng == "pe"
            self.q[eng].append(lambda E, fn=fn: fn(E))
            self._mark(("E", eng, self.cnt[eng] + 1), reads, writes)
            return
        self.cnt[eng] += 1
        seq = self.cnt[eng]
        assert seq <= EPOCH * NEPOCH, f"too many instructions on {eng}"
        sem = self.esem[eng][(seq - 1) // EPOCH]
        self.q[eng].append(lambda E, fn=fn, sem=sem: fn(E).then_inc(sem, 1))
        self.tr[eng].append(('inc', id(sem), 1))
        self._mark(("E", eng, seq), reads, writes)

    def dma(self, q, out, in_, reads=(), writes=()):
        self._deps(q, reads, writes)
        idx = self.dcnt[q]
        self.dcnt[q] += 1
        if idx >= NSLOT:
            self._wait(q, ("D", q, idx - NSLOT))
        sem = self.dsem[q][idx % NSLOT]
        self.q[q].append(lambda E, out=out, in_=in_, sem=sem: E.dma_start(out=out, in_=in_).then_inc(sem, 16))
        self.tr[q].append(('inc', id(sem), 16))
        self._mark(("D", q, idx), reads, writes)

    def barrier(self):
        for e in ENGS:
            for e2 in ENGS:
                if self.cnt[e2] > 0:
                    self._wait(e, ("E", e2, self.cnt[e2]))
            for q in DMAQ:
                n = self.dcnt[q]
                for idx in range(max(0, n - NSLOT), n):
                    self._wait(e, ("D", q, idx))

    def finish(self):
        for q in DMAQ:
            n = self.dcnt[q]
            for idx in range(max(0, n - NSLOT), n):
                self._wait("sp", ("D", q, idx))
        for e in ENGS:
            if e != "sp" and self.cnt[e] > 0:
                self._wait("sp", ("E", e, self.cnt[e]))

    def emit(self, block):
        def mk(name):
            def f(E):
                for c in self.q[name]:
                    c(E)
            return f
        block.tensor(mk("pe"))
        block.scalar(mk("act"))
        block.vector(mk("dve"))
        block.gpsimd(mk("pool"))
        block.sync(mk("sp"))


def _sb(nc, ph, name, shape, dtype):
    return Tl(ph.enter_context(nc.sbuf_tensor("sb_" + name, list(shape), dtype)), name)


def _ps(nc, ph, name, shape, dtype):
    return Tl(ph.enter_context(nc.psum_tensor("pp_" + name, list(shape), dtype)), name, psum=True)


def _layer_norm(P, z, outt, junk, g_bc, b_bc, st, eps):
    n = float(D)
    P.op("act", lambda E: E.activation(out=junk[:], in_=z[:], func=AF.Identity, accum_out=st[:, 0:1]),
         reads=[z.res], writes=[junk.res, st.res])
    P.op("act", lambda E: E.activation(out=junk[:], in_=z[:], func=AF.Square, accum_out=st[:, 1:2]),
         reads=[z.res], writes=[junk.res, st.res])
    P.op("dve", lambda E: E.tensor_scalar(out=st[:, 2:3], in0=st[:, 0:1], scalar1=-1.0 / n, scalar2=None, op0=ALU.mult),
         reads=[st.res], writes=[st.res])
    P.op("dve", lambda E: E.tensor_tensor(out=st[:, 3:4], in0=st[:, 2:3], in1=st[:, 2:3], op=ALU.mult),
         reads=[st.res], writes=[st.res])
    P.op("dve", lambda E: E.scalar_tensor_tensor(out=st[:, 4:5], in0=st[:, 1:2], scalar=1.0 / n, in1=st[:, 3:4],
                                                  op0=ALU.mult, op1=ALU.subtract),
         reads=[st.res], writes=[st.res])
    P.op("dve", lambda E: E.tensor_scalar(out=st[:, 4:5], in0=st[:, 4:5], scalar1=eps, scalar2=None, op0=ALU.add),
         reads=[st.res], writes=[st.res])
    P.op("act", lambda E: E.activation(out=st[:, 5:6], in_=st[:, 4:5], func=AF.Sqrt),
         reads=[st.res], writes=[st.res])
    P.op("dve", lambda E: E.reciprocal(out=st[:, 6:7], in_=st[:, 5:6]), reads=[st.res], writes=[st.res])
    P.op("dve", lambda E: E.tensor_scalar(out=outt[:], in0=z[:], scalar1=st[:, 2:3], scalar2=st[:, 6:7],
                                          op0=ALU.add, op1=ALU.mult),
         reads=[z.res, st.res], writes=[outt.res])
    P.op("dve", lambda E: E.tensor_tensor(out=outt[:], in0=outt[:], in1=g_bc[:], op=ALU.mult),
         reads=[outt.res, g_bc.res], writes=[outt.res])
    P.op("dve", lambda E: E.tensor_tensor(out=outt[:], in0=outt[:], in1=b_bc[:], op=ALU.add),
         reads=[outt.res, b_bc.res], writes=[outt.res])


def _wview(ap, p=128):
    return ap.rearrange("(c p) n -> p c n", p=p)


def _phase_mlstm(P, nc, Dm, ymT_s):
    with ExitStack() as ph:
        sb = lambda n, shp, dt: _sb(nc, ph, "m_" + n, shp, dt)
        wmq, wmk = sb("wmq", [128, 8, 512], BF16), sb("wmk", [128, 8, 512], BF16)
        wmv, wmo = sb("wmv", [128, 8, D], BF16), sb("wmo", [128, 8, D], BF16)
        wmif = sb("wmif", [128, 8, 16], BF16)
        for t, n in ((wmq, "wmq"), (wmk, "wmk"), (wmv, "wmv"), (wmo, "wmo"), (wmif, "wmif")):
            P.dma("pool", t[:], _wview(Dm[n]), writes=[t.res])
        bmq, bmk = sb("bmq", [128, 4], F32), sb("bmk", [128, 4], F32)
        cwq, cwk = sb("cwq", [128, 16], F32), sb("cwk", [128, 16], F32)
        bmv, bmo, gnb = sb("bmv", [128, D], F32), sb("bmo", [128, D], F32), sb("gnb", [128, D], F32)
        bif = sb("bif", [128, 16], F32)
        tri, ones, identf = sb("tri", [128, 128], F32), sb("ones", [128, 128], F32), sb("identf", [128, 128], F32)
        identb = sb("identb", [128, 128], BF16)
        cmk, cmkT = sb("cmk", [128, 1024], F32), sb("cmkT", [128, 1024], F32)
        vflag, ineg = sb("vflag", [128, 1], F32), sb("ineg", [128, 1], F32)
        for t, n in ((bmq, "bmq"), (bmk, "bmk"), (cwq, "cwq"), (cwk, "cwk"), (bmv, "bmv_bc"), (bmo, "bmo_bc"),
                     (gnb, "gn_bc"), (bif, "bif_bc"), (tri, "tri"), (ones, "ones"), (identf, "ident"),
                     (cmk, "cmask_rep"), (cmkT, "cmaskT_rep"), (vflag, "vflag"), (ineg, "ineg")):
            P.dma("sp", t[:], Dm[n][:, :], writes=[t.res])
        P.dma("pool", identb[:], Dm["ident"][:, :], writes=[identb.res])
        xg = sb("xg", [128, 8, 512], BF16)
        cb = [sb(f"cb{j}", [128, 515], F32) for j in range(8)]
        ctmp = [sb(f"ctmp{j}", [128, 512], F32) for j in range(2)]
        mqT, mkT = sb("mqT", [128, 4, 512], BF16), sb("mkT", [128, 4, 512], BF16)
        vaug = [sb(f"vaug{j}", [128, 8, 129], BF16) for j in range(4)]
        gt = sb("gt", [128, 16], F32)
        ef = sb("ef", [128, 8], F32)
        lf = sb("lf", [128, 8], F32)
        bt = sb("bt", [128, 16], F32)
        av = sb("av", [128, 8], F32)
        col = sb("col", [128, 12, 8], F32)
        diag = sb("diag", [128, 8, 128], F32)
        Am = sb("Am", [128, 8, 128], F32)
        DT = sb("DT", [128, 8, 128], F32)
        PT = sb("PT", [128, 1024], BF16)
        og = sb("og", [128, D], F32)
        n1s = sb("n1s", [128, 129], F32)
        nums = sb("nums", [128, 129], F32)
        sc1 = sb("sc1", [128, 4], F32)
        hbuf = sb("hbuf", [128, 8, 128], F32)
        sqt = sb("sqt", [128, 8, 128], F32)
        ybf = sb("ybf", [128, D], BF16)
        yT = sb("yT", [128, 8, 128], BF16)
        wk = sb("wk", [128, 8, 64], BF16)
        Cst = sb("Cst", [128, 4, 129], F32)
        Cbf = sb("Cbf", [128, 4, 129], BF16)
        mprev = sb("mprev", [128, 8], F32)
        pj = [_ps(nc, ph, f"m_pj{j}", [128, 512], F32) for j in range(2)]
        pbc = _ps(nc, ph, "m_pbc", [128, 1024], F32)
        psS = _ps(nc, ph, "m_psS", [128, 1024], F32)
        pT = _ps(nc, ph, "m_pT", [128, 1024], BF16)
        CMAX, INTER, MT, RR, WINT, EMT, AMX, MNB, MNEW, CD, ES, TMP = range(12)
        diag_r = [Res(f"diag{h}") for h in range(8)]
        DT_r = [Res(f"DT{h}") for h in range(8)]
        hbuf_r = [Res(f"hbuf{h}") for h in range(8)]
        wk_r = [Res(f"wk{h}") for h in range(8)]
        Cst_r = [Res(f"Cst{h}") for h in range(8)]
        for j in range(8):
            P.op("pool", lambda E, j=j: E.memset(cb[j][:, 0:3], 0.0), writes=[cb[j].res])
        for j in range(4):
            P.op("pool", lambda E, j=j: E.memset(vaug[j][:, :, 128:129], 1.0), writes=[vaug[j].res])
        P.op("pool", lambda E: E.memset(Cst[:], 0.0), writes=Cst_r)
        P.op("pool", lambda E: E.memset(Cbf[:], 0.0), writes=[Cbf.res])
        P.op("pool", lambda E: E.memset(mprev[:], 0.0), writes=[mprev.res])
        xTa = Dm["xT"].rearrange("(c p) t -> p c t", p=128)
        hcol = lambda h: (h % 2) * 4 + h // 2

        def c8(idx):
            return col[:, idx, :]

        for g in range(S // 512):
            sl = slice(g * 512, (g + 1) * 512)
            P.dma("pool", xg[:], xTa[:, :, sl], writes=[xg.res])
            for c in range(8):
                isq = c < 4
                W, Bt, CW, OUT = (wmq, bmq, cwq, mqT) if isq else (wmk, bmk, cwk, mkT)
                cc = c % 4
                A = pj[c % 2]
                CB = cb[c]
                T = ctmp[c % 2]
                for k in range(8):
                    P.op("pe", lambda E, A=A, W=W, k=k, cc=cc: E.matmul(
                        A[:], W[:, k, cc * 128:(cc + 1) * 128], xg[:, k, :], start=(k == 0), stop=(k == 7)),
                        reads=[W.res, xg.res], writes=[A.res], inc=(k == 7))
                P.op("dve", lambda E, CB=CB, A=A, Bt=Bt, cc=cc: E.tensor_scalar(
                    out=CB[:, 3:515], in0=A[:], scalar1=Bt[:, cc:cc + 1], scalar2=None, op0=ALU.add),
                    reads=[A.res, Bt.res], writes=[CB.res])
                if g == 0:
                    P.op("dve", lambda E, CB=CB: E.tensor_scalar(
                        out=CB[:, 3:131], in0=CB[:, 3:131], scalar1=vflag[:, 0:1], scalar2=None, op0=ALU.mult),
                        reads=[CB.res, vflag.res], writes=[CB.res])
                P.op("dve", lambda E, T=T, CB=CB, CW=CW, cc=cc: E.tensor_scalar(
                    out=T[:], in0=CB[:, 0:512], scalar1=CW[:, cc * 4:cc * 4 + 1], scalar2=None, op0=ALU.mult),
                    reads=[CB.res, CW.res], writes=[T.res])
                for j in range(1, 4):
                    P.op("dve", lambda E, T=T, CB=CB, CW=CW, cc=cc, j=j: E.scalar_tensor_tensor(
                        out=T[:], in0=CB[:, j:j + 512], scalar=CW[:, cc * 4 + j:cc * 4 + j + 1], in1=T[:],
                        op0=ALU.mult, op1=ALU.add),
                        reads=[CB.res, CW.res, T.res], writes=[T.res])
                P.op("pool", lambda E, CB=CB: E.tensor_copy(out=CB[:, 0:3], in_=CB[:, 512:515]),
                     reads=[CB.res], writes=[CB.res])
                if isq:
                    P.op("act", lambda E, T=T, cc=cc: E.activation(out=mqT[:, cc, :], in_=T[:], func=AF.Silu),
                         reads=[T.res], writes=[mqT.res])
                else:
                    P.op("act", lambda E, T=T, cc=cc: E.activation(out=mkT[:, cc, :], in_=T[:], func=AF.Silu),
                         reads=[T.res], writes=[mkT.res])
            for blk in range(4):
                bsl = slice(blk * 128, (blk + 1) * 128)
                VA = vaug[blk]
                for half in range(2):
                    A = pj[half]
                    for k in range(8):
                        P.op("pe", lambda E, A=A, k=k, bsl=bsl, half=half: E.matmul(
                            A[:], xg[:, k, bsl], wmv[:, k, half * 512:(half + 1) * 512], start=(k == 0), stop=(k == 7)),
                            reads=[xg.res, wmv.res], writes=[A.res], inc=(k == 7))
                    P.op("dve", lambda E, A=A, VA=VA, half=half: E.tensor_tensor(
                        out=VA[:, half * 4:(half + 1) * 4, 0:128], in0=A[:].rearrange("p (h d) -> p h d", h=4),
                        in1=bmv[:, half * 512:(half + 1) * 512].rearrange("p (h d) -> p h d", h=4), op=ALU.add),
                        reads=[A.res, bmv.res], writes=[VA.res])
            for blk in range(4):
                pb = g * 4 + blk
                own = (pb % 2 == 1)
                bsl = slice(blk * 128, (blk + 1) * 128)
                VA = vaug[blk]
                for k in range(8):
                    P.op("pe", lambda E, k=k, bsl=bsl: E.matmul(
                        pj[0][:, 0:16], xg[:, k, bsl], wmif[:, k, :], start=(k == 0), stop=(k == 7)),
                        reads=[xg.res, wmif.res], writes=[pj[0].res])
                P.op("dve", lambda E: E.tensor_tensor(out=gt[:], in0=pj[0][:, 0:16], in1=bif[:], op=ALU.add),
                     reads=[pj[0].res, bif.res], writes=[gt.res])
                P.op("act", lambda E: E.activation(out=ef[:], in_=gt[:, 8:16], func=AF.Exp, scale=-1.0),
                     reads=[gt.res], writes=[ef.res])
                P.op("act", lambda E: E.activation(out=ef[:], in_=ef[:], func=AF.Ln, bias=1.0),
                     reads=[ef.res], writes=[ef.res])
                P.op("dve", lambda E: E.tensor_scalar(out=lf[:], in0=ef[:], scalar1=-1.0, scalar2=None, op0=ALU.mult),
                     reads=[ef.res], writes=[lf.res])
                if pb == 0:
                    P.op("dve", lambda E: E.tensor_scalar(out=lf[:], in0=lf[:], scalar1=vflag[:, 0:1], scalar2=None, op0=ALU.mult),
                         reads=[lf.res, vflag.res], writes=[lf.res])
                    P.op("dve", lambda E: E.tensor_scalar(out=gt[:, 0:8], in0=gt[:, 0:8], scalar1=vflag[:, 0:1],
                                                          scalar2=ineg[:, 0:1], op0=ALU.mult, op1=ALU.add),
                         reads=[gt.res, vflag.res, ineg.res], writes=[gt.res])
                if own:
                    for half in range(2):
                        A = pj[1]
                        hs = slice(half * 512, (half + 1) * 512)
                        for k in range(8):
                            P.op("pe", lambda E, A=A, k=k, bsl=bsl, hs=hs: E.matmul(
                                A[:], xg[:, k, bsl], wmo[:, k, hs], start=(k == 0), stop=(k == 7)),
                                reads=[xg.res, wmo.res], writes=[A.res], inc=(k == 7))
                        P.op("dve", lambda E, A=A, hs=hs: E.tensor_tensor(out=og[:, hs], in0=A[:], in1=bmo[:, hs], op=ALU.add),
                             reads=[A.res, bmo.res], writes=[og.res])
                    P.op("act", lambda E: E.activation(out=og[:], in_=og[:], func=AF.Exp, scale=-1.0), reads=[og.res], writes=[og.res])
                    P.op("dve", lambda E: E.tensor_scalar(out=og[:], in0=og[:], scalar1=1.0, scalar2=None, op0=ALU.add),
                         reads=[og.res], writes=[og.res])
                    P.op("dve", lambda E: E.reciprocal(out=og[:], in_=og[:]), reads=[og.res], writes=[og.res])
                P.op("pe", lambda E: E.matmul(pj[1][:, 0:8], tri[:], lf[:], start=True, stop=True),
                     reads=[tri.res, lf.res], writes=[pj[1].res])
                P.op("pe", lambda E: E.matmul(pj[1][:, 8:16], ones[:], lf[:], start=True, stop=True),
                     reads=[ones.res, lf.res], writes=[pj[1].res])
                P.op("dve", lambda E: E.tensor_copy(out=bt[:], in_=pj[1][:, 0:16]), reads=[pj[1].res], writes=[bt.res])
                P.op("dve", lambda E: E.tensor_tensor(out=av[:], in0=gt[:, 0:8], in1=bt[:, 0:8], op=ALU.subtract),
                     reads=[gt.res, bt.res], writes=[av.res])
                for h in range(8):
                    P.op("dve", lambda E, h=h: E.tensor_scalar(
                        out=diag[:, h, :], in0=identf[:], scalar1=av[:, h:h + 1], scalar2=None, op0=ALU.mult),
                        reads=[identf.res, av.res], writes=[diag_r[h]])
                for half in range(2):
                    P.op("pe", lambda E, half=half: E.matmul(
                        pbc[:, half * 512:(half + 1) * 512], ones[:],
                        diag[:, half * 4:(half + 1) * 4, :].rearrange("p h s -> p (h s)"), start=True, stop=True),
                        reads=[ones.res] + diag_r[half * 4:(half + 1) * 4], writes=[pbc.res])
                P.op("dve", lambda E: E.tensor_reduce(out=c8(AMX), in_=pbc[:].rearrange("p (h s) -> p h s", h=8), axis=AX.X, op=ALU.max),
                     reads=[pbc.res], writes=[col.res])
                P.op("dve", lambda E: E.tensor_tensor(out=c8(MNB), in0=c8(AMX), in1=mprev[:], op=ALU.max),
                     reads=[col.res, mprev.res], writes=[col.res])
                if own:
                    P.op("dve", lambda E: E.tensor_tensor(out=Am[:].rearrange("p h s -> p (h s)"), in0=pbc[:], in1=cmk[:], op=ALU.add),
                         reads=[pbc.res, cmk.res], writes=[Am.res])
                    P.op("dve", lambda E: E.tensor_reduce(out=c8(CMAX), in_=Am[:], axis=AX.X, op=ALU.max),
                         reads=[Am.res], writes=[col.res])
                    P.op("dve", lambda E: E.tensor_tensor(out=c8(INTER), in0=bt[:, 0:8], in1=mprev[:], op=ALU.add),
                         reads=[bt.res, mprev.res], writes=[col.res])
                    P.op("dve", lambda E: E.tensor_tensor(out=c8(MT), in0=bt[:, 0:8], in1=c8(CMAX), op=ALU.add),
                         reads=[bt.res, col.res], writes=[col.res])
                    P.op("dve", lambda E: E.tensor_tensor(out=c8(MT), in0=c8(MT), in1=c8(INTER), op=ALU.max),
                         reads=[col.res], writes=[col.res])
                    P.op("dve", lambda E: E.tensor_tensor(out=c8(RR), in0=bt[:, 0:8], in1=c8(MT), op=ALU.subtract),
                         reads=[bt.res, col.res], writes=[col.res])
                    P.op("dve", lambda E: E.tensor_tensor(out=c8(TMP), in0=c8(INTER), in1=c8(MT), op=ALU.subtract),
                         reads=[col.res], writes=[col.res])
                    P.op("act", lambda E: E.activation(out=c8(WINT), in_=c8(TMP), func=AF.Exp, bias=LN_EIGHTH), reads=[col.res], writes=[col.res])
                    P.op("act", lambda E: E.activation(out=c8(EMT), in_=c8(MT), func=AF.Exp, scale=-1.0), reads=[col.res], writes=[col.res])
                    for h in range(8):
                        P.op("dve", lambda E, h=h: E.tensor_scalar(
                            out=diag[:, h, :], in0=identf[:], scalar1=col[:, RR, h:h + 1], scalar2=None, op0=ALU.mult),
                            reads=[identf.res, col.res], writes=[diag_r[h]])
                    for half in range(2):
                        P.op("pe", lambda E, half=half: E.matmul(
                            pbc[:, half * 512:(half + 1) * 512], ones[:],
                            diag[:, half * 4:(half + 1) * 4, :].rearrange("p h s -> p (h s)"), start=True, stop=True),
                            reads=[ones.res] + diag_r[half * 4:(half + 1) * 4], writes=[pbc.res])
                    P.op("dve", lambda E: E.tensor_tensor(out=Am[:].rearrange("p h s -> p (h s)"), in0=pbc[:], in1=cmkT[:], op=ALU.add),
                         reads=[pbc.res, cmkT.res], writes=[Am.res])
                    for h in range(8):
                        P.op("act", lambda E, h=h: E.activation(
                            out=DT[:, hcol(h), :], in_=Am[:, h, :], func=AF.Exp, bias=av[:, h:h + 1]),
                            reads=[Am.res, av.res], writes=[DT_r[h]])
                    for h in range(8):
                        hp = slice((h % 2) * 64, (h % 2) * 64 + 64)
                        c0 = hcol(h) * 128
                        P.op("pe", lambda E, h=h, hp=hp, c0=c0, bsl=bsl: E.matmul(
                            psS[:, c0:c0 + 128], mkT[hp, h // 2, bsl], mqT[hp, h // 2, bsl], start=True, stop=True),
                            reads=[mkT.res, mqT.res], writes=[psS.res])
                    P.op("dve", lambda E: E.tensor_tensor(out=PT[:], in0=psS[:], in1=DT[:].rearrange("p h s -> p (h s)"), op=ALU.mult),
                         reads=[psS.res] + DT_r, writes=[PT.res])
                    for h in range(8):
                        hp = slice((h % 2) * 64, (h % 2) * 64 + 64)
                        c0 = hcol(h) * 128
                        P.op("pe", lambda E, c0=c0, VA=VA, h=h: E.matmul(
                            pj[0][:, 0:129], PT[:, c0:c0 + 128], VA[:, h, :], start=True, stop=True),
                            reads=[PT.res, VA.res], writes=[pj[0].res])
                        P.op("pe", lambda E, hp=hp, h=h, bsl=bsl: E.matmul(
                            pj[1][:, 0:129], mqT[hp, h // 2, bsl], Cbf[hp, h // 2, :], start=True, stop=True),
                            reads=[mqT.res, Cbf.res], writes=[pj[1].res])
                        P.op("dve", lambda E: E.tensor_scalar(out=n1s[:], in0=pj[0][:, 0:129], scalar1=0.125, scalar2=None, op0=ALU.mult),
                             reads=[pj[0].res], writes=[n1s.res])
                        P.op("dve", lambda E, h=h: E.scalar_tensor_tensor(
                            out=nums[:], in0=pj[1][:, 0:129], scalar=col[:, WINT, h:h + 1], in1=n1s[:],
                            op0=ALU.mult, op1=ALU.add),
                            reads=[pj[1].res, col.res, n1s.res], writes=[nums.res])
                        P.op("dve", lambda E, h=h: E.tensor_tensor(out=sc1[:, 0:1], in0=nums[:, 128:129], in1=col[:, EMT, h:h + 1], op=ALU.max),
                             reads=[nums.res, col.res], writes=[sc1.res])
                        P.op("dve", lambda E: E.scalar_tensor_tensor(out=sc1[:, 1:2], in0=nums[:, 128:129], scalar=-1.0, in1=sc1[:, 0:1],
                                                                      op0=ALU.mult, op1=ALU.max),
                             reads=[nums.res, sc1.res], writes=[sc1.res])
                        P.op("dve", lambda E: E.reciprocal(out=sc1[:, 2:3], in_=sc1[:, 1:2]), reads=[sc1.res], writes=[sc1.res])
                        P.op("dve", lambda E, h=h: E.tensor_scalar(
                            out=hbuf[:, h, :], in0=nums[:, 0:128], scalar1=sc1[:, 2:3], scalar2=None, op0=ALU.mult),
                            reads=[nums.res, sc1.res], writes=[hbuf_r[h]])
                    P.op("dve", lambda E: E.tensor_reduce(out=c8(TMP), in_=hbuf[:], axis=AX.X, op=ALU.add),
                         reads=hbuf_r, writes=[col.res])
                    P.op("dve", lambda E: E.tensor_tensor(out=sqt[:], in0=hbuf[:], in1=hbuf[:], op=ALU.mult), reads=hbuf_r, writes=[sqt.res])
                    P.op("dve", lambda E: E.tensor_reduce(out=c8(CMAX), in_=sqt[:], axis=AX.X, op=ALU.add),
                         reads=[sqt.res], writes=[col.res])
                    P.op("dve", lambda E: E.tensor_scalar(out=c8(TMP), in0=c8(TMP), scalar1=-1.0 / 128.0, scalar2=None, op0=ALU.mult),
                         reads=[col.res], writes=[col.res])
                    P.op("dve", lambda E: E.tensor_tensor(out=c8(INTER), in0=c8(TMP), in1=c8(TMP), op=ALU.mult),
                         reads=[col.res], writes=[col.res])
                    P.op("dve", lambda E: E.scalar_tensor_tensor(out=c8(CMAX), in0=c8(CMAX), scalar=1.0 / 128.0, in1=c8(INTER),
                                                                  op0=ALU.mult, op1=ALU.subtract),
                         reads=[col.res], writes=[col.res])
                    P.op("dve", lambda E: E.tensor_scalar(out=c8(CMAX), in0=c8(CMAX), scalar1=GN_EPS, scalar2=None, op0=ALU.add),
                         reads=[col.res], writes=[col.res])
                    P.op("act", lambda E: E.activation(out=c8(CMAX), in_=c8(CMAX), func=AF.Ln), reads=[col.res], writes=[col.res])
                    P.op("act", lambda E: E.activation(out=c8(CMAX), in_=c8(CMAX), func=AF.Exp, scale=-0.5), reads=[col.res], writes=[col.res])
                    for h in range(8):
                        P.op("dve", lambda E, h=h: E.tensor_scalar(
                            out=hbuf[:, h, :], in0=hbuf[:, h, :], scalar1=col[:, TMP, h:h + 1], scalar2=col[:, CMAX, h:h + 1],
                            op0=ALU.add, op1=ALU.mult),
                            reads=[hbuf_r[h], col.res], writes=[hbuf_r[h]])
                    P.op("dve", lambda E: E.tensor_tensor(out=hbuf[:].rearrange("p h s -> p (h s)"),
                                                          in0=hbuf[:].rearrange("p h s -> p (h s)"), in1=gnb[:], op=ALU.mult),
                         reads=hbuf_r + [gnb.res], writes=hbuf_r)
                    P.op("dve", lambda E: E.tensor_tensor(out=ybf[:], in0=hbuf[:].rearrange("p h s -> p (h s)"), in1=og[:], op=ALU.mult),
                         reads=hbuf_r + [og.res], writes=[ybf.res])
                    for c in range(8):
                        P.op("pe", lambda E, c=c: E.transpose(out=pT[:, c * 128:(c + 1) * 128], in_=ybf[:, c * 128:(c + 1) * 128], identity=identb[:]),
                             reads=[ybf.res, identb.res], writes=[pT.res])
                    P.op("act", lambda E: E.activation(out=yT[:], in_=pT[:].rearrange("p (c t) -> p c t", c=8), func=AF.Identity),
                         reads=[pT.res], writes=[yT.res])
                    i = pb // 2
                    P.dma("sp", ymT_s[:, :, i * 128:(i + 1) * 128], yT[:], reads=[yT.res])
                P.op("dve", lambda E: E.tensor_tensor(out=c8(MNEW), in0=bt[:, 8:16], in1=c8(MNB), op=ALU.add),
                     reads=[bt.res, col.res], writes=[col.res])
                P.op("dve", lambda E: E.tensor_tensor(out=c8(CD), in0=bt[:, 8:16], in1=mprev[:], op=ALU.add),
                     reads=[bt.res, mprev.res], writes=[col.res])
                P.op("dve", lambda E: E.tensor_tensor(out=c8(CD), in0=c8(CD), in1=c8(MNEW), op=ALU.subtract),
                     reads=[col.res], writes=[col.res])
                P.op("act", lambda E: E.activation(out=c8(CD), in_=c8(CD), func=AF.Exp), reads=[col.res], writes=[col.res])
                P.op("dve", lambda E: E.tensor_tensor(out=c8(ES), in0=av[:], in1=bt[:, 8:16], op=ALU.add),
                     reads=[av.res, bt.res], writes=[col.res])
                P.op("dve", lambda E: E.tensor_tensor(out=c8(ES), in0=c8(ES), in1=c8(MNEW), op=ALU.subtract),
                     reads=[col.res], writes=[col.res])
                P.op("act", lambda E: E.activation(out=c8(ES), in_=c8(ES), func=AF.Exp), reads=[col.res], writes=[col.res])
                for c in range(4):
                    P.op("pe", lambda E, c=c, bsl=bsl: E.transpose(out=pT[:, c * 128:(c + 1) * 128], in_=mkT[:, c, bsl], identity=identb[:]),
                         reads=[mkT.res, identb.res], writes=[pT.res])
                for h in range(8):
                    P.op("dve", lambda E, h=h: E.tensor_scalar(
                        out=wk[:, h, :], in0=pT[:, h * 64:(h + 1) * 64], scalar1=col[:, ES, h:h + 1], scalar2=None, op0=ALU.mult),
                        reads=[pT.res, col.res], writes=[wk_r[h]])
                for h in range(8):
                    hp = slice((h % 2) * 64, (h % 2) * 64 + 64)
                    off = ((h // 2) // 2) * 512 + ((h // 2) % 2) * 129
                    P.op("pe", lambda E, h=h, hp=hp, off=off, VA=VA: E.matmul(
                        pbc[hp, off:off + 129], wk[:, h, :], VA[:, h, :], start=True, stop=True),
                        reads=[wk_r[h], VA.res], writes=[pbc.res])
                for h in range(8):
                    hp = slice((h % 2) * 64, (h % 2) * 64 + 64)
                    off = ((h // 2) // 2) * 512 + ((h // 2) % 2) * 129
                    P.op("dve", lambda E, h=h, hp=hp, off=off: E.scalar_tensor_tensor(
                        out=Cst[hp, h // 2, :], in0=Cst[hp, h // 2, :], scalar=col[hp, CD, h:h + 1], in1=pbc[hp, off:off + 129],
                        op0=ALU.mult, op1=ALU.add),
                        reads=[Cst_r[h], col.res, pbc.res], writes=[Cst_r[h]])
                P.op("act", lambda E: E.activation(out=Cbf[:], in_=Cst[:], func=AF.Identity), reads=Cst_r, writes=[Cbf.res])
                P.op("dve", lambda E: E.tensor_copy(out=mprev[:], in_=c8(MNEW)), reads=[col.res], writes=[mprev.res])
    P.barrier()


IN_SPECS = {
    "xT": ([D, S], F32), "xT_own": ([D, NOWN], F32), "x_own": ([NOWN, D], F32),
    "cosk": ([128, S], F32), "sink": ([128, S], F32),
    "cosq": ([128, NOWN], F32), "sinq": ([128, NOWN], F32),
    "cosi": ([128, NOWN], F32), "sini": ([128, NOWN], F32),
    "ident": ([128, 128], F32), "irep": ([128, 512], F32),
    "negm_diag": ([128, 128], F32), "negm_b0": ([128, 128], F32),
    "wq": ([D, 512], F32), "wq_s": ([D, 512], F32), "wk": ([D, 512], F32), "wk_s": ([D, 512], F32),
    "wv": ([D, 512], F32), "wiq": ([D, 256], F32), "wiq_s": ([D, 256], F32),
    "wik2": ([D, 128], F32), "wik2_s": ([D, 128], F32), "wiw": ([D, 4], F32),
    "bq": ([128, 4], F32), "bq_s": ([128, 4], F32), "bk": ([128, 4], F32), "bk_s": ([128, 4], F32),
    "biq": ([128, 2], F32), "biq_s": ([128, 2], F32), "bik2": ([128, 1], F32), "bik2_s": ([128, 1], F32),
    "bv_bc": ([128, 512], F32), "biw_bc": ([128, 4], F32),
    "wga": ([D, D], F32), "wgm": ([D, D], F32), "bga": ([128, 8], F32), "bgm": ([128, 8], F32),
    "wba": ([512, D], F32), "wbm": ([D, D], F32), "wout": ([D, D], F32),
    "ln1g_bc": ([128, D], F32), "ln1b_bc": ([128, D], F32), "ln2g_bc": ([128, D], F32), "ln2b_bc": ([128, D], F32),
    "wr": ([D, 20], F32), "br_bc": ([128, 20], F32),
    "weg": ([16, D, 512], F32), "weu": ([16, D, 512], F32), "wed": ([16, 512, D], F32),
    "wmq": ([D, 512], F32), "wmk": ([D, 512], F32), "bmq": ([128, 4], F32), "bmk": ([128, 4], F32),
    "cwq": ([128, 16], F32), "cwk": ([128, 16], F32),
    "wmv": ([D, D], F32), "bmv_bc": ([128, D], F32), "wmif": ([D, 16], F32), "bif_bc": ([128, 16], F32),
    "wmo": ([D, D], F32), "bmo_bc": ([128, D], F32), "gn_bc": ([128, D], F32),
    "tri": ([128, 128], F32), "ones": ([128, 128], F32),
    "cmask_rep": ([128, 1024], F32), "cmaskT_rep": ([128, 1024], F32),
    "vflag": ([128, 1], F32), "ineg": ([128, 1], F32),
    "pow2": ([128, 32], F32),
}
GN_EPS = 1e-6
LN_EIGHTH = float(np.log(0.125))
C1SUB = os.environ.get("K_C1SUB", "")
ALPHA = float(2.0 ** 0.25)
LN_EPS = 1e-5


def build_nc(debug=(), stop=None):
    nc = bass.Bass("TRN2", target_bir_lowering=False)
    Dm = {}
    skip = set()
    if stop in ("q", "kv", "A1", "A2", "A3", "C1", "M", "A"):
        skip |= {"weg", "weu", "wed"}
    for name, (shape, dt) in IN_SPECS.items():
        if name in skip:
            continue
        Dm[name] = nc.dram_tensor(name, shape, dt, kind="ExternalInput").ap()
    nc._declared = set(Dm)

    def scratch(name, shape, dt):
        kind = "ExternalOutput" if name in debug else "Internal"
        return nc.dram_tensor(name, shape, dt, kind=kind).ap()

    QT_s = scratch("QT_s", [128, 4, NOWN], BF16)
    qiT_s = scratch("qiT_s", [128, 2, NOWN], BF16)
    yat_s = scratch("yat_s", [NOWN, 512], BF16)
    ymT_s = nc.dram_tensor("ymT_s", [128, 8, NOWN], BF16, kind=("ExternalInput" if "ymT_in" in debug else ("ExternalOutput" if "ymT_s" in debug else "Internal"))).ap()
    x1_s = scratch("x1_s", [NOWN, D], F32)
    x1T_s = scratch("x1T_s", [128, 8, NOWN], BF16)
    out = nc.dram_tensor("out", [NOWN, D], F32, kind="ExternalOutput").ap()

    es = ExitStack()
    with es:
        P = Prog(nc, es)
        top = ExitStack()
        es.enter_context(top)
        Wabs = _sb(nc, top, "Wabs", [128, NPAIR, 4], F32)
        Wsgn = _sb(nc, top, "Wsgn", [128, NPAIR, 4], F32)

        with ExitStack() as ph:
          if stop != 'M':
            wq = _sb(nc, ph, "wq", [128, 8, 512], BF16)
            wqs = _sb(nc, ph, "wqs", [128, 8, 512], BF16)
            wiq = _sb(nc, ph, "wiq", [128, 8, 256], BF16)
            wiqs = _sb(nc, ph, "wiqs", [128, 8, 256], BF16)
            wiw = _sb(nc, ph, "wiw", [128, 8, 4], BF16)
            bq = _sb(nc, ph, "bq", [128, 4], F32)
            bqs = _sb(nc, ph, "bqs", [128, 4], F32)
            biq = _sb(nc, ph, "biq", [128, 2], F32)
            biqs = _sb(nc, ph, "biqs", [128, 2], F32)
            biw = _sb(nc, ph, "biw", [128, 4], F32)
            for t, n in ((wq, "wq"), (wqs, "wq_s"), (wiq, "wiq"), (wiqs, "wiq_s"), (wiw, "wiw")):
                P.dma("pool", t[:], _wview(Dm[n]), writes=[t.res])
            for t, n in ((bq, "bq"), (bqs, "bq_s"), (biq, "biq"), (biqs, "biq_s"), (biw, "biw_bc")):
                P.dma("sp", t[:], Dm[n][:, :], writes=[t.res])
            xo = [_sb(nc, ph, f"xo{j}", [128, 8, 512], BF16) for j in range(2)]
            tabs = [[_sb(nc, ph, f"tab{j}_{n}", [128, 512], F32) for n in range(4)] for j in range(2)]
            t1 = [_sb(nc, ph, f"t1_{j}", [128, 512], F32) for j in range(2)]
            t2 = [_sb(nc, ph, f"t2_{j}", [128, 512], F32) for j in range(2)]
            qo = [_sb(nc, ph, f"qo{j}", [128, 4, 512], BF16) for j in range(2)]
            qio = [_sb(nc, ph, f"qio{j}", [128, 2, 512], BF16) for j in range(2)]
            wtmp = _sb(nc, ph, "wtmp", [128, 4], F32)
            psA = [_ps(nc, ph, f"psA{j}", [128, 512], F32) for j in range(3)]
            psB = [_ps(nc, ph, f"psB{j}", [128, 512], F32) for j in range(3)]
            psW = _ps(nc, ph, "psW", [128, 512], F32)
            xTo = Dm["xT_own"].rearrange("(c p) t -> p c t", p=128)
            rot = 0
            for g in range(NOWN // 512):
                sl = slice(g * 512, (g + 1) * 512)
                X = xo[g % 2]
                TB = tabs[g % 2]
                P.dma("pool", X[:], xTo[:, :, sl], writes=[X.res])
                for n, nm in enumerate(("cosq", "sinq", "cosi", "sini")):
                    P.dma("sp", TB[n][:], Dm[nm][:, sl], writes=[TB[n].res])
                QO, QIO = qo[g % 2], qio[g % 2]
                for c in range(6):
                    isq = c < 4
                    W, Ws, Bt, Bs = (wq, wqs, bq, bqs) if isq else (wiq, wiqs, biq, biqs)
                    cc = c if isq else c - 4
                    ct, st = (TB[0], TB[1]) if isq else (TB[2], TB[3])
                    A, B = psA[rot % 3], psB[rot % 3]
                    T1, T2 = t1[rot % 2], t2[rot % 2]
                    rot += 1
                    for k in range(8):
                        P.op("pe", lambda E, A=A, W=W, X=X, k=k, cc=cc: E.matmul(
                            A[:], W[:, k, cc * 128:(cc + 1) * 128], X[:, k, :], start=(k == 0), stop=(k == 7)),
                            reads=[W.res, X.res], writes=[A.res], inc=(k == 7))
                    for k in range(8):
                        P.op("pe", lambda E, B=B, Ws=Ws, X=X, k=k, cc=cc: E.matmul(
                            B[:], Ws[:, k, cc * 128:(cc + 1) * 128], X[:, k, :], start=(k == 0), stop=(k == 7)),
                            reads=[Ws.res, X.res], writes=[B.res], inc=(k == 7))
                    P.op("dve", lambda E, T1=T1, A=A, Bt=Bt, cc=cc, ct=ct: E.scalar_tensor_tensor(
                        out=T1[:], in0=A[:], scalar=Bt[:, cc:cc + 1], in1=ct[:], op0=ALU.add, op1=ALU.mult),
                        reads=[A.res, Bt.res, ct.res], writes=[T1.res])
                    P.op("dve", lambda E, T2=T2, B=B, Bs=Bs, cc=cc, st=st: E.scalar_tensor_tensor(
                        out=T2[:], in0=B[:], scalar=Bs[:, cc:cc + 1], in1=st[:], op0=ALU.add, op1=ALU.mult),
                        reads=[B.res, Bs.res, st.res], writes=[T2.res])
                    O = QO if isq else QIO
                    P.op("pool", lambda E, O=O, cc=cc, T1=T1, T2=T2: E.tensor_tensor(
                        out=O[:, cc, :], in0=T1[:], in1=T2[:], op=ALU.add),
                        reads=[T1.res, T2.res], writes=[O.res])
                P.dma("sp", QT_s[:, :, sl], QO[:], reads=[QO.res])
                P.dma("sp", qiT_s[:, :, sl], QIO[:], reads=[QIO.res])
                for blk in range(4):
                    i = g * 4 + blk
                    for k in range(8):
                        P.op("pe", lambda E, X=X, k=k, blk=blk: E.matmul(
                            psW[:, 0:4], X[:, k, blk * 128:(blk + 1) * 128], wiw[:, k, :],
                            start=(k == 0), stop=(k == 7)),
                            reads=[X.res, wiw.res], writes=[psW.res], inc=(k == 7))
                    P.op("dve", lambda E: E.tensor_tensor(out=wtmp[:], in0=psW[:, 0:4], in1=biw[:], op=ALU.add),
                         reads=[psW.res, biw.res], writes=[wtmp.res])
                    P.op("act", lambda E, i=i: E.activation(
                        out=Wabs[:, i, :], in_=wtmp[:], func=AF.Abs, scale=1.0 / 16.0),
                        reads=[wtmp.res], writes=[Wabs.res])
                    P.op("dve", lambda E, i=i: E.tensor_scalar(
                        out=Wsgn[:, i, :], in0=wtmp[:], scalar1=0.0, scalar2=2.0,
                        op0=ALU.is_ge, op1=ALU.mult),
                        reads=[wtmp.res], writes=[Wsgn.res])
                    P.op("dve", lambda E, i=i: E.tensor_scalar(
                        out=Wsgn[:, i, :], in0=Wsgn[:, i, :], scalar1=-1.0, scalar2=None, op0=ALU.add),
                        reads=[Wsgn.res], writes=[Wsgn.res])

        P.barrier()
        with ExitStack() as kv:
          if stop not in ('q', 'M'):
            KT = _sb(nc, kv, "KT", [128, 4, S], BF16)
            V = _sb(nc, kv, "V", [128, 64, 8, 65], BF16)
            kiT = _sb(nc, kv, "kiT", [128, S], BF16)
            KTr = [Res(f"KT{g}") for g in range(16)]
            Vr = [Res(f"V{g}") for g in range(16)]
            kir = [Res(f"ki{g}") for g in range(16)]
            P.op("pool", lambda E: E.memset(V[:, :, :, 64:65], 1.0), writes=Vr)
            with ExitStack() as ph:
                wk = _sb(nc, ph, "wk", [128, 8, 512], BF16)
                wks = _sb(nc, ph, "wks", [128, 8, 512], BF16)
                wv = _sb(nc, ph, "wv", [128, 8, 512], BF16)
                wik = _sb(nc, ph, "wik", [128, 8, 128], BF16)
                wiks = _sb(nc, ph, "wiks", [128, 8, 128], BF16)
                bk = _sb(nc, ph, "bk", [128, 4], F32)
                bks = _sb(nc, ph, "bks", [128, 4], F32)
                bik = _sb(nc, ph, "bik", [128, 1], F32)
                biks = _sb(nc, ph, "biks", [128, 1], F32)
                bv = _sb(nc, ph, "bv", [128, 512], F32)
                for t, n in ((wk, "wk"), (wks, "wk_s"), (wv, "wv"), (wik, "wik2"), (wiks, "wik2_s")):
                    P.dma("pool", t[:], _wview(Dm[n]), writes=[t.res])
                for t, n in ((bk, "bk"), (bks, "bk_s"), (bik, "bik2"), (biks, "bik2_s"), (bv, "bv_bc")):
                    P.dma("sp", t[:], Dm[n][:, :], writes=[t.res])
                xg = _sb(nc, ph, "xg", [128, 8, 512], BF16)
                ck = _sb(nc, ph, "ck", [128, 512], F32)
                sk = _sb(nc, ph, "sk", [128, 512], F32)
                t1 = [_sb(nc, ph, f"k_t1_{j}", [128, 512], F32) for j in range(2)]
                t2 = [_sb(nc, ph, f"k_t2_{j}", [128, 512], F32) for j in range(2)]
                psA = [_ps(nc, ph, f"kpsA{j}", [128, 512], F32) for j in range(3)]
                psB = [_ps(nc, ph, f"kpsB{j}", [128, 512], F32) for j in range(3)]
                psV = [_ps(nc, ph, f"kpsV{j}", [128, 512], F32) for j in range(2)]
                xTa = Dm["xT"].rearrange("(c p) t -> p c t", p=128)
                rot = 0
                for g in range(S // 512):
                    sl = slice(g * 512, (g + 1) * 512)
                    P.dma("pool", xg[:], xTa[:, :, sl], writes=[xg.res])
                    P.dma("sp", ck[:], Dm["cosk"][:, sl], writes=[ck.res])
                    P.dma("sp", sk[:], Dm["sink"][:, sl], writes=[sk.res])
                    for c in range(5):
                        isk = c < 4
                        W, Ws, Bt, Bs = (wk, wks, bk, bks) if isk else (wik, wiks, bik, biks)
                        cc = c if isk else 0
                        A, B = psA[rot % 3], psB[rot % 3]
                        T1, T2 = t1[rot % 2], t2[rot % 2]
                        rot += 1
                        for k in range(8):
                            P.op("pe", lambda E, A=A, W=W, k=k, cc=cc: E.matmul(
                                A[:], W[:, k, cc * 128:(cc + 1) * 128], xg[:, k, :], start=(k == 0), stop=(k == 7)),
                                reads=[W.res, xg.res], writes=[A.res], inc=(k == 7))
                        for k in range(8):
                            P.op("pe", lambda E, B=B, Ws=Ws, k=k, cc=cc: E.matmul(
                                B[:], Ws[:, k, cc * 128:(cc + 1) * 128], xg[:, k, :], start=(k == 0), stop=(k == 7)),
                                reads=[Ws.res, xg.res], writes=[B.res], inc=(k == 7))
                        P.op("dve", lambda E, T1=T1, A=A, Bt=Bt, cc=cc: E.scalar_tensor_tensor(
                            out=T1[:], in0=A[:], scalar=Bt[:, cc:cc + 1], in1=ck[:], op0=ALU.add, op1=ALU.mult),
                            reads=[A.res, Bt.res, ck.res], writes=[T1.res])
                        P.op("dve", lambda E, T2=T2, B=B, Bs=Bs, cc=cc: E.scalar_tensor_tensor(
                            out=T2[:], in0=B[:], scalar=Bs[:, cc:cc + 1], in1=sk[:], op0=ALU.add, op1=ALU.mult),
                            reads=[B.res, Bs.res, sk.res], writes=[T2.res])
                        if isk:
                            P.op("pool", lambda E, cc=cc, T1=T1, T2=T2, sl=sl: E.tensor_tensor(
                                out=KT[:, cc, sl], in0=T1[:], in1=T2[:], op=ALU.add),
                                reads=[T1.res, T2.res], writes=[KTr[g]])
                        else:
                            P.op("pool", lambda E, T1=T1, T2=T2, sl=sl: E.tensor_tensor(
                                out=kiT[:, sl], in0=T1[:], in1=T2[:], op=ALU.add),
                                reads=[T1.res, T2.res], writes=[kir[g]])
                    for blk in range(4):
                        pb = g * 4 + blk
                        PV = psV[blk % 2]
                        for k in range(8):
                            P.op("pe", lambda E, PV=PV, k=k, blk=blk: E.matmul(
                                PV[:], xg[:, k, blk * 128:(blk + 1) * 128], wv[:, k, :],
                                start=(k == 0), stop=(k == 7)),
                                reads=[xg.res, wv.res], writes=[PV.res], inc=(k == 7))
                        P.op("dve", lambda E, PV=PV, pb=pb: E.tensor_tensor(
                            out=V[:, pb, :, 0:64], in0=PV[:].rearrange("p (h d) -> p h d", h=8),
                            in1=bv[:].rearrange("p (h d) -> p h d", h=8), op=ALU.add),
                            reads=[PV.res, bv.res], writes=[Vr[g]])

            P.barrier()
            with ExitStack() as ph:
                score = _sb(nc, ph, "score", [128, S], F32)
                Mneg = _sb(nc, ph, "Mneg", [128, S], F8)
                junkt = _sb(nc, ph, "junkt", [128, S], U8)
                irep = _sb(nc, ph, "irep", [128, 512], BF16)
                nmd = _sb(nc, ph, "nmd", [128, 128], BF16)
                nm0 = _sb(nc, ph, "nm0", [128, 128], BF16)
                P.dma("pool", irep[:], Dm["irep"][:, :], writes=[irep.res])
                P.dma("pool", nmd[:], Dm["negm_diag"][:, :], writes=[nmd.res])
                P.dma("pool", nm0[:], Dm["negm_b0"][:, :], writes=[nm0.res])
                QTb = _sb(nc, ph, "QTb", [128, 4, 128], BF16)
                qiTbs = [_sb(nc, ph, f"qiTb{j}", [128, 2, 128], BF16) for j in range(2)]
                rt = [_sb(nc, ph, f"rt{j}", [128, 512], F32) for j in range(2)]
                PT = [_sb(nc, ph, f"PT{j}", [128, 1024], BF16) for j in range(2)]
                ytm = _sb(nc, ph, "ytm", [128, 512], BF16)
                lo = _sb(nc, ph, "lo", [128, 1], F32)
                hi = _sb(nc, ph, "hi", [128, 1], F32)
                mid = _sb(nc, ph, "mid", [128, 1], F32)
                cnt = _sb(nc, ph, "cnt", [128, 1], F32)
                uu = _sb(nc, ph, "uu", [128, 1], F32)
                pw2 = _sb(nc, ph, "pw2", [128, 32], F32)
                hv = _sb(nc, ph, "hv", [128, 32], F32)
                tw = _sb(nc, ph, "tw", [128, 32], F32)
                P.dma("sp", pw2[:], Dm["pow2"][:, :], writes=[pw2.res])
                rz = _sb(nc, ph, "rz", [128, 8], F32)
                psL = [_ps(nc, ph, f"psL{j}", [128, 1024], F32) for j in range(2)]
                psO = [_ps(nc, ph, f"psO{j}", [128, 512], F32) for j in range(2)]
                psI = [_ps(nc, ph, f"psI{j}", [128, 512], F32) for j in range(2)]
                NA = 0 if stop == 'kv' else (3 if stop in ('A1', 'A2', 'A3') else NBLK)

                def a_index(i):
                    nkb = 2 * i + 2
                    Sc = nkb * 128
                    osl = slice(i * 128, (i + 1) * 128)
                    qiTb = qiTbs[i % 2]
                    P.dma("sp", qiTb[:], qiT_s[:, :, osl], writes=[qiTb.res])
                    for kc in range((Sc + 511) // 512):
                        n = min(512, Sc - kc * 512)
                        ksl = slice(kc * 512, kc * 512 + n)
                        for h in range(4):
                            T = psI[h % 2]
                            pr = slice((h % 2) * 64, (h % 2) * 64 + 64)
                            P.op("pe", lambda E, T=T, n=n, pr=pr, h=h, ksl=ksl, qiTb=qiTb: E.matmul(
                                T[:, 0:n], qiTb[pr, h // 2, :], kiT[pr, ksl], start=True, stop=True),
                                reads=[qiTb.res, kir[kc]], writes=[T.res])
                            if h == 0:
                                P.op("act", lambda E, T=T, n=n, ksl=ksl, i=i: E.activation(
                                    out=score[:, ksl], in_=T[:, 0:n], func=AF.Relu, scale=Wabs[:, i, 0:1]),
                                    reads=[T.res, Wabs.res], writes=[score.res])
                                P.op("dve", lambda E, ksl=ksl, i=i: E.tensor_scalar(
                                    out=score[:, ksl], in0=score[:, ksl], scalar1=Wsgn[:, i, 0:1], scalar2=None,
                                    op0=ALU.mult),
                                    reads=[score.res, Wsgn.res], writes=[score.res])
                            else:
                                R = rt[h % 2]
                                P.op("act", lambda E, T=T, n=n, R=R, i=i, h=h: E.activation(
                                    out=R[:, 0:n], in_=T[:, 0:n], func=AF.Relu, scale=Wabs[:, i, h:h + 1]),
                                    reads=[T.res, Wabs.res], writes=[R.res])
                                P.op("dve", lambda E, R=R, n=n, ksl=ksl, i=i, h=h: E.scalar_tensor_tensor(
                                    out=score[:, ksl], in0=R[:, 0:n], scalar=Wsgn[:, i, h:h + 1], in1=score[:, ksl],
                                    op0=ALU.mult, op1=ALU.add),
                                    reads=[R.res, Wsgn.res, score.res], writes=[score.res])
                    P.op("dve", lambda E, Sc=Sc: E.tensor_reduce(out=hi[:], in_=score[:, 0:Sc], axis=AX.X, op=ALU.max),
                         reads=[score.res], writes=[hi.res])
                    P.op("dve", lambda E, Sc=Sc: E.tensor_reduce(out=lo[:], in_=score[:, 0:Sc], axis=AX.X, op=ALU.min),
                         reads=[score.res], writes=[lo.res])
                    P.op("dve", lambda E: E.tensor_tensor(out=score[:, 0:128], in0=score[:, 0:128], in1=nm0[:], op=ALU.add),
                         reads=[score.res, nm0.res], writes=[score.res])
                    P.op("dve", lambda E, Sc=Sc: E.tensor_tensor(
                        out=score[:, Sc - 128:Sc], in0=score[:, Sc - 128:Sc], in1=nmd[:], op=ALU.add),
                        reads=[score.res, nmd.res], writes=[score.res])
                    P.op("dve", lambda E: E.tensor_tensor(out=uu[:], in0=hi[:], in1=lo[:], op=ALU.subtract),
                         reads=[hi.res, lo.res], writes=[uu.res])
                    P.op("dve", lambda E: E.tensor_scalar(out=hv[:], in0=pw2[:], scalar1=uu[:, 0:1], scalar2=None, op0=ALU.mult),
                         reads=[pw2.res, uu.res], writes=[hv.res])
                    P.op("dve", lambda E: E.tensor_scalar(out=tw[:], in0=pw2[:], scalar1=uu[:, 0:1], scalar2=2.0,
                                                          op0=ALU.mult, op1=ALU.mult),
                         reads=[pw2.res, uu.res], writes=[tw.res])
                    P.op("dve", lambda E: E.tensor_tensor(out=mid[:], in0=lo[:], in1=hv[:, 0:1], op=ALU.add),
                         reads=[lo.res, hv.res], writes=[mid.res])
                    for it in range(NIT):
                        last = it == NIT - 1
                        j = it if last else it + 1
                        SA = hv if last else tw
                        P.op("dve", lambda E, Sc=Sc: E.tensor_scalar(
                            out=junkt[:, 0:Sc], in0=score[:, 0:Sc], scalar1=mid[:, 0:1], scalar2=None,
                            op0=ALU.is_ge, op1=ALU.add, accum_out=cnt[:]),
                            reads=[score.res, mid.res], writes=[junkt.res, cnt.res])
                        P.op("dve", lambda E, SA=SA, j=j: E.tensor_scalar(
                            out=uu[:], in0=cnt[:], scalar1=TOPK - 0.5, scalar2=SA[:, j:j + 1], op0=ALU.is_ge, op1=ALU.mult),
                            reads=[cnt.res, SA.res], writes=[uu.res])
                        P.op("dve", lambda E, j=j: E.scalar_tensor_tensor(
                            out=mid[:], in0=uu[:], scalar=hv[:, j:j + 1], in1=mid[:], op0=ALU.subtract, op1=ALU.add),
                            reads=[uu.res, hv.res, mid.res], writes=[mid.res])

                def a_mask(i):
                    Sc = (2 * i + 2) * 128
                    P.op("dve", lambda E, Sc=Sc: E.tensor_scalar(
                        out=Mneg[:, 0:Sc], in0=score[:, 0:Sc], scalar1=mid[:, 0:1], scalar2=-BIG,
                        op0=ALU.is_lt, op1=ALU.mult, saturate=False),
                        reads=[score.res, mid.res], writes=[Mneg.res])

                def a_attend(i):
                    nkb = 2 * i + 2
                    osl = slice(i * 128, (i + 1) * 128)
                    P.dma("sp", QTb[:], QT_s[:, :, osl], writes=[QTb.res])
                    for kb in range(nkb):
                        L = psL[kb % 2]
                        Pt = PT[kb % 2]
                        bsl = slice(kb * 128, (kb + 1) * 128)
                        g = kb // 4
                        for half in range(2):
                            P.op("pe", lambda E, L=L, half=half, bsl=bsl: E.matmul(
                                L[:, half * 512:(half + 1) * 512], Mneg[:, bsl], irep[:], start=True, stop=False),
                                reads=[Mneg.res, irep.res], writes=[L.res], inc=False)
                        for hh in range(4):
                            for half in range(2):
                                h = 2 * hh + half
                                pr = slice(half * 64, half * 64 + 64)
                                c0 = half * 512 + hh * 128
                                P.op("pe", lambda E, L=L, h=h, pr=pr, bsl=bsl, hh=hh, c0=c0: E.matmul(
                                    L[:, c0:c0 + 128], KT[pr, h // 2, bsl], QTb[pr, h // 2, :],
                                    start=False, stop=(hh == 3)),
                                    reads=[KTr[g], QTb.res], writes=[L.res], inc=(hh == 3 and half == 1))
                        P.op("act", lambda E, L=L, Pt=Pt: E.activation(out=Pt[:], in_=L[:], func=AF.Exp),
                             reads=[L.res], writes=[Pt.res])
                        for h in range(8):
                            O = psO[h // 4]
                            c0 = (h % 4) * 65
                            pc = (h % 2) * 512 + (h // 2) * 128
                            P.op("pe", lambda E, O=O, c0=c0, Pt=Pt, h=h, kb=kb, nkb=nkb, pc=pc: E.matmul(
                                O[:, c0:c0 + 65], Pt[:, pc:pc + 128], V[:, kb, h, :],
                                start=(kb == 0 and h % 4 == 0), stop=(kb == nkb - 1), skip_group_check=True),
                                reads=[Pt.res, Vr[g]], writes=[O.res], inc=(h == 7))
                    for b2 in range(2):
                        O = psO[b2]
                        P.op("dve", lambda E, O=O, b2=b2: E.reciprocal(
                            out=rz[:, b2 * 4:(b2 + 1) * 4],
                            in_=O[:, 0:260].rearrange("p (h d) -> p h d", d=65)[:, :, 64]),
                            reads=[O.res], writes=[rz.res])
                    for h in range(8):
                        O = psO[h // 4]
                        c0 = (h % 4) * 65
                        P.op("dve", lambda E, O=O, c0=c0, h=h: E.tensor_scalar(
                            out=ytm[:, h * 64:(h + 1) * 64], in0=O[:, c0:c0 + 64], scalar1=rz[:, h:h + 1],
                            scalar2=None, op0=ALU.mult),
                            reads=[O.res, rz.res], writes=[ytm.res])
                    P.dma("sp", yat_s[osl, :], ytm[:], reads=[ytm.res])

                if NA > 0:
                    a_index(0)
                    a_mask(0)
                for i in range(NA):
                    if i + 1 < NA:
                        a_index(i + 1)
                    a_attend(i)
                    if i + 1 < NA:
                        a_mask(i + 1)

        P.barrier()
        comb = _sb(nc, top, "comb", [128, NPAIR, 16], F32)
        if stop not in ("q", "kv", "A1", "A2", "A3", "A") and "ymT_in" not in debug:
            _phase_mlstm(P, nc, Dm, ymT_s)
        if stop not in ("q", "kv", "A1", "A2", "A3", "M", "A"):
          with ExitStack() as ph:
            wga = _sb(nc, ph, "wga", [128, 8, D], BF16)
            wgm = _sb(nc, ph, "wgm", [128, 8, D], BF16)
            wba = _sb(nc, ph, "wba", [128, 4, D], BF16)
            wbm = _sb(nc, ph, "wbm", [128, 8, D], BF16)
            wout = _sb(nc, ph, "wout", [128, 8, D], BF16)
            for t, n in ((wga, "wga"), (wgm, "wgm"), (wba, "wba"), (wbm, "wbm"), (wout, "wout")):
                P.dma("pool", t[:], _wview(Dm[n]), writes=[t.res])
            bga = _sb(nc, ph, "bga", [128, 8], F32)
            bgm = _sb(nc, ph, "bgm", [128, 8], F32)
            g1 = _sb(nc, ph, "g1", [128, D], F32)
            b1 = _sb(nc, ph, "b1", [128, D], F32)
            wr = _sb(nc, ph, "wr", [128, 8, 20], F32)
            brb = _sb(nc, ph, "brb", [128, 20], F32)
            identf = _sb(nc, ph, "identf", [128, 128], F32)
            identb = _sb(nc, ph, "identb", [128, 128], BF16)
            ones4 = _sb(nc, ph, "ones4", [128, 4], F32)
            for t, n in ((bga, "bga"), (bgm, "bgm"), (g1, "ln1g_bc"), (b1, "ln1b_bc"), (brb, "br_bc"), (identf, "ident")):
                P.dma("sp", t[:], Dm[n][:, :], writes=[t.res])
            P.dma("sp", wr[:], _wview(Dm["wr"]), writes=[wr.res])
            P.dma("pool", identb[:], Dm["ident"][:, :], writes=[identb.res])
            P.op("pool", lambda E: E.memset(ones4[:], 1.0), writes=[ones4.res])
            xo = _sb(nc, ph, "c_xo", [128, 8, 512], BF16)
            ymT = _sb(nc, ph, "c_ymT", [128, 8, 512], BF16)
            yaT = _sb(nc, ph, "c_yaT", [128, 4, 512], BF16)
            yab = [_sb(nc, ph, f"c_yab{j}", [128, 512], BF16) for j in range(2)]
            mT = _sb(nc, ph, "c_mT", [128, 8, 512], BF16)
            sa = [_sb(nc, ph, f"c_sa{j}", [128, 512], F32) for j in range(2)]
            sm = [_sb(nc, ph, f"c_sm{j}", [128, 512], F32) for j in range(2)]
            m1 = [_sb(nc, ph, f"c_m1{j}", [128, 512], F32) for j in range(2)]
            m2 = [_sb(nc, ph, f"c_m2{j}", [128, 512], F32) for j in range(2)]
            xb_2 = [_sb(nc, ph, f"c_xb{j}", [128, D], F32) for j in range(2)]
            z_2 = [_sb(nc, ph, f"c_z{j}", [128, D], F32) for j in range(2)]
            x1_2 = [_sb(nc, ph, f"c_x1{j}", [128, D], F32) for j in range(2)]
            junk = _sb(nc, ph, "c_junk", [128, D], F32)
            x1Tf_2 = [_sb(nc, ph, f"c_x1Tf{j}", [128, 8, 128], F32) for j in range(2)]
            x1Tb_2 = [_sb(nc, ph, f"c_x1Tb{j}", [128, 8, 128], BF16) for j in range(2)]
            st_2 = [_sb(nc, ph, f"c_st{j}", [128, 8], F32) for j in range(2)]
            rl_2 = [_sb(nc, ph, f"c_rl{j}", [128, 20], F32) for j in range(2)]
            rs_2 = [_sb(nc, ph, f"c_rs{j}", [128, 16], F32) for j in range(2)]
            gm_2 = [_sb(nc, ph, f"c_gm{j}", [128, 16], F32) for j in range(2)]
            elm_2 = [_sb(nc, ph, f"c_elm{j}", [128, 16], F32) for j in range(2)]
            oh1_2 = [_sb(nc, ph, f"c_oh1{j}", [128, 16], F32) for j in range(2)]
            oh2_2 = [_sb(nc, ph, f"c_oh2{j}", [128, 16], F32) for j in range(2)]
            goh_2 = [_sb(nc, ph, f"c_goh{j}", [128, 4], F32) for j in range(2)]
            gex_2 = [_sb(nc, ph, f"c_gex{j}", [128, 4], F32) for j in range(2)]
            psA = _ps(nc, ph, "c_psA", [128, 512], F32)
            psB = _ps(nc, ph, "c_psB", [128, 512], F32)
            psC = _ps(nc, ph, "c_psC", [128, 512], F32)
            psD = _ps(nc, ph, "c_psD", [128, 512], F32)
            psO = _ps(nc, ph, "c_psO", [128, 512], F32)
            psT = _ps(nc, ph, "c_psT", [128, 1024], BF16)
            psX = _ps(nc, ph, "c_psX", [128, 512], F32)
            psR = _ps(nc, ph, "c_psR", [128, 512], F32)
            xTo = Dm["xT_own"].rearrange("(c p) t -> p c t", p=128)
            BIGM = 1.0e4
            def c1_block(g, blk, xb, z, x1, x1Tf, x1Tb, st, rl, rs, gm, elm, oh1, oh2, goh, gex):
                i = g * 4 + blk
                tsl = slice(blk * 128, (blk + 1) * 128)
                rows = slice(i * 128, (i + 1) * 128)
                P.dma("sp", xb[:], Dm["x_own"][rows, :], writes=[xb.res])
                for half in range(2):
                    hs = slice(half * 512, (half + 1) * 512)
                    for c in range(8):
                        P.op("pe", lambda E, c=c, tsl=tsl, hs=hs: E.matmul(psO[:], mT[:, c, tsl], wout[:, c, hs], start=(c == 0), stop=(c == 7)),
                             reads=[mT.res, wout.res], writes=[psO.res], inc=(c == 7))
                    P.op("dve", lambda E, hs=hs: E.scalar_tensor_tensor(
                        out=z[:, hs], in0=xb[:, hs], scalar=ALPHA, in1=psO[:], op0=ALU.mult, op1=ALU.add),
                        reads=[xb.res, psO.res], writes=[z.res])
                _layer_norm(P, z, x1, junk, g1, b1, st, LN_EPS)
                P.dma("sp", x1_s[rows, :], x1[:], reads=[x1.res])
                if C1SUB == 'c':
                    return
                for rnd in range(2):
                    for c4 in range(4):
                        c = rnd * 4 + c4
                        P.op("pe", lambda E, c=c, c4=c4: E.transpose(
                            out=psX[:, c4 * 128:(c4 + 1) * 128], in_=x1[:, c * 128:(c + 1) * 128], identity=identf[:]),
                            reads=[x1.res, identf.res], writes=[psX.res])
                    P.op("act", lambda E, rnd=rnd: E.activation(
                        out=x1Tf[:, rnd * 4:(rnd + 1) * 4, :], in_=psX[:].rearrange("p (c t) -> p c t", c=4), func=AF.Identity),
                        reads=[psX.res], writes=[x1Tf.res])
                    P.op("dve", lambda E, rnd=rnd: E.tensor_copy(
                        out=x1Tb[:, rnd * 4:(rnd + 1) * 4, :], in_=psX[:].rearrange("p (c t) -> p c t", c=4)),
                        reads=[psX.res], writes=[x1Tb.res])
                P.dma("sp", x1T_s[:, :, rows], x1Tb[:], reads=[x1Tb.res])
                if C1SUB == 'd':
                    return
                for c in range(8):
                    P.op("pe", lambda E, c=c: E.matmul(psR[:, 0:20], x1Tf[:, c, :], wr[:, c, :], start=(c == 0), stop=(c == 7)),
                         reads=[x1Tf.res, wr.res], writes=[psR.res], inc=(c == 7))
                P.op("dve", lambda E: E.tensor_tensor(out=rl[:], in0=psR[:, 0:20], in1=brb[:], op=ALU.add),
                     reads=[psR.res, brb.res], writes=[rl.res])
                P.op("dve", lambda E: E.tensor_reduce(out=rs[:, 0:1], in_=rl[:, 0:4], axis=AX.X, op=ALU.max),
                     reads=[rl.res], writes=[rs.res])
                P.op("dve", lambda E: E.tensor_scalar(out=rs[:, 1:2], in0=rs[:, 0:1], scalar1=-1.0, scalar2=None, op0=ALU.mult),
                     reads=[rs.res], writes=[rs.res])
                P.op("act", lambda E: E.activation(out=gex[:], in_=rl[:, 0:4], func=AF.Exp, bias=rs[:, 1:2], accum_out=rs[:, 2:3]),
                     reads=[rl.res, rs.res], writes=[gex.res, rs.res])
                P.op("dve", lambda E: E.reciprocal(out=rs[:, 3:4], in_=rs[:, 2:3]), reads=[rs.res], writes=[rs.res])
                P.op("dve", lambda E: E.tensor_scalar(out=goh[:], in0=rl[:, 0:4], scalar1=rs[:, 0:1], scalar2=None, op0=ALU.is_ge),
                     reads=[rl.res, rs.res], writes=[goh.res])
                for j in range(4):
                    P.op("dve", lambda E, j=j: E.tensor_scalar(
                        out=gm[:, j * 4:(j + 1) * 4], in0=ones4[:], scalar1=goh[:, j:j + 1], scalar2=None, op0=ALU.mult),
                        reads=[ones4.res, goh.res], writes=[gm.res])
                P.op("dve", lambda E: E.tensor_scalar(out=gm[:], in0=gm[:], scalar1=-1.0, scalar2=BIGM, op0=ALU.add, op1=ALU.mult),
                     reads=[gm.res], writes=[gm.res])
                P.op("dve", lambda E: E.tensor_tensor(out=elm[:], in0=gm[:], in1=rl[:, 4:20], op=ALU.add),
                     reads=[gm.res, rl.res], writes=[elm.res])
                P.op("dve", lambda E: E.tensor_reduce(out=rs[:, 4:5], in_=elm[:], axis=AX.X, op=ALU.max),
                     reads=[elm.res], writes=[rs.res])
                P.op("dve", lambda E: E.tensor_scalar(out=oh1[:], in0=elm[:], scalar1=rs[:, 4:5], scalar2=None, op0=ALU.is_ge),
                     reads=[elm.res, rs.res], writes=[oh1.res])
                P.op("dve", lambda E: E.scalar_tensor_tensor(out=elm[:], in0=oh1[:], scalar=-BIGM, in1=elm[:], op0=ALU.mult, op1=ALU.add),
                     reads=[oh1.res, elm.res], writes=[elm.res])
                P.op("dve", lambda E: E.tensor_reduce(out=rs[:, 5:6], in_=elm[:], axis=AX.X, op=ALU.max),
                     reads=[elm.res], writes=[rs.res])
                P.op("dve", lambda E: E.tensor_scalar(out=oh2[:], in0=elm[:], scalar1=rs[:, 5:6], scalar2=None, op0=ALU.is_ge),
                     reads=[elm.res, rs.res], writes=[oh2.res])
                P.op("dve", lambda E: E.tensor_tensor(out=rs[:, 6:7], in0=rs[:, 5:6], in1=rs[:, 4:5], op=ALU.subtract),
                     reads=[rs.res], writes=[rs.res])
                P.op("act", lambda E: E.activation(out=rs[:, 7:8], in_=rs[:, 6:7], func=AF.Exp),
                     reads=[rs.res], writes=[rs.res])
                P.op("dve", lambda E: E.tensor_scalar(out=rs[:, 8:9], in0=rs[:, 7:8], scalar1=1.0, scalar2=None, op0=ALU.add),
                     reads=[rs.res], writes=[rs.res])
                P.op("dve", lambda E: E.reciprocal(out=rs[:, 8:9], in_=rs[:, 8:9]), reads=[rs.res], writes=[rs.res])
                P.op("dve", lambda E: E.tensor_tensor(out=rs[:, 9:10], in0=rs[:, 7:8], in1=rs[:, 8:9], op=ALU.mult),
                     reads=[rs.res], writes=[rs.res])
                P.op("dve", lambda E: E.tensor_scalar(out=oh1[:], in0=oh1[:], scalar1=rs[:, 8:9], scalar2=None, op0=ALU.mult),
                     reads=[oh1.res, rs.res], writes=[oh1.res])
                P.op("dve", lambda E: E.scalar_tensor_tensor(out=oh2[:], in0=oh2[:], scalar=rs[:, 9:10], in1=oh1[:], op0=ALU.mult, op1=ALU.add),
                     reads=[oh2.res, rs.res, oh1.res], writes=[oh2.res])
                P.op("dve", lambda E, i=i: E.tensor_scalar(out=comb[:, i, :], in0=oh2[:], scalar1=rs[:, 3:4], scalar2=None, op0=ALU.mult),
                     reads=[oh2.res, rs.res], writes=[comb.res])
            for g in range(NBLK // 4):
                sl = slice(g * 512, (g + 1) * 512)
                P.dma("pool", xo[:], xTo[:, :, sl], writes=[xo.res])
                P.dma("sp", ymT[:], ymT_s[:, :, sl], writes=[ymT.res])
                for blk in range(4):
                    Yb = yab[blk % 2]
                    P.dma("sp", Yb[:], yat_s[g * 512 + blk * 128:g * 512 + (blk + 1) * 128, :], writes=[Yb.res])
                    for c in range(4):
                        P.op("pe", lambda E, Yb=Yb, c=c: E.transpose(
                            out=psT[:, c * 128:(c + 1) * 128], in_=Yb[:, c * 128:(c + 1) * 128], identity=identb[:]),
                            reads=[Yb.res, identb.res], writes=[psT.res])
                    P.op("act", lambda E, blk=blk: E.activation(
                        out=yaT[:, :, blk * 128:(blk + 1) * 128], in_=psT[:, 0:512].rearrange("p (c t) -> p c t", c=4),
                        func=AF.Identity),
                        reads=[psT.res], writes=[yaT.res])
                if C1SUB == 'a':
                    continue
                for mc in range(8):
                    msl = slice(mc * 128, (mc + 1) * 128)
                    SA, SM, M1, M2 = sa[mc % 2], sm[mc % 2], m1[mc % 2], m2[mc % 2]
                    for c in range(4):
                        P.op("pe", lambda E, c=c, msl=msl: E.matmul(psA[:], wba[:, c, msl], yaT[:, c, :], start=(c == 0), stop=(c == 3)),
                             reads=[wba.res, yaT.res], writes=[psA.res], inc=(c == 3))
                    for c in range(8):
                        P.op("pe", lambda E, c=c, msl=msl: E.matmul(psB[:], wbm[:, c, msl], ymT[:, c, :], start=(c == 0), stop=(c == 7)),
                             reads=[wbm.res, ymT.res], writes=[psB.res], inc=(c == 7))
                    for c in range(8):
                        P.op("pe", lambda E, c=c, msl=msl: E.matmul(psC[:], wga[:, c, msl], xo[:, c, :], start=(c == 0), stop=(c == 7)),
                             reads=[wga.res, xo.res], writes=[psC.res], inc=(c == 7))
                    for c in range(8):
                        P.op("pe", lambda E, c=c, msl=msl: E.matmul(psD[:], wgm[:, c, msl], xo[:, c, :], start=(c == 0), stop=(c == 7)),
                             reads=[wgm.res, xo.res], writes=[psD.res], inc=(c == 7))
                    P.op("act", lambda E, SA=SA, mc=mc: E.activation(out=SA[:], in_=psC[:], func=AF.Sigmoid, bias=bga[:, mc:mc + 1]),
                         reads=[psC.res, bga.res], writes=[SA.res])
                    P.op("act", lambda E, SM=SM, mc=mc: E.activation(out=SM[:], in_=psD[:], func=AF.Sigmoid, bias=bgm[:, mc:mc + 1]),
                         reads=[psD.res, bgm.res], writes=[SM.res])
                    P.op("dve", lambda E, M1=M1, SA=SA: E.tensor_tensor(out=M1[:], in0=psA[:], in1=SA[:], op=ALU.mult),
                         reads=[psA.res, SA.res], writes=[M1.res])
                    P.op("dve", lambda E, M2=M2, SM=SM: E.tensor_tensor(out=M2[:], in0=psB[:], in1=SM[:], op=ALU.mult),
                         reads=[psB.res, SM.res], writes=[M2.res])
                    P.op("pool", lambda E, M1=M1, M2=M2, mc=mc: E.tensor_tensor(out=mT[:, mc, :], in0=M1[:], in1=M2[:], op=ALU.add),
                         reads=[M1.res, M2.res], writes=[mT.res])
                if C1SUB == 'b':
                    continue
                for blk in range(4):
                    j2 = (g * 4 + blk) % 2
                    c1_block(g, blk, xb_2[j2], z_2[j2], x1_2[j2], x1Tf_2[j2], x1Tb_2[j2], st_2[j2], rl_2[j2], rs_2[j2], gm_2[j2], elm_2[j2], oh1_2[j2], oh2_2[j2], goh_2[j2], gex_2[j2])
          P.barrier()

        if stop not in ("q", "kv", "A1", "A2", "A3", "C1", "M", "A"):
          with ExitStack() as ph:
            NBH = min(NBLK, 16)
            NH = NBH * 128
            acc = _sb(nc, ph, "e_acc", [128, NBH, D], F32)
            x1T = _sb(nc, ph, "e_x1T", [128, 8, NH], BF16)
            wg = [_sb(nc, ph, f"e_wg{j}", [128, 8, 512], BF16) for j in range(2)]
            wu = [_sb(nc, ph, f"e_wu{j}", [128, 8, 512], BF16) for j in range(2)]
            wd = [_sb(nc, ph, f"e_wd{j}", [128, 4, D], BF16) for j in range(2)]
            sg = [_sb(nc, ph, f"e_sg{j}", [128, 512], F32) for j in range(2)]
            hdn = [_sb(nc, ph, f"e_hdn{j}", [128, 4, 512], BF16) for j in range(2)]
            g2 = _sb(nc, ph, "e_g2", [128, D], F32)
            b2 = _sb(nc, ph, "e_b2", [128, D], F32)
            junk = _sb(nc, ph, "e_junk", [128, D], F32)
            ot = [_sb(nc, ph, f"e_ot{j}", [128, D], F32) for j in range(2)]
            zt = _sb(nc, ph, "e_zt", [128, D], F32)
            st = _sb(nc, ph, "e_st", [128, 8], F32)
            P.dma("sp", g2[:], Dm["ln2g_bc"][:, :], writes=[g2.res])
            P.dma("sp", b2[:], Dm["ln2b_bc"][:, :], writes=[b2.res])
            psG = [_ps(nc, ph, f"e_psG{j}", [128, 512], F32) for j in range(2)]
            psU = [_ps(nc, ph, f"e_psU{j}", [128, 512], F32) for j in range(2)]
            psDn = [_ps(nc, ph, f"e_psDn{j}", [128, 512], F32) for j in range(4)]
            accr = [Res(f"acc{b}") for b in range(NBH)]
            for hf in range(2 if NBLK == 32 else 1):
                tok = slice(hf * NH, (hf + 1) * NH)
                P.dma("sp", x1T[:], x1T_s[:, :, tok], writes=[x1T.res])
                for b in range(NBH):
                    rows = slice(hf * NH + b * 128, hf * NH + (b + 1) * 128)
                    P.dma("sp", acc[:, b, :], x1_s[rows, :], writes=[accr[b]])
                    P.op("act", lambda E, b=b: E.activation(out=acc[:, b, :], in_=acc[:, b, :], func=AF.Identity, scale=ALPHA),
                         reads=[accr[b]], writes=[accr[b]])
                for e in range(16):
                    WG, WU, WD = wg[e % 2], wu[e % 2], wd[e % 2]
                    P.dma("pool", WG[:], _wview(Dm["weg"][e]), writes=[WG.res])
                    P.dma("pool", WU[:], _wview(Dm["weu"][e]), writes=[WU.res])
                    P.dma("pool", WD[:], _wview(Dm["wed"][e]), writes=[WD.res])
                    for tg in range(NH // 512):
                        tsl = slice(tg * 512, (tg + 1) * 512)
                        H = hdn[tg % 2]
                        for fc in range(4):
                            fsl = slice(fc * 128, (fc + 1) * 128)
                            G, U, SG = psG[fc % 2], psU[fc % 2], sg[fc % 2]
                            for k in range(8):
                                P.op("pe", lambda E, G=G, WG=WG, k=k, fsl=fsl, tsl=tsl: E.matmul(
                                    G[:], WG[:, k, fsl], x1T[:, k, tsl], start=(k == 0), stop=(k == 7)),
                                    reads=[WG.res, x1T.res], writes=[G.res], inc=(k == 7))
                            for k in range(8):
                                P.op("pe", lambda E, U=U, WU=WU, k=k, fsl=fsl, tsl=tsl: E.matmul(
                                    U[:], WU[:, k, fsl], x1T[:, k, tsl], start=(k == 0), stop=(k == 7)),
                                    reads=[WU.res, x1T.res], writes=[U.res], inc=(k == 7))
                            P.op("act", lambda E, SG=SG, G=G: E.activation(out=SG[:], in_=G[:], func=AF.Silu),
                                 reads=[G.res], writes=[SG.res])
                            P.op("dve", lambda E, H=H, fc=fc, SG=SG, U=U: E.tensor_tensor(out=H[:, fc, :], in0=U[:], in1=SG[:], op=ALU.mult),
                                 reads=[U.res, SG.res], writes=[H.res])
                        for blk in range(4):
                            b = tg * 4 + blk
                            ib = hf * NBH + b
                            bsl = slice(blk * 128, (blk + 1) * 128)
                            for half in range(2):
                                hs = slice(half * 512, (half + 1) * 512)
                                Dp = psDn[(blk * 2 + half) % 4]
                                for fc in range(4):
                                    P.op("pe", lambda E, Dp=Dp, H=H, fc=fc, bsl=bsl, WD=WD, hs=hs: E.matmul(
                                        Dp[:], H[:, fc, bsl], WD[:, fc, hs], start=(fc == 0), stop=(fc == 3)),
                                        reads=[H.res, WD.res], writes=[Dp.res], inc=(fc == 3))
                                P.op("dve", lambda E, Dp=Dp, b=b, hs=hs, ib=ib, e=e: E.scalar_tensor_tensor(
                                    out=acc[:, b, hs], in0=Dp[:], scalar=comb[:, ib, e:e + 1], in1=acc[:, b, hs],
                                    op0=ALU.mult, op1=ALU.add),
                                    reads=[Dp.res, comb.res, accr[b]], writes=[accr[b]])
                for b in range(NBH):
                    rows = slice(hf * NH + b * 128, hf * NH + (b + 1) * 128)
                    O = ot[b % 2]
                    zb = Tl(acc.t[:, b, :], f"accv{b}")
                    zb.res = accr[b]
                    _layer_norm(P, zb, O, junk, g2, b2, st, LN_EPS)
                    P.dma("sp", out[rows, :], O[:], reads=[O.res])
          P.barrier()
        P.finish()
        nc._prog = P
        with nc.Block() as block:
            P.emit(block)
    return nc


IN_SPLITS = (512, 512, 512, 256, 64, 4, 512, 512, 1024, 8, 8, 1024, 1024, 1024)
OFF = np.concatenate([[0], np.cumsum(IN_SPLITS)]).astype(int)


def _swap_perm(nheads):
    idx = []
    for h in range(nheads):
        idx += list(range(h * 64 + 32, h * 64 + 64)) + list(range(h * 64, h * 64 + 32))
    return np.array(idx)


def _fm_bias(b):
    return np.ascontiguousarray(b.reshape(-1, 128).T)


def _rope_tabs(pos, scale=1.0):
    inv = (np.float32(10000.0) ** (-np.arange(0, 64, 2, dtype=np.float32) / np.float32(64))).astype(np.float32)
    ang = pos.astype(np.float32)[None, :] * inv[:, None]
    c = np.cos(ang).astype(np.float32)
    s = np.sin(ang).astype(np.float32)
    cos = np.concatenate([c, c, c, c], 0)
    sin = np.concatenate([-s, s, -s, s], 0)
    return np.ascontiguousarray(cos * np.float32(scale)), np.ascontiguousarray(sin * np.float32(scale))


def _shared_inputs(w_in, b_in, inputs):
    sh = {}
    f = lambda k: np.asarray(inputs[k], np.float32)
    bc = lambda v, n: np.broadcast_to(v[None, :], (128, n))
    sh["wba"], sh["wbm"], sh["wout"] = f("w_branch_attn"), f("w_branch_mlstm"), f("w_out")
    sh["ln1g_bc"], sh["ln1b_bc"] = bc(f("ln1_gain"), D), bc(f("ln1_bias"), D)
    sh["ln2g_bc"], sh["ln2b_bc"] = bc(f("ln2_gain"), D), bc(f("ln2_bias"), D)
    sh["wr"] = np.concatenate([f("w_router_group"), f("w_router_expert")], 1)
    sh["br_bc"] = bc(np.concatenate([f("b_router_group"), f("b_router_expert")]), 20)
    sh["weg"], sh["weu"], sh["wed"] = f("w_exp_gate"), f("w_exp_up"), f("w_exp_down")
    sw8, sw4, sw1 = _swap_perm(8), _swap_perm(4), _swap_perm(1)
    aq, ak, av = w_in[:, OFF[0]:OFF[1]], w_in[:, OFF[1]:OFF[2]], w_in[:, OFF[2]:OFF[3]]
    iq, ik, iw = w_in[:, OFF[3]:OFF[4]], w_in[:, OFF[4]:OFF[5]], w_in[:, OFF[5]:OFF[6]]
    baq, bak, bav = b_in[OFF[0]:OFF[1]], b_in[OFF[1]:OFF[2]], b_in[OFF[2]:OFF[3]]
    biq, bik, biw = b_in[OFF[3]:OFF[4]], b_in[OFF[4]:OFF[5]], b_in[OFF[5]:OFF[6]]
    sh["wq"], sh["wq_s"] = aq, aq[:, sw8]
    sh["wk"], sh["wk_s"] = ak, ak[:, sw8]
    sh["wv"] = av
    sh["wiq"], sh["wiq_s"] = iq, iq[:, sw4]
    sh["wik2"] = np.concatenate([ik, ik], 1)
    sh["wik2_s"] = np.concatenate([ik[:, sw1], ik[:, sw1]], 1)
    sh["wiw"] = iw
    sh["bq"], sh["bq_s"] = _fm_bias(baq), _fm_bias(baq[sw8])
    sh["bk"], sh["bk_s"] = _fm_bias(bak), _fm_bias(bak[sw8])
    sh["biq"], sh["biq_s"] = _fm_bias(biq), _fm_bias(biq[sw4])
    sh["bik2"] = _fm_bias(np.concatenate([bik, bik]))
    sh["bik2_s"] = _fm_bias(np.concatenate([bik[sw1], bik[sw1]]))
    sh["bv_bc"] = np.broadcast_to(bav[None, :], (128, 512))
    sh["biw_bc"] = np.broadcast_to(biw[None, :], (128, 4))
    sh["wga"], sh["wgm"] = w_in[:, OFF[12]:OFF[13]], w_in[:, OFF[13]:OFF[14]]
    sh["bga"], sh["bgm"] = _fm_bias(b_in[OFF[12]:OFF[13]]), _fm_bias(b_in[OFF[13]:OFF[14]])
    sh["wmq"], sh["wmk"] = w_in[:, OFF[6]:OFF[7]], w_in[:, OFF[7]:OFF[8]]
    sh["bmq"], sh["bmk"] = _fm_bias(b_in[OFF[6]:OFF[7]]), _fm_bias(b_in[OFF[7]:OFF[8]])
    cm = f("conv_m")
    sh["cwq"] = cm[:, 0:512].T.reshape(4, 128, 4).transpose(1, 0, 2).reshape(128, 16)
    sh["cwk"] = cm[:, 512:1024].T.reshape(4, 128, 4).transpose(1, 0, 2).reshape(128, 16)
    sh["wmv"], sh["bmv_bc"] = w_in[:, OFF[8]:OFF[9]], bc(b_in[OFF[8]:OFF[9]], D)
    sh["wmif"], sh["bif_bc"] = w_in[:, OFF[9]:OFF[11]], bc(b_in[OFF[9]:OFF[11]], 16)
    sh["wmo"], sh["bmo_bc"] = w_in[:, OFF[11]:OFF[12]], bc(b_in[OFF[11]:OFF[12]], D)
    sh["gn_bc"] = bc(f("gn_m_gain"), D)
    qq = np.arange(128)
    sh["tri"] = (qq[:, None] <= qq[None, :]).astype(np.float32)
    sh["ones"] = np.ones((128, 128), np.float32)
    sh["pow2"] = np.broadcast_to((2.0 ** -(np.arange(32) + 1.0)).astype(np.float32)[None, :], (128, 32))
    cmask = np.where(qq[None, :] <= qq[:, None], 0.0, NEG).astype(np.float32)
    sh["cmask_rep"] = np.tile(cmask, (1, 8))
    sh["cmaskT_rep"] = np.tile(cmask.T, (1, 8))
    eye = np.eye(128, dtype=np.float32)
    sh["ident"] = eye
    sh["irep"] = np.concatenate([eye] * 4, 1)
    q = np.arange(128)
    sh["negm_diag"] = np.where(q[None, :] <= q[:, None], 0.0, NEG).astype(np.float32)
    return {k: np.ascontiguousarray(v, dtype=np.float32) for k, v in sh.items()}


def _core_inputs(x, c):
    b, p = c // 2, c % 2
    xb = x[b]
    if p == 1:
        xprog = xb
        pos = np.arange(S)
    else:
        xprog = np.concatenate([np.zeros((128, D), np.float32), xb[:S - 128]], 0)
        pos = np.concatenate([np.zeros(128), np.arange(S - 128)])
    own = xprog.reshape(64, 128, D)[1::2].reshape(NOWN, D)
    pos_own = pos.reshape(64, 128)[1::2].reshape(NOWN)
    m = {"xT": np.ascontiguousarray(xprog.T), "xT_own": np.ascontiguousarray(own.T),
         "x_own": np.ascontiguousarray(own)}
    m["cosk"], m["sink"] = _rope_tabs(pos)
    m["cosq"], m["sinq"] = _rope_tabs(pos_own, 0.125)
    m["cosi"], m["sini"] = _rope_tabs(pos_own)
    m["negm_b0"] = np.full((128, 128), NEG if p == 0 else 0.0, np.float32)
    m["vflag"] = np.full((128, 1), 0.0 if p == 0 else 1.0, np.float32)
    m["ineg"] = np.full((128, 1), NEG if p == 0 else 0.0, np.float32)
    return m


def make_in_maps(inputs):
    x = np.asarray(inputs["x"], np.float32)
    sh = _shared_inputs(np.asarray(inputs["w_in"], np.float32), np.asarray(inputs["b_in"], np.float32), inputs)
    maps = []
    for c in range(8):
        m = dict(sh)
        m.update(_core_inputs(x, c))
        maps.append({k: m[k] for k in IN_SPECS})
    return maps


def kernel(**inputs):
    nc = build_nc()
    in_maps = make_in_maps(inputs)
    res = run_bass_kernel_spmd(nc, in_maps, core_ids=list(range(8)))
    B = 4
    out = np.zeros((B, S, D), np.float32)
    for c in range(8):
        b, p = c // 2, c % 2
        o = np.asarray(res.results[c]["out"]).reshape(NPAIR, 128, D)
        out[b].reshape(64, 128, D)[p::2] = o
    return out
```

```python
from contextlib import ExitStack

import numpy as np
import concourse.bass as bass
import concourse.mybir as mybir
from concourse.bass_utils import run_bass_kernel_spmd

F32 = mybir.dt.float32
BF16 = mybir.dt.bfloat16
U32 = mybir.dt.uint32
U8 = mybir.dt.uint8
F8 = mybir.dt.float8e5
AF = mybir.ActivationFunctionType
ALU = mybir.AluOpType
AX = mybir.AxisListType

D = 1024
S = 8192
NPAIR = 32
NOWN = 4096
BIG = 30000.0
NEG = -1.0e30
import os
NIT = int(os.environ.get('K_NIT', '13'))
TOPK = 256
NBLK = int(os.environ.get('K_NBLK', '32'))

EPOCH = 16000
NEPOCH = 8
NSLOT = 8
ENGS = ("pe", "act", "dve", "pool", "sp")
DMAQ = ("sp", "pool")


class Res:
    __slots__ = ("name", "w", "r", "psum")

    def __init__(self, name="", psum=False):
        self.name = name
        self.w = None
        self.r = {}
        self.psum = psum


class Tl:
    def __init__(self, t, name, psum=False):
        self.t = t
        self.res = Res(name, psum)

    def __getitem__(self, k):
        return self.t[k]


class Prog:
    def __init__(self, nc, es):
        self.nc = nc
        self.q = {e: [] for e in ENGS}
        self.tr = {e: [] for e in ENGS}
        self.cnt = {e: 0 for e in ENGS}
        self.dcnt = {q: 0 for q in DMAQ}
        self.known = {e: {} for e in ENGS}
        self.esem = {e: [es.enter_context(nc.semaphore(f"s_{e}{k}")) for k in range(NEPOCH)] for e in ENGS}
        self.dsem = {q: [es.enter_context(nc.semaphore(f"d_{q}{k}")) for k in range(NSLOT)] for q in DMAQ}

    def _wait(self, eng, sp):
        if sp[0] == "E":
            _, e2, seq = sp
            if e2 == eng and eng == "pe":
                return
            key = ("E", e2)
            if self.known[eng].get(key, 0) >= seq:
                return
            self.known[eng][key] = seq
            sem = self.esem[e2][(seq - 1) // EPOCH]
            val = (seq - 1) % EPOCH + 1
        else:
            _, q, idx = sp
            slot = idx % NSLOT
            need = idx // NSLOT + 1
            key = ("D", q, slot)
            if self.known[eng].get(key, 0) >= need:
                return
            self.known[eng][key] = need
            sem = self.dsem[q][slot]
            val = 16 * need
        self.q[eng].append(lambda E, sem=sem, val=val: E.wait_ge(sem, val))
        self.tr[eng].append(('wait', id(sem), val))

    def _deps(self, eng, reads, writes):
        for r in reads:
            if r.w is not None:
                self._wait(eng, r.w)
            if r.psum:
                for key, v in r.r.items():
                    if key[0] == "E" and key[1] != eng:
                        self._wait(eng, ("E", key[1], v))
        for w in writes:
            if w.w is not None:
                self._wait(eng, w.w)
            for key, v in w.r.items():
                self._wait(eng, ("E", key[1], v) if key[0] == "E" else ("D", key[1], v))

    def _mark(self, me, reads, writes):
        key = ("E", me[1]) if me[0] == "E" else ("D", me[1], me[2] % NSLOT)
        for r in reads:
            if r.r.get(key, -1) < me[2]:
                r.r[key] = me[2]
        for w in writes:
            w.w = me
            w.r = {}

    def op(self, eng, fn, reads=(), writes=(), inc=True):
        self._deps(eng, reads, writes)
        if not inc:
            assert eng == "pe"
            self.q[eng].append(lambda E, fn=fn: fn(E))
            self._mark(("E", eng, self.cnt[eng] + 1), reads, writes)
            return
        self.cnt[eng] += 1
        seq = self.cnt[eng]
        assert seq <= EPOCH * NEPOCH, f"too many instructions on {eng}"
        sem = self.esem[eng][(seq - 1) // EPOCH]
        self.q[eng].append(lambda E, fn=fn, sem=sem: fn(E).then_inc(sem, 1))
        self.tr[eng].append(('inc', id(sem), 1))
        self._mark(("E", eng, seq), reads, writes)

    def dma(self, q, out, in_, reads=(), writes=()):
        self._deps(q, reads, writes)
        idx = self.dcnt[q]
        self.dcnt[q] += 1
        if idx >= NSLOT:
            self._wait(q, ("D", q, idx - NSLOT))
        sem = self.dsem[q][idx % NSLOT]
        self.q[q].append(lambda E, out=out, in_=in_, sem=sem: E.dma_start(out=out, in_=in_).then_inc(sem, 16))
        self.tr[q].append(('inc', id(sem), 16))
        self._mark(("D", q, idx), reads, writes)

    def barrier(self):
        for e in ENGS:
            for e2 in ENGS:
                if self.cnt[e2] > 0:
                    self._wait(e, ("E", e2, self.cnt[e2]))
            for q in DMAQ:
                n = self.dcnt[q]
                for idx in range(max(0, n - NSLOT), n):
                    self._wait(e, ("D", q, idx))

    def finish(self):
        for q in DMAQ:
            n = self.dcnt[q]
            for idx in range(max(0, n - NSLOT), n):
                self._wait("sp", ("D", q, idx))
        for e in ENGS:
            if e != "sp" and self.cnt[e] > 0:
                self._wait("sp", ("E", e, self.cnt[e]))

    def emit(self, block):
        def mk(name):
            def f(E):
                for c in self.q[name]:
                    c(E)
            return f
        block.tensor(mk("pe"))
        block.scalar(mk("act"))
        block.vector(mk("dve"))
        block.gpsimd(mk("pool"))
        block.sync(mk("sp"))


def _sb(nc, ph, name, shape, dtype):
    return Tl(ph.enter_context(nc.sbuf_tensor("sb_" + name, list(shape), dtype)), name)


def _ps(nc, ph, name, shape, dtype):
    return Tl(ph.enter_context(nc.psum_tensor("pp_" + name, list(shape), dtype)), name, psum=True)


def _layer_norm(P, z, outt, junk, g_bc, b_bc, st, eps):
    n = float(D)
    P.op("act", lambda E: E.activation(out=junk[:], in_=z[:], func=AF.Identity, accum_out=st[:, 0:1]),
         reads=[z.res], writes=[junk.res, st.res])
    P.op("act", lambda E: E.activation(out=junk[:], in_=z[:], func=AF.Square, accum_out=st[:, 1:2]),
         reads=[z.res], writes=[junk.res, st.res])
    P.op("dve", lambda E: E.tensor_scalar(out=st[:, 2:3], in0=st[:, 0:1], scalar1=-1.0 / n, scalar2=None, op0=ALU.mult),
         reads=[st.res], writes=[st.res])
    P.op("dve", lambda E: E.tensor_tensor(out=st[:, 3:4], in0=st[:, 2:3], in1=st[:, 2:3], op=ALU.mult),
         reads=[st.res], writes=[st.res])
    P.op("dve", lambda E: E.scalar_tensor_tensor(out=st[:, 4:5], in0=st[:, 1:2], scalar=1.0 / n, in1=st[:, 3:4],
                                                  op0=ALU.mult, op1=ALU.subtract),
         reads=[st.res], writes=[st.res])
    P.op("dve", lambda E: E.tensor_scalar(out=st[:, 4:5], in0=st[:, 4:5], scalar1=eps, scalar2=None, op0=ALU.add),
         reads=[st.res], writes=[st.res])
    P.op("act", lambda E: E.activation(out=st[:, 5:6], in_=st[:, 4:5], func=AF.Sqrt),
         reads=[st.res], writes=[st.res])
    P.op("dve", lambda E: E.reciprocal(out=st[:, 6:7], in_=st[:, 5:6]), reads=[st.res], writes=[st.res])
    P.op("dve", lambda E: E.tensor_scalar(out=outt[:], in0=z[:], scalar1=st[:, 2:3], scalar2=st[:, 6:7],
                                          op0=ALU.add, op1=ALU.mult),
         reads=[z.res, st.res], writes=[outt.res])
    P.op("dve", lambda E: E.tensor_tensor(out=outt[:], in0=outt[:], in1=g_bc[:], op=ALU.mult),
         reads=[outt.res, g_bc.res], writes=[outt.res])
    P.op("dve", lambda E: E.tensor_tensor(out=outt[:], in0=outt[:], in1=b_bc[:], op=ALU.add),
         reads=[outt.res, b_bc.res], writes=[outt.res])


def _wview(ap, p=128):
    return ap.rearrange("(c p) n -> p c n", p=p)


def _phase_mlstm(P, nc, Dm, ymT_s):
    with ExitStack() as ph:
        sb = lambda n, shp, dt: _sb(nc, ph, "m_" + n, shp, dt)
        wmq, wmk = sb("wmq", [128, 8, 512], BF16), sb("wmk", [128, 8, 512], BF16)
        wmv, wmo = sb("wmv", [128, 8, D], BF16), sb("wmo", [128, 8, D], BF16)
        wmif = sb("wmif", [128, 8, 16], BF16)
        for t, n in ((wmq, "wmq"), (wmk, "wmk"), (wmv, "wmv"), (wmo, "wmo"), (wmif, "wmif")):
            P.dma("pool", t[:], _wview(Dm[n]), writes=[t.res])
        bmq, bmk = sb("bmq", [128, 4], F32), sb("bmk", [128, 4], F32)
        cwq, cwk = sb("cwq", [128, 16], F32), sb("cwk", [128, 16], F32)
        bmv, bmo, gnb = sb("bmv", [128, D], F32), sb("bmo", [128, D], F32), sb("gnb", [128, D], F32)
        bif = sb("bif", [128, 16], F32)
        tri, ones, identf = sb("tri", [128, 128], F32), sb("ones", [128, 128], F32), sb("identf", [128, 128], F32)
        identb = sb("identb", [128, 128], BF16)
        cmk, cmkT = sb("cmk", [128, 1024], F32), sb("cmkT", [128, 1024], F32)
        vflag, ineg = sb("vflag", [128, 1], F32), sb("ineg", [128, 1], F32)
        for t, n in ((bmq, "bmq"), (bmk, "bmk"), (cwq, "cwq"), (cwk, "cwk"), (bmv, "bmv_bc"), (bmo, "bmo_bc"),
                     (gnb, "gn_bc"), (bif, "bif_bc"), (tri, "tri"), (ones, "ones"), (identf, "ident"),
                     (cmk, "cmask_rep"), (cmkT, "cmaskT_rep"), (vflag, "vflag"), (ineg, "ineg")):
            P.dma("sp", t[:], Dm[n][:, :], writes=[t.res])
        P.dma("pool", identb[:], Dm["ident"][:, :], writes=[identb.res])
        xgs = [sb(f"xg{j}", [128, 8, 512], BF16) for j in range(2)]
        cb = [sb(f"cb{j}", [128, 515], F32) for j in range(8)]
        ctmp = [sb(f"ctmp{j}", [128, 512], F32) for j in range(2)]
        mqT, mkT = sb("mqT", [128, 4, 512], BF16), sb("mkT", [128, 4, 512], BF16)
        vaug = [sb(f"vaug{j}", [128, 8, 129], BF16) for j in range(4)]
        gt = sb("gt", [128, 16], F32)
        ef = sb("ef", [128, 8], F32)
        lf = sb("lf", [128, 8], F32)
        bt = sb("bt", [128, 16], F32)
        av = sb("av", [128, 8], F32)
        col = sb("col", [128, 12, 8], F32)
        diag = sb("diag", [128, 8, 128], F32)
        Am = sb("Am", [128, 8, 128], F32)
        DT = sb("DT", [128, 8, 128], F32)
        PT = sb("PT", [128, 1024], BF16)
        og = sb("og", [128, D], F32)
        n1s = sb("n1s", [128, 129], F32)
        nums = sb("nums", [128, 129], F32)
        sc1 = sb("sc1", [128, 4], F32)
        hbuf = sb("hbuf", [128, 8, 128], F32)
        sqt = sb("sqt", [128, 8, 128], F32)
        ybf = sb("ybf", [128, D], BF16)
        yT = sb("yT", [128, 8, 128], BF16)
        wk = sb("wk", [128, 8, 64], BF16)
        Cst = sb("Cst", [128, 4, 129], F32)
        Cbf = sb("Cbf", [128, 4, 129], BF16)
        mprev = sb("mprev", [128, 8], F32)
        pj = [_ps(nc, ph, f"m_pj{j}", [128, 512], F32) for j in range(2)]
        pbc = _ps(nc, ph, "m_pbc", [128, 1024], F32)
        psS = _ps(nc, ph, "m_psS", [128, 1024], F32)
        pT = _ps(nc, ph, "m_pT", [128, 1024], BF16)
        CMAX, INTER, MT, RR, WINT, EMT, AMX, MNB, MNEW, CD, ES, TMP = range(12)
        diag_r = [Res(f"diag{h}") for h in range(8)]
        DT_r = [Res(f"DT{h}") for h in range(8)]
        hbuf_r = [Res(f"hbuf{h}") for h in range(8)]
        wk_r = [Res(f"wk{h}") for h in range(8)]
        Cst_r = [Res(f"Cst{h}") for h in range(8)]
        for j in range(8):
            P.op("pool", lambda E, j=j: E.memset(cb[j][:, 0:3], 0.0), writes=[cb[j].res])
        for j in range(4):
            P.op("pool", lambda E, j=j: E.memset(vaug[j][:, :, 128:129], 1.0), writes=[vaug[j].res])
        P.op("pool", lambda E: E.memset(Cst[:], 0.0), writes=Cst_r)
        P.op("pool", lambda E: E.memset(Cbf[:], 0.0), writes=[Cbf.res])
        P.op("pool", lambda E: E.memset(mprev[:], 0.0), writes=[mprev.res])
        xTa = Dm["xT"].rearrange("(c p) t -> p c t", p=128)
        hcol = lambda h: (h % 2) * 4 + h // 2

        def c8(idx):
            return col[:, idx, :]

        def m_group(g, xg):
            sl = slice(g * 512, (g + 1) * 512)
            P.dma("pool", xg[:], xTa[:, :, sl], writes=[xg.res])
            for c in range(8):
                isq = c < 4
                W, Bt, CW, OUT = (wmq, bmq, cwq, mqT) if isq else (wmk, bmk, cwk, mkT)
                cc = c % 4
                A = pj[c % 2]
                CB = cb[c]
                T = ctmp[c % 2]
                for k in range(8):
                    P.op("pe", lambda E, A=A, W=W, k=k, cc=cc: E.matmul(
                        A[:], W[:, k, cc * 128:(cc + 1) * 128], xg[:, k, :], start=(k == 0), stop=(k == 7)),
                        reads=[W.res, xg.res], writes=[A.res], inc=(k == 7))
                P.op("dve", lambda E, CB=CB, A=A, Bt=Bt, cc=cc: E.tensor_scalar(
                    out=CB[:, 3:515], in0=A[:], scalar1=Bt[:, cc:cc + 1], scalar2=None, op0=ALU.add),
                    reads=[A.res, Bt.res], writes=[CB.res])
                if g == 0:
                    P.op("dve", lambda E, CB=CB: E.tensor_scalar(
                        out=CB[:, 3:131], in0=CB[:, 3:131], scalar1=vflag[:, 0:1], scalar2=None, op0=ALU.mult),
                        reads=[CB.res, vflag.res], writes=[CB.res])
                P.op("dve", lambda E, T=T, CB=CB, CW=CW, cc=cc: E.tensor_scalar(
                    out=T[:], in0=CB[:, 0:512], scalar1=CW[:, cc * 4:cc * 4 + 1], scalar2=None, op0=ALU.mult),
                    reads=[CB.res, CW.res], writes=[T.res])
                for j in range(1, 4):
                    P.op("dve", lambda E, T=T, CB=CB, CW=CW, cc=cc, j=j: E.scalar_tensor_tensor(
                        out=T[:], in0=CB[:, j:j + 512], scalar=CW[:, cc * 4 + j:cc * 4 + j + 1], in1=T[:],
                        op0=ALU.mult, op1=ALU.add),
                        reads=[CB.res, CW.res, T.res], writes=[T.res])
                P.op("pool", lambda E, CB=CB: E.tensor_copy(out=CB[:, 0:3], in_=CB[:, 512:515]),
                     reads=[CB.res], writes=[CB.res])
                if isq:
                    P.op("act", lambda E, T=T, cc=cc: E.activation(out=mqT[:, cc, :], in_=T[:], func=AF.Silu),
                         reads=[T.res], writes=[mqT.res])
                else:
                    P.op("act", lambda E, T=T, cc=cc: E.activation(out=mkT[:, cc, :], in_=T[:], func=AF.Silu),
                         reads=[T.res], writes=[mkT.res])
            for blk in range(4):
                bsl = slice(blk * 128, (blk + 1) * 128)
                VA = vaug[blk]
                for half in range(2):
                    A = pj[half]
                    for k in range(8):
                        P.op("pe", lambda E, A=A, k=k, bsl=bsl, half=half: E.matmul(
                            A[:], xg[:, k, bsl], wmv[:, k, half * 512:(half + 1) * 512], start=(k == 0), stop=(k == 7)),
                            reads=[xg.res, wmv.res], writes=[A.res], inc=(k == 7))
                    P.op("dve", lambda E, A=A, VA=VA, half=half: E.tensor_tensor(
                        out=VA[:, half * 4:(half + 1) * 4, 0:128], in0=A[:].rearrange("p (h d) -> p h d", h=4),
                        in1=bmv[:, half * 512:(half + 1) * 512].rearrange("p (h d) -> p h d", h=4), op=ALU.add),
                        reads=[A.res, bmv.res], writes=[VA.res])
            for blk in range(4):
                pb = g * 4 + blk
                own = (pb % 2 == 1)
                bsl = slice(blk * 128, (blk + 1) * 128)
                VA = vaug[blk]
                for k in range(8):
                    P.op("pe", lambda E, k=k, bsl=bsl: E.matmul(
                        pj[0][:, 0:16], xg[:, k, bsl], wmif[:, k, :], start=(k == 0), stop=(k == 7)),
                        reads=[xg.res, wmif.res], writes=[pj[0].res])
                P.op("dve", lambda E: E.tensor_tensor(out=gt[:], in0=pj[0][:, 0:16], in1=bif[:], op=ALU.add),
                     reads=[pj[0].res, bif.res], writes=[gt.res])
                P.op("act", lambda E: E.activation(out=ef[:], in_=gt[:, 8:16], func=AF.Exp, scale=-1.0),
                     reads=[gt.res], writes=[ef.res])
                P.op("act", lambda E: E.activation(out=ef[:], in_=ef[:], func=AF.Ln, bias=1.0),
                     reads=[ef.res], writes=[ef.res])
                P.op("dve", lambda E: E.tensor_scalar(out=lf[:], in0=ef[:], scalar1=-1.0, scalar2=None, op0=ALU.mult),
                     reads=[ef.res], writes=[lf.res])
                if pb == 0:
                    P.op("dve", lambda E: E.tensor_scalar(out=lf[:], in0=lf[:], scalar1=vflag[:, 0:1], scalar2=None, op0=ALU.mult),
                         reads=[lf.res, vflag.res], writes=[lf.res])
                    P.op("dve", lambda E: E.tensor_scalar(out=gt[:, 0:8], in0=gt[:, 0:8], scalar1=vflag[:, 0:1],
                                                          scalar2=ineg[:, 0:1], op0=ALU.mult, op1=ALU.add),
                         reads=[gt.res, vflag.res, ineg.res], writes=[gt.res])
                if own:
                    for half in range(2):
                        A = pj[1]
                        hs = slice(half * 512, (half + 1) * 512)
                        for k in range(8):
                            P.op("pe", lambda E, A=A, k=k, bsl=bsl, hs=hs: E.matmul(
                                A[:], xg[:, k, bsl], wmo[:, k, hs], start=(k == 0), stop=(k == 7)),
                                reads=[xg.res, wmo.res], writes=[A.res], inc=(k == 7))
                        P.op("dve", lambda E, A=A, hs=hs: E.tensor_tensor(out=og[:, hs], in0=A[:], in1=bmo[:, hs], op=ALU.add),
                             reads=[A.res, bmo.res], writes=[og.res])
                    P.op("act", lambda E: E.activation(out=og[:], in_=og[:], func=AF.Exp, scale=-1.0), reads=[og.res], writes=[og.res])
                    P.op("dve", lambda E: E.tensor_scalar(out=og[:], in0=og[:], scalar1=1.0, scalar2=None, op0=ALU.add),
                         reads=[og.res], writes=[og.res])
                    P.op("dve", lambda E: E.reciprocal(out=og[:], in_=og[:]), reads=[og.res], writes=[og.res])
                P.op("pe", lambda E: E.matmul(pj[1][:, 0:8], tri[:], lf[:], start=True, stop=True),
                     reads=[tri.res, lf.res], writes=[pj[1].res])
                P.op("pe", lambda E: E.matmul(pj[1][:, 8:16], ones[:], lf[:], start=True, stop=True),
                     reads=[ones.res, lf.res], writes=[pj[1].res])
                P.op("dve", lambda E: E.tensor_copy(out=bt[:], in_=pj[1][:, 0:16]), reads=[pj[1].res], writes=[bt.res])
                P.op("dve", lambda E: E.tensor_tensor(out=av[:], in0=gt[:, 0:8], in1=bt[:, 0:8], op=ALU.subtract),
                     reads=[gt.res, bt.res], writes=[av.res])
                for h in range(8):
                    P.op("dve", lambda E, h=h: E.tensor_scalar(
                        out=diag[:, h, :], in0=identf[:], scalar1=av[:, h:h + 1], scalar2=None, op0=ALU.mult),
                        reads=[identf.res, av.res], writes=[diag_r[h]])
                for half in range(2):
                    P.op("pe", lambda E, half=half: E.matmul(
                        pbc[:, half * 512:(half + 1) * 512], ones[:],
                        diag[:, half * 4:(half + 1) * 4, :].rearrange("p h s -> p (h s)"), start=True, stop=True),
                        reads=[ones.res] + diag_r[half * 4:(half + 1) * 4], writes=[pbc.res])
                P.op("dve", lambda E: E.tensor_reduce(out=c8(AMX), in_=pbc[:].rearrange("p (h s) -> p h s", h=8), axis=AX.X, op=ALU.max),
                     reads=[pbc.res], writes=[col.res])
                P.op("dve", lambda E: E.tensor_tensor(out=c8(MNB), in0=c8(AMX), in1=mprev[:], op=ALU.max),
                     reads=[col.res, mprev.res], writes=[col.res])
                if own:
                    P.op("dve", lambda E: E.tensor_tensor(out=Am[:].rearrange("p h s -> p (h s)"), in0=pbc[:], in1=cmk[:], op=ALU.add),
                         reads=[pbc.res, cmk.res], writes=[Am.res])
                    P.op("dve", lambda E: E.tensor_reduce(out=c8(CMAX), in_=Am[:], axis=AX.X, op=ALU.max),
                         reads=[Am.res], writes=[col.res])
                    P.op("dve", lambda E: E.tensor_tensor(out=c8(INTER), in0=bt[:, 0:8], in1=mprev[:], op=ALU.add),
                         reads=[bt.res, mprev.res], writes=[col.res])
                    P.op("dve", lambda E: E.tensor_tensor(out=c8(MT), in0=bt[:, 0:8], in1=c8(CMAX), op=ALU.add),
                         reads=[bt.res, col.res], writes=[col.res])
                    P.op("dve", lambda E: E.tensor_tensor(out=c8(MT), in0=c8(MT), in1=c8(INTER), op=ALU.max),
                         reads=[col.res], writes=[col.res])
                    P.op("dve", lambda E: E.tensor_tensor(out=c8(RR), in0=bt[:, 0:8], in1=c8(MT), op=ALU.subtract),
                         reads=[bt.res, col.res], writes=[col.res])
                    P.op("dve", lambda E: E.tensor_tensor(out=c8(TMP), in0=c8(INTER), in1=c8(MT), op=ALU.subtract),
                         reads=[col.res], writes=[col.res])
                    P.op("act", lambda E: E.activation(out=c8(WINT), in_=c8(TMP), func=AF.Exp, bias=LN_EIGHTH), reads=[col.res], writes=[col.res])
                    P.op("act", lambda E: E.activation(out=c8(EMT), in_=c8(MT), func=AF.Exp, scale=-1.0), reads=[col.res], writes=[col.res])
                    for h in range(8):
                        P.op("dve", lambda E, h=h: E.tensor_scalar(
                            out=diag[:, h, :], in0=identf[:], scalar1=col[:, RR, h:h + 1], scalar2=None, op0=ALU.mult),
                            reads=[identf.res, col.res], writes=[diag_r[h]])
                    for half in range(2):
                        P.op("pe", lambda E, half=half: E.matmul(
                            pbc[:, half * 512:(half + 1) * 512], ones[:],
                            diag[:, half * 4:(half + 1) * 4, :].rearrange("p h s -> p (h s)"), start=True, stop=True),
                            reads=[ones.res] + diag_r[half * 4:(half + 1) * 4], writes=[pbc.res])
                    P.op("dve", lambda E: E.tensor_tensor(out=Am[:].rearrange("p h s -> p (h s)"), in0=pbc[:], in1=cmkT[:], op=ALU.add),
                         reads=[pbc.res, cmkT.res], writes=[Am.res])
                    for h in range(8):
                        P.op("act", lambda E, h=h: E.activation(
                            out=DT[:, hcol(h), :], in_=Am[:, h, :], func=AF.Exp, bias=av[:, h:h + 1]),
                            reads=[Am.res, av.res], writes=[DT_r[h]])
                    for h in range(8):
                        hp = slice((h % 2) * 64, (h % 2) * 64 + 64)
                        c0 = hcol(h) * 128
                        P.op("pe", lambda E, h=h, hp=hp, c0=c0, bsl=bsl: E.matmul(
                            psS[:, c0:c0 + 128], mkT[hp, h // 2, bsl], mqT[hp, h // 2, bsl], start=True, stop=True),
                            reads=[mkT.res, mqT.res], writes=[psS.res])
                    P.op("dve", lambda E: E.tensor_tensor(out=PT[:], in0=psS[:], in1=DT[:].rearrange("p h s -> p (h s)"), op=ALU.mult),
                         reads=[psS.res] + DT_r, writes=[PT.res])
                    for h in range(8):
                        hp = slice((h % 2) * 64, (h % 2) * 64 + 64)
                        c0 = hcol(h) * 128
                        P.op("pe", lambda E, c0=c0, VA=VA, h=h: E.matmul(
                            pj[0][:, 0:129], PT[:, c0:c0 + 128], VA[:, h, :], start=True, stop=True),
                            reads=[PT.res, VA.res], writes=[pj[0].res])
                        P.op("pe", lambda E, hp=hp, h=h, bsl=bsl: E.matmul(
                            pj[1][:, 0:129], mqT[hp, h // 2, bsl], Cbf[hp, h // 2, :], start=True, stop=True),
                            reads=[mqT.res, Cbf.res], writes=[pj[1].res])
                        P.op("dve", lambda E: E.tensor_scalar(out=n1s[:], in0=pj[0][:, 0:129], scalar1=0.125, scalar2=None, op0=ALU.mult),
                             reads=[pj[0].res], writes=[n1s.res])
                        P.op("dve", lambda E, h=h: E.scalar_tensor_tensor(
                            out=nums[:], in0=pj[1][:, 0:129], scalar=col[:, WINT, h:h + 1], in1=n1s[:],
                            op0=ALU.mult, op1=ALU.add),
                            reads=[pj[1].res, col.res, n1s.res], writes=[nums.res])
                        P.op("dve", lambda E, h=h: E.tensor_tensor(out=sc1[:, 0:1], in0=nums[:, 128:129], in1=col[:, EMT, h:h + 1], op=ALU.max),
                             reads=[nums.res, col.res], writes=[sc1.res])
                        P.op("dve", lambda E: E.scalar_tensor_tensor(out=sc1[:, 1:2], in0=nums[:, 128:129], scalar=-1.0, in1=sc1[:, 0:1],
                                                                      op0=ALU.mult, op1=ALU.max),
                             reads=[nums.res, sc1.res], writes=[sc1.res])
                        P.op("dve", lambda E: E.reciprocal(out=sc1[:, 2:3], in_=sc1[:, 1:2]), reads=[sc1.res], writes=[sc1.res])
                        P.op("dve", lambda E, h=h: E.tensor_scalar(
                            out=hbuf[:, h, :], in0=nums[:, 0:128], scalar1=sc1[:, 2:3], scalar2=None, op0=ALU.mult),
                            reads=[nums.res, sc1.res], writes=[hbuf_r[h]])
                    P.op("dve", lambda E: E.tensor_reduce(out=c8(TMP), in_=hbuf[:], axis=AX.X, op=ALU.add),
                         reads=hbuf_r, writes=[col.res])
                    P.op("dve", lambda E: E.tensor_tensor(out=sqt[:], in0=hbuf[:], in1=hbuf[:], op=ALU.mult), reads=hbuf_r, writes=[sqt.res])
                    P.op("dve", lambda E: E.tensor_reduce(out=c8(CMAX), in_=sqt[:], axis=AX.X, op=ALU.add),
                         reads=[sqt.res], writes=[col.res])
                    P.op("dve", lambda E: E.tensor_scalar(out=c8(TMP), in0=c8(TMP), scalar1=-1.0 / 128.0, scalar2=None, op0=ALU.mult),
                         reads=[col.res], writes=[col.res])
                    P.op("dve", lambda E: E.tensor_tensor(out=c8(INTER), in0=c8(TMP), in1=c8(TMP), op=ALU.mult),
                         reads=[col.res], writes=[col.res])
                    P.op("dve", lambda E: E.scalar_tensor_tensor(out=c8(CMAX), in0=c8(CMAX), scalar=1.0 / 128.0, in1=c8(INTER),
                                                                  op0=ALU.mult, op1=ALU.subtract),
                         reads=[col.res], writes=[col.res])
                    P.op("dve", lambda E: E.tensor_scalar(out=c8(CMAX), in0=c8(CMAX), scalar1=GN_EPS, scalar2=None, op0=ALU.add),
                         reads=[col.res], writes=[col.res])
                    P.op("act", lambda E: E.activation(out=c8(CMAX), in_=c8(CMAX), func=AF.Ln), reads=[col.res], writes=[col.res])
                    P.op("act", lambda E: E.activation(out=c8(CMAX), in_=c8(CMAX), func=AF.Exp, scale=-0.5), reads=[col.res], writes=[col.res])
                    for h in range(8):
                        P.op("dve", lambda E, h=h: E.tensor_scalar(
                            out=hbuf[:, h, :], in0=hbuf[:, h, :], scalar1=col[:, TMP, h:h + 1], scalar2=col[:, CMAX, h:h + 1],
                            op0=ALU.add, op1=ALU.mult),
                            reads=[hbuf_r[h], col.res], writes=[hbuf_r[h]])
                    P.op("dve", lambda E: E.tensor_tensor(out=hbuf[:].rearrange("p h s -> p (h s)"),
                                                          in0=hbuf[:].rearrange("p h s -> p (h s)"), in1=gnb[:], op=ALU.mult),
                         reads=hbuf_r + [gnb.res], writes=hbuf_r)
                    P.op("dve", lambda E: E.tensor_tensor(out=ybf[:], in0=hbuf[:].rearrange("p h s -> p (h s)"), in1=og[:], op=ALU.mult),
                         reads=hbuf_r + [og.res], writes=[ybf.res])
                    for c in range(8):
                        P.op("pe", lambda E, c=c: E.transpose(out=pT[:, c * 128:(c + 1) * 128], in_=ybf[:, c * 128:(c + 1) * 128], identity=identb[:]),
                             reads=[ybf.res, identb.res], writes=[pT.res])
                    P.op("act", lambda E: E.activation(out=yT[:], in_=pT[:].rearrange("p (c t) -> p c t", c=8), func=AF.Identity),
                         reads=[pT.res], writes=[yT.res])
                    i = pb // 2
                    P.dma("sp", ymT_s[:, :, i * 128:(i + 1) * 128], yT[:], reads=[yT.res])
                P.op("dve", lambda E: E.tensor_tensor(out=c8(MNEW), in0=bt[:, 8:16], in1=c8(MNB), op=ALU.add),
                     reads=[bt.res, col.res], writes=[col.res])
                P.op("dve", lambda E: E.tensor_tensor(out=c8(CD), in0=bt[:, 8:16], in1=mprev[:], op=ALU.add),
                     reads=[bt.res, mprev.res], writes=[col.res])
                P.op("dve", lambda E: E.tensor_tensor(out=c8(CD), in0=c8(CD), in1=c8(MNEW), op=ALU.subtract),
                     reads=[col.res], writes=[col.res])
                P.op("act", lambda E: E.activation(out=c8(CD), in_=c8(CD), func=AF.Exp), reads=[col.res], writes=[col.res])
                P.op("dve", lambda E: E.tensor_tensor(out=c8(ES), in0=av[:], in1=bt[:, 8:16], op=ALU.add),
                     reads=[av.res, bt.res], writes=[col.res])
                P.op("dve", lambda E: E.tensor_tensor(out=c8(ES), in0=c8(ES), in1=c8(MNEW), op=ALU.subtract),
                     reads=[col.res], writes=[col.res])
                P.op("act", lambda E: E.activation(out=c8(ES), in_=c8(ES), func=AF.Exp), reads=[col.res], writes=[col.res])
                for c in range(4):
                    P.op("pe", lambda E, c=c, bsl=bsl: E.transpose(out=pT[:, c * 128:(c + 1) * 128], in_=mkT[:, c, bsl], identity=identb[:]),
                         reads=[mkT.res, identb.res], writes=[pT.res])
                for h in range(8):
                    P.op("dve", lambda E, h=h: E.tensor_scalar(
                        out=wk[:, h, :], in0=pT[:, h * 64:(h + 1) * 64], scalar1=col[:, ES, h:h + 1], scalar2=None, op0=ALU.mult),
                        reads=[pT.res, col.res], writes=[wk_r[h]])
                for h in range(8):
                    hp = slice((h % 2) * 64, (h % 2) * 64 + 64)
                    off = ((h // 2) // 2) * 512 + ((h // 2) % 2) * 129
                    P.op("pe", lambda E, h=h, hp=hp, off=off, VA=VA: E.matmul(
                        pbc[hp, off:off + 129], wk[:, h, :], VA[:, h, :], start=True, stop=True),
                        reads=[wk_r[h], VA.res], writes=[pbc.res])
                for h in range(8):
                    hp = slice((h % 2) * 64, (h % 2) * 64 + 64)
                    off = ((h // 2) // 2) * 512 + ((h // 2) % 2) * 129
                    P.op("dve", lambda E, h=h, hp=hp, off=off: E.scalar_tensor_tensor(
                        out=Cst[hp, h // 2, :], in0=Cst[hp, h // 2, :], scalar=col[hp, CD, h:h + 1], in1=pbc[hp, off:off + 129],
                        op0=ALU.mult, op1=ALU.add),
                        reads=[Cst_r[h], col.res, pbc.res], writes=[Cst_r[h]])
                P.op("act", lambda E: E.activation(out=Cbf[:], in_=Cst[:], func=AF.Identity), reads=Cst_r, writes=[Cbf.res])
                P.op("dve", lambda E: E.tensor_copy(out=mprev[:], in_=c8(MNEW)), reads=[col.res], writes=[mprev.res])
        for g in range(S // 512):
            m_group(g, xgs[g % 2])
    P.barrier()


IN_SPECS = {
    "xT": ([D, S], F32), "xT_own": ([D, NOWN], F32), "x_own": ([NOWN, D], F32),
    "cosk": ([128, S], F32), "sink": ([128, S], F32),
    "cosq": ([128, NOWN], F32), "sinq": ([128, NOWN], F32),
    "cosi": ([128, NOWN], F32), "sini": ([128, NOWN], F32),
    "ident": ([128, 128], F32), "irep": ([128, 512], F32),
    "negm_diag": ([128, 128], F32), "negm_b0": ([128, 128], F32),
    "wq": ([D, 512], F32), "wq_s": ([D, 512], F32), "wk": ([D, 512], F32), "wk_s": ([D, 512], F32),
    "wv": ([D, 512], F32), "wiq": ([D, 256], F32), "wiq_s": ([D, 256], F32),
    "wik2": ([D, 128], F32), "wik2_s": ([D, 128], F32), "wiw": ([D, 4], F32),
    "bq": ([128, 4], F32), "bq_s": ([128, 4], F32), "bk": ([128, 4], F32), "bk_s": ([128, 4], F32),
    "biq": ([128, 2], F32), "biq_s": ([128, 2], F32), "bik2": ([128, 1], F32), "bik2_s": ([128, 1], F32),
    "bv_bc": ([128, 512], F32), "biw_bc": ([128, 4], F32),
    "wga": ([D, D], F32), "wgm": ([D, D], F32), "bga": ([128, 8], F32), "bgm": ([128, 8], F32),
    "wba": ([512, D], F32), "wbm": ([D, D], F32), "wout": ([D, D], F32),
    "ln1g_bc": ([128, D], F32), "ln1b_bc": ([128, D], F32), "ln2g_bc": ([128, D], F32), "ln2b_bc": ([128, D], F32),
    "wr": ([D, 20], F32), "br_bc": ([128, 20], F32),
    "weg": ([16, D, 512], F32), "weu": ([16, D, 512], F32), "wed": ([16, 512, D], F32),
    "wmq": ([D, 512], F32), "wmk": ([D, 512], F32), "bmq": ([128, 4], F32), "bmk": ([128, 4], F32),
    "cwq": ([128, 16], F32), "cwk": ([128, 16], F32),
    "wmv": ([D, D], F32), "bmv_bc": ([128, D], F32), "wmif": ([D, 16], F32), "bif_bc": ([128, 16], F32),
    "wmo": ([D, D], F32), "bmo_bc": ([128, D], F32), "gn_bc": ([128, D], F32),
    "tri": ([128, 128], F32), "ones": ([128, 128], F32),
    "cmask_rep": ([128, 1024], F32), "cmaskT_rep": ([128, 1024], F32),
    "vflag": ([128, 1], F32), "ineg": ([128, 1], F32),
    "pow2": ([128, 32], F32),
}
GN_EPS = 1e-6
LN_EIGHTH = float(np.log(0.125))
C1SUB = os.environ.get("K_C1SUB", "")
ALPHA = float(2.0 ** 0.25)
LN_EPS = 1e-5


def build_nc(debug=(), stop=None):
    nc = bass.Bass("TRN2", target_bir_lowering=False)
    Dm = {}
    skip = set()
    if stop in ("q", "kv", "A1", "A2", "A3", "C1", "M", "A"):
        skip |= {"weg", "weu", "wed"}
    for name, (shape, dt) in IN_SPECS.items():
        if name in skip:
            continue
        Dm[name] = nc.dram_tensor(name, shape, dt, kind="ExternalInput").ap()
    nc._declared = set(Dm)

    def scratch(name, shape, dt):
        kind = "ExternalOutput" if name in debug else "Internal"
        return nc.dram_tensor(name, shape, dt, kind=kind).ap()

    QT_s = scratch("QT_s", [128, 4, NOWN], BF16)
    qiT_s = scratch("qiT_s", [128, 2, NOWN], BF16)
    yat_s = scratch("yat_s", [NOWN, 512], BF16)
    ymT_s = nc.dram_tensor("ymT_s", [128, 8, NOWN], BF16, kind=("ExternalInput" if "ymT_in" in debug else ("ExternalOutput" if "ymT_s" in debug else "Internal"))).ap()
    x1_s = scratch("x1_s", [NOWN, D], F32)
    x1T_s = scratch("x1T_s", [128, 8, NOWN], BF16)
    out = nc.dram_tensor("out", [NOWN, D], F32, kind="ExternalOutput").ap()

    es = ExitStack()
    with es:
        P = Prog(nc, es)
        top = ExitStack()
        es.enter_context(top)
        Wabs = _sb(nc, top, "Wabs", [128, NPAIR, 4], F32)
        Wsgn = _sb(nc, top, "Wsgn", [128, NPAIR, 4], F32)

        with ExitStack() as ph:
          if stop != 'M':
            wq = _sb(nc, ph, "wq", [128, 8, 512], BF16)
            wqs = _sb(nc, ph, "wqs", [128, 8, 512], BF16)
            wiq = _sb(nc, ph, "wiq", [128, 8, 256], BF16)
            wiqs = _sb(nc, ph, "wiqs", [128, 8, 256], BF16)
            wiw = _sb(nc, ph, "wiw", [128, 8, 4], BF16)
            bq = _sb(nc, ph, "bq", [128, 4], F32)
            bqs = _sb(nc, ph, "bqs", [128, 4], F32)
            biq = _sb(nc, ph, "biq", [128, 2], F32)
            biqs = _sb(nc, ph, "biqs", [128, 2], F32)
            biw = _sb(nc, ph, "biw", [128, 4], F32)
            for t, n in ((wq, "wq"), (wqs, "wq_s"), (wiq, "wiq"), (wiqs, "wiq_s"), (wiw, "wiw")):
                P.dma("pool", t[:], _wview(Dm[n]), writes=[t.res])
            for t, n in ((bq, "bq"), (bqs, "bq_s"), (biq, "biq"), (biqs, "biq_s"), (biw, "biw_bc")):
                P.dma("sp", t[:], Dm[n][:, :], writes=[t.res])
            xo = [_sb(nc, ph, f"xo{j}", [128, 8, 512], BF16) for j in range(2)]
            tabs = [[_sb(nc, ph, f"tab{j}_{n}", [128, 512], F32) for n in range(4)] for j in range(2)]
            t1 = [_sb(nc, ph, f"t1_{j}", [128, 512], F32) for j in range(2)]
            t2 = [_sb(nc, ph, f"t2_{j}", [128, 512], F32) for j in range(2)]
            qo = [_sb(nc, ph, f"qo{j}", [128, 4, 512], BF16) for j in range(2)]
            qio = [_sb(nc, ph, f"qio{j}", [128, 2, 512], BF16) for j in range(2)]
            wtmp = _sb(nc, ph, "wtmp", [128, 4], F32)
            psA = [_ps(nc, ph, f"psA{j}", [128, 512], F32) for j in range(3)]
            psB = [_ps(nc, ph, f"psB{j}", [128, 512], F32) for j in range(3)]
            psW = _ps(nc, ph, "psW", [128, 512], F32)
            xTo = Dm["xT_own"].rearrange("(c p) t -> p c t", p=128)
            rot = 0
            for g in range(NOWN // 512):
                sl = slice(g * 512, (g + 1) * 512)
                X = xo[g % 2]
                TB = tabs[g % 2]
                P.dma("pool", X[:], xTo[:, :, sl], writes=[X.res])
                for n, nm in enumerate(("cosq", "sinq", "cosi", "sini")):
                    P.dma("sp", TB[n][:], Dm[nm][:, sl], writes=[TB[n].res])
                QO, QIO = qo[g % 2], qio[g % 2]
                for c in range(6):
                    isq = c < 4
                    W, Ws, Bt, Bs = (wq, wqs, bq, bqs) if isq else (wiq, wiqs, biq, biqs)
                    cc = c if isq else c - 4
                    ct, st = (TB[0], TB[1]) if isq else (TB[2], TB[3])
                    A, B = psA[rot % 3], psB[rot % 3]
                    T1, T2 = t1[rot % 2], t2[rot % 2]
                    rot += 1
                    for k in range(8):
                        P.op("pe", lambda E, A=A, W=W, X=X, k=k, cc=cc: E.matmul(
                            A[:], W[:, k, cc * 128:(cc + 1) * 128], X[:, k, :], start=(k == 0), stop=(k == 7)),
                            reads=[W.res, X.res], writes=[A.res], inc=(k == 7))
                    for k in range(8):
                        P.op("pe", lambda E, B=B, Ws=Ws, X=X, k=k, cc=cc: E.matmul(
                            B[:], Ws[:, k, cc * 128:(cc + 1) * 128], X[:, k, :], start=(k == 0), stop=(k == 7)),
                            reads=[Ws.res, X.res], writes=[B.res], inc=(k == 7))
                    P.op("dve", lambda E, T1=T1, A=A, Bt=Bt, cc=cc, ct=ct: E.scalar_tensor_tensor(
                        out=T1[:], in0=A[:], scalar=Bt[:, cc:cc + 1], in1=ct[:], op0=ALU.add, op1=ALU.mult),
                        reads=[A.res, Bt.res, ct.res], writes=[T1.res])
                    P.op("dve", lambda E, T2=T2, B=B, Bs=Bs, cc=cc, st=st: E.scalar_tensor_tensor(
                        out=T2[:], in0=B[:], scalar=Bs[:, cc:cc + 1], in1=st[:], op0=ALU.add, op1=ALU.mult),
                        reads=[B.res, Bs.res, st.res], writes=[T2.res])
                    O = QO if isq else QIO
                    P.op("pool", lambda E, O=O, cc=cc, T1=T1, T2=T2: E.tensor_tensor(
                        out=O[:, cc, :], in0=T1[:], in1=T2[:], op=ALU.add),
                        reads=[T1.res, T2.res], writes=[O.res])
                P.dma("sp", QT_s[:, :, sl], QO[:], reads=[QO.res])
                P.dma("sp", qiT_s[:, :, sl], QIO[:], reads=[QIO.res])
                for blk in range(4):
                    i = g * 4 + blk
                    for k in range(8):
                        P.op("pe", lambda E, X=X, k=k, blk=blk: E.matmul(
                            psW[:, 0:4], X[:, k, blk * 128:(blk + 1) * 128], wiw[:, k, :],
                            start=(k == 0), stop=(k == 7)),
                            reads=[X.res, wiw.res], writes=[psW.res], inc=(k == 7))
                    P.op("dve", lambda E: E.tensor_tensor(out=wtmp[:], in0=psW[:, 0:4], in1=biw[:], op=ALU.add),
                         reads=[psW.res, biw.res], writes=[wtmp.res])
                    P.op("act", lambda E, i=i: E.activation(
                        out=Wabs[:, i, :], in_=wtmp[:], func=AF.Abs, scale=1.0 / 16.0),
                        reads=[wtmp.res], writes=[Wabs.res])
                    P.op("dve", lambda E, i=i: E.tensor_scalar(
                        out=Wsgn[:, i, :], in0=wtmp[:], scalar1=0.0, scalar2=2.0,
                        op0=ALU.is_ge, op1=ALU.mult),
                        reads=[wtmp.res], writes=[Wsgn.res])
                    P.op("dve", lambda E, i=i: E.tensor_scalar(
                        out=Wsgn[:, i, :], in0=Wsgn[:, i, :], scalar1=-1.0, scalar2=None, op0=ALU.add),
                        reads=[Wsgn.res], writes=[Wsgn.res])

        P.barrier()
        with ExitStack() as kv:
          if stop not in ('q', 'M'):
            KT = _sb(nc, kv, "KT", [128, 4, S], BF16)
            V = _sb(nc, kv, "V", [128, 64, 8, 65], BF16)
            kiT = _sb(nc, kv, "kiT", [128, S], BF16)
            KTr = [Res(f"KT{g}") for g in range(16)]
            Vr = [Res(f"V{g}") for g in range(16)]
            kir = [Res(f"ki{g}") for g in range(16)]
            P.op("pool", lambda E: E.memset(V[:, :, :, 64:65], 1.0), writes=Vr)
            with ExitStack() as ph:
                wk = _sb(nc, ph, "wk", [128, 8, 512], BF16)
                wks = _sb(nc, ph, "wks", [128, 8, 512], BF16)
                wv = _sb(nc, ph, "wv", [128, 8, 512], BF16)
                wik = _sb(nc, ph, "wik", [128, 8, 128], BF16)
                wiks = _sb(nc, ph, "wiks", [128, 8, 128], BF16)
                bk = _sb(nc, ph, "bk", [128, 4], F32)
                bks = _sb(nc, ph, "bks", [128, 4], F32)
                bik = _sb(nc, ph, "bik", [128, 1], F32)
                biks = _sb(nc, ph, "biks", [128, 1], F32)
                bv = _sb(nc, ph, "bv", [128, 512], F32)
                for t, n in ((wk, "wk"), (wks, "wk_s"), (wv, "wv"), (wik, "wik2"), (wiks, "wik2_s")):
                    P.dma("pool", t[:], _wview(Dm[n]), writes=[t.res])
                for t, n in ((bk, "bk"), (bks, "bk_s"), (bik, "bik2"), (biks, "bik2_s"), (bv, "bv_bc")):
                    P.dma("sp", t[:], Dm[n][:, :], writes=[t.res])
                xg = _sb(nc, ph, "xg", [128, 8, 512], BF16)
                ck = _sb(nc, ph, "ck", [128, 512], F32)
                sk = _sb(nc, ph, "sk", [128, 512], F32)
                t1 = [_sb(nc, ph, f"k_t1_{j}", [128, 512], F32) for j in range(2)]
                t2 = [_sb(nc, ph, f"k_t2_{j}", [128, 512], F32) for j in range(2)]
                psA = [_ps(nc, ph, f"kpsA{j}", [128, 512], F32) for j in range(3)]
                psB = [_ps(nc, ph, f"kpsB{j}", [128, 512], F32) for j in range(3)]
                psV = [_ps(nc, ph, f"kpsV{j}", [128, 512], F32) for j in range(2)]
                xTa = Dm["xT"].rearrange("(c p) t -> p c t", p=128)
                rot = 0
                for g in range(S // 512):
                    sl = slice(g * 512, (g + 1) * 512)
                    P.dma("pool", xg[:], xTa[:, :, sl], writes=[xg.res])
                    P.dma("sp", ck[:], Dm["cosk"][:, sl], writes=[ck.res])
                    P.dma("sp", sk[:], Dm["sink"][:, sl], writes=[sk.res])
                    for c in range(5):
                        isk = c < 4
                        W, Ws, Bt, Bs = (wk, wks, bk, bks) if isk else (wik, wiks, bik, biks)
                        cc = c if isk else 0
                        A, B = psA[rot % 3], psB[rot % 3]
                        T1, T2 = t1[rot % 2], t2[rot % 2]
                        rot += 1
                        for k in range(8):
                            P.op("pe", lambda E, A=A, W=W, k=k, cc=cc: E.matmul(
                                A[:], W[:, k, cc * 128:(cc + 1) * 128], xg[:, k, :], start=(k == 0), stop=(k == 7)),
                                reads=[W.res, xg.res], writes=[A.res], inc=(k == 7))
                        for k in range(8):
                            P.op("pe", lambda E, B=B, Ws=Ws, k=k, cc=cc: E.matmul(
                                B[:], Ws[:, k, cc * 128:(cc + 1) * 128], xg[:, k, :], start=(k == 0), stop=(k == 7)),
                                reads=[Ws.res, xg.res], writes=[B.res], inc=(k == 7))
                        P.op("dve", lambda E, T1=T1, A=A, Bt=Bt, cc=cc: E.scalar_tensor_tensor(
                            out=T1[:], in0=A[:], scalar=Bt[:, cc:cc + 1], in1=ck[:], op0=ALU.add, op1=ALU.mult),
                            reads=[A.res, Bt.res, ck.res], writes=[T1.res])
                        P.op("dve", lambda E, T2=T2, B=B, Bs=Bs, cc=cc: E.scalar_tensor_tensor(
                            out=T2[:], in0=B[:], scalar=Bs[:, cc:cc + 1], in1=sk[:], op0=ALU.add, op1=ALU.mult),
                            reads=[B.res, Bs.res, sk.res], writes=[T2.res])
                        if isk:
                            P.op("pool", lambda E, cc=cc, T1=T1, T2=T2, sl=sl: E.tensor_tensor(
                                out=KT[:, cc, sl], in0=T1[:], in1=T2[:], op=ALU.add),
                                reads=[T1.res, T2.res], writes=[KTr[g]])
                        else:
                            P.op("pool", lambda E, T1=T1, T2=T2, sl=sl: E.tensor_tensor(
                                out=kiT[:, sl], in0=T1[:], in1=T2[:], op=ALU.add),
                                reads=[T1.res, T2.res], writes=[kir[g]])
                    for blk in range(4):
                        pb = g * 4 + blk
                        PV = psV[blk % 2]
                        for k in range(8):
                            P.op("pe", lambda E, PV=PV, k=k, blk=blk: E.matmul(
                                PV[:], xg[:, k, blk * 128:(blk + 1) * 128], wv[:, k, :],
                                start=(k == 0), stop=(k == 7)),
                                reads=[xg.res, wv.res], writes=[PV.res], inc=(k == 7))
                        P.op("dve", lambda E, PV=PV, pb=pb: E.tensor_tensor(
                            out=V[:, pb, :, 0:64], in0=PV[:].rearrange("p (h d) -> p h d", h=8),
                            in1=bv[:].rearrange("p (h d) -> p h d", h=8), op=ALU.add),
                            reads=[PV.res, bv.res], writes=[Vr[g]])

            P.barrier()
            with ExitStack() as ph:
                score = _sb(nc, ph, "score", [128, S], F32)
                Mneg = _sb(nc, ph, "Mneg", [128, S], F8)
                junkt = _sb(nc, ph, "junkt", [128, S], U8)
                irep = _sb(nc, ph, "irep", [128, 512], BF16)
                nmd = _sb(nc, ph, "nmd", [128, 128], BF16)
                nm0 = _sb(nc, ph, "nm0", [128, 128], BF16)
                P.dma("pool", irep[:], Dm["irep"][:, :], writes=[irep.res])
                P.dma("pool", nmd[:], Dm["negm_diag"][:, :], writes=[nmd.res])
                P.dma("pool", nm0[:], Dm["negm_b0"][:, :], writes=[nm0.res])
                QTb = _sb(nc, ph, "QTb", [128, 4, 128], BF16)
                qiTbs = [_sb(nc, ph, f"qiTb{j}", [128, 2, 128], BF16) for j in range(2)]
                rt = [_sb(nc, ph, f"rt{j}", [128, 512], F32) for j in range(2)]
                PT = [_sb(nc, ph, f"PT{j}", [128, 1024], BF16) for j in range(2)]
                ytm = _sb(nc, ph, "ytm", [128, 512], BF16)
                lo = _sb(nc, ph, "lo", [128, 1], F32)
                hi = _sb(nc, ph, "hi", [128, 1], F32)
                mid = _sb(nc, ph, "mid", [128, 1], F32)
                cnt = _sb(nc, ph, "cnt", [128, 1], F32)
                uu = _sb(nc, ph, "uu", [128, 1], F32)
                pw2 = _sb(nc, ph, "pw2", [128, 32], F32)
                hv = _sb(nc, ph, "hv", [128, 32], F32)
                tw = _sb(nc, ph, "tw", [128, 32], F32)
                P.dma("sp", pw2[:], Dm["pow2"][:, :], writes=[pw2.res])
                rz = _sb(nc, ph, "rz", [128, 8], F32)
                psL = [_ps(nc, ph, f"psL{j}", [128, 1024], F32) for j in range(2)]
                psO = [_ps(nc, ph, f"psO{j}", [128, 512], F32) for j in range(2)]
                psI = [_ps(nc, ph, f"psI{j}", [128, 512], F32) for j in range(2)]
                NA = 0 if stop == 'kv' else (3 if stop in ('A1', 'A2', 'A3') else NBLK)

                def a_index(i):
                    nkb = 2 * i + 2
                    Sc = nkb * 128
                    osl = slice(i * 128, (i + 1) * 128)
                    qiTb = qiTbs[i % 2]
                    P.dma("sp", qiTb[:], qiT_s[:, :, osl], writes=[qiTb.res])
                    for kc in range((Sc + 511) // 512):
                        n = min(512, Sc - kc * 512)
                        ksl = slice(kc * 512, kc * 512 + n)
                        for h in range(4):
                            T = psI[h % 2]
                            pr = slice((h % 2) * 64, (h % 2) * 64 + 64)
                            P.op("pe", lambda E, T=T, n=n, pr=pr, h=h, ksl=ksl, qiTb=qiTb: E.matmul(
                                T[:, 0:n], qiTb[pr, h // 2, :], kiT[pr, ksl], start=True, stop=True),
                                reads=[qiTb.res, kir[kc]], writes=[T.res])
                            if h == 0:
                                P.op("act", lambda E, T=T, n=n, ksl=ksl, i=i: E.activation(
                                    out=score[:, ksl], in_=T[:, 0:n], func=AF.Relu, scale=Wabs[:, i, 0:1]),
                                    reads=[T.res, Wabs.res], writes=[score.res])
                                P.op("dve", lambda E, ksl=ksl, i=i: E.tensor_scalar(
                                    out=score[:, ksl], in0=score[:, ksl], scalar1=Wsgn[:, i, 0:1], scalar2=None,
                                    op0=ALU.mult),
                                    reads=[score.res, Wsgn.res], writes=[score.res])
                            else:
                                R = rt[h % 2]
                                P.op("act", lambda E, T=T, n=n, R=R, i=i, h=h: E.activation(
                                    out=R[:, 0:n], in_=T[:, 0:n], func=AF.Relu, scale=Wabs[:, i, h:h + 1]),
                                    reads=[T.res, Wabs.res], writes=[R.res])
                                P.op("dve", lambda E, R=R, n=n, ksl=ksl, i=i, h=h: E.scalar_tensor_tensor(
                                    out=score[:, ksl], in0=R[:, 0:n], scalar=Wsgn[:, i, h:h + 1], in1=score[:, ksl],
                                    op0=ALU.mult, op1=ALU.add),
                                    reads=[R.res, Wsgn.res, score.res], writes=[score.res])
                    P.op("dve", lambda E, Sc=Sc: E.tensor_reduce(out=hi[:], in_=score[:, 0:Sc], axis=AX.X, op=ALU.max),
                         reads=[score.res], writes=[hi.res])
                    P.op("dve", lambda E, Sc=Sc: E.tensor_reduce(out=lo[:], in_=score[:, 0:Sc], axis=AX.X, op=ALU.min),
                         reads=[score.res], writes=[lo.res])
                    P.op("dve", lambda E: E.tensor_tensor(out=score[:, 0:128], in0=score[:, 0:128], in1=nm0[:], op=ALU.add),
                         reads=[score.res, nm0.res], writes=[score.res])
                    P.op("dve", lambda E, Sc=Sc: E.tensor_tensor(
                        out=score[:, Sc - 128:Sc], in0=score[:, Sc - 128:Sc], in1=nmd[:], op=ALU.add),
                        reads=[score.res, nmd.res], writes=[score.res])
                    P.op("dve", lambda E: E.tensor_tensor(out=uu[:], in0=hi[:], in1=lo[:], op=ALU.subtract),
                         reads=[hi.res, lo.res], writes=[uu.res])
                    P.op("dve", lambda E: E.tensor_scalar(out=hv[:], in0=pw2[:], scalar1=uu[:, 0:1], scalar2=None, op0=ALU.mult),
                         reads=[pw2.res, uu.res], writes=[hv.res])
                    P.op("dve", lambda E: E.tensor_scalar(out=tw[:], in0=pw2[:], scalar1=uu[:, 0:1], scalar2=2.0,
                                                          op0=ALU.mult, op1=ALU.mult),
                         reads=[pw2.res, uu.res], writes=[tw.res])
                    P.op("dve", lambda E: E.tensor_tensor(out=mid[:], in0=lo[:], in1=hv[:, 0:1], op=ALU.add),
                         reads=[lo.res, hv.res], writes=[mid.res])
                    for it in range(NIT):
                        last = it == NIT - 1
                        j = it if last else it + 1
                        SA = hv if last else tw
                        P.op("dve", lambda E, Sc=Sc: E.tensor_scalar(
                            out=junkt[:, 0:Sc], in0=score[:, 0:Sc], scalar1=mid[:, 0:1], scalar2=None,
                            op0=ALU.is_ge, op1=ALU.add, accum_out=cnt[:]),
                            reads=[score.res, mid.res], writes=[junkt.res, cnt.res])
                        P.op("dve", lambda E, SA=SA, j=j: E.tensor_scalar(
                            out=uu[:], in0=cnt[:], scalar1=TOPK - 0.5, scalar2=SA[:, j:j + 1], op0=ALU.is_ge, op1=ALU.mult),
                            reads=[cnt.res, SA.res], writes=[uu.res])
                        P.op("dve", lambda E, j=j: E.scalar_tensor_tensor(
                            out=mid[:], in0=uu[:], scalar=hv[:, j:j + 1], in1=mid[:], op0=ALU.subtract, op1=ALU.add),
                            reads=[uu.res, hv.res, mid.res], writes=[mid.res])

                def a_mask(i):
                    Sc = (2 * i + 2) * 128
                    P.op("dve", lambda E, Sc=Sc: E.tensor_scalar(
                        out=Mneg[:, 0:Sc], in0=score[:, 0:Sc], scalar1=mid[:, 0:1], scalar2=-BIG,
                        op0=ALU.is_lt, op1=ALU.mult, saturate=False),
                        reads=[score.res, mid.res], writes=[Mneg.res])

                def a_attend(i):
                    nkb = 2 * i + 2
                    osl = slice(i * 128, (i + 1) * 128)
                    P.dma("sp", QTb[:], QT_s[:, :, osl], writes=[QTb.res])
                    for kb in range(nkb):
                        L = psL[kb % 2]
                        Pt = PT[kb % 2]
                        bsl = slice(kb * 128, (kb + 1) * 128)
                        g = kb // 4
                        for half in range(2):
                            P.op("pe", lambda E, L=L, half=half, bsl=bsl: E.matmul(
                                L[:, half * 512:(half + 1) * 512], Mneg[:, bsl], irep[:], start=True, stop=False),
                                reads=[Mneg.res, irep.res], writes=[L.res], inc=False)
                        for hh in range(4):
                            for half in range(2):
                                h = 2 * hh + half
                                pr = slice(half * 64, half * 64 + 64)
                                c0 = half * 512 + hh * 128
                                P.op("pe", lambda E, L=L, h=h, pr=pr, bsl=bsl, hh=hh, c0=c0: E.matmul(
                                    L[:, c0:c0 + 128], KT[pr, h // 2, bsl], QTb[pr, h // 2, :],
                                    start=False, stop=(hh == 3)),
                                    reads=[KTr[g], QTb.res], writes=[L.res], inc=(hh == 3 and half == 1))
                        P.op("act", lambda E, L=L, Pt=Pt: E.activation(out=Pt[:], in_=L[:], func=AF.Exp),
                             reads=[L.res], writes=[Pt.res])
                        for h in range(8):
                            O = psO[h // 4]
                            c0 = (h % 4) * 65
                            pc = (h % 2) * 512 + (h // 2) * 128
                            P.op("pe", lambda E, O=O, c0=c0, Pt=Pt, h=h, kb=kb, nkb=nkb, pc=pc: E.matmul(
                                O[:, c0:c0 + 65], Pt[:, pc:pc + 128], V[:, kb, h, :],
                                start=(kb == 0 and h % 4 == 0), stop=(kb == nkb - 1), skip_group_check=True),
                                reads=[Pt.res, Vr[g]], writes=[O.res], inc=(h == 7))
                    for b2 in range(2):
                        O = psO[b2]
                        P.op("dve", lambda E, O=O, b2=b2: E.reciprocal(
                            out=rz[:, b2 * 4:(b2 + 1) * 4],
                            in_=O[:, 0:260].rearrange("p (h d) -> p h d", d=65)[:, :, 64]),
                            reads=[O.res], writes=[rz.res])
                    for h in range(8):
                        O = psO[h // 4]
                        c0 = (h % 4) * 65
                        P.op("dve", lambda E, O=O, c0=c0, h=h: E.tensor_scalar(
                            out=ytm[:, h * 64:(h + 1) * 64], in0=O[:, c0:c0 + 64], scalar1=rz[:, h:h + 1],
                            scalar2=None, op0=ALU.mult),
                            reads=[O.res, rz.res], writes=[ytm.res])
                    P.dma("sp", yat_s[osl, :], ytm[:], reads=[ytm.res])

                if NA > 0:
                    a_index(0)
                    a_mask(0)
                for i in range(NA):
                    if i + 1 < NA:
                        a_index(i + 1)
                    a_attend(i)
                    if i + 1 < NA:
                        a_mask(i + 1)

        P.barrier()
        comb = _sb(nc, top, "comb", [128, NPAIR, 16], F32)
        if stop not in ("q", "kv", "A1", "A2", "A3", "A") and "ymT_in" not in debug:
            _phase_mlstm(P, nc, Dm, ymT_s)
        if stop not in ("q", "kv", "A1", "A2", "A3", "M", "A"):
          with ExitStack() as ph:
            wga = _sb(nc, ph, "wga", [128, 8, D], BF16)
            wgm = _sb(nc, ph, "wgm", [128, 8, D], BF16)
            wba = _sb(nc, ph, "wba", [128, 4, D], BF16)
            wbm = _sb(nc, ph, "wbm", [128, 8, D], BF16)
            wout = _sb(nc, ph, "wout", [128, 8, D], BF16)
            for t, n in ((wga, "wga"), (wgm, "wgm"), (wba, "wba"), (wbm, "wbm"), (wout, "wout")):
                P.dma("pool", t[:], _wview(Dm[n]), writes=[t.res])
            bga = _sb(nc, ph, "bga", [128, 8], F32)
            bgm = _sb(nc, ph, "bgm", [128, 8], F32)
            g1 = _sb(nc, ph, "g1", [128, D], F32)
            b1 = _sb(nc, ph, "b1", [128, D], F32)
            wr = _sb(nc, ph, "wr", [128, 8, 20], F32)
            brb = _sb(nc, ph, "brb", [128, 20], F32)
            identf = _sb(nc, ph, "identf", [128, 128], F32)
            identb = _sb(nc, ph, "identb", [128, 128], BF16)
            ones4 = _sb(nc, ph, "ones4", [128, 4], F32)
            for t, n in ((bga, "bga"), (bgm, "bgm"), (g1, "ln1g_bc"), (b1, "ln1b_bc"), (brb, "br_bc"), (identf, "ident")):
                P.dma("sp", t[:], Dm[n][:, :], writes=[t.res])
            P.dma("sp", wr[:], _wview(Dm["wr"]), writes=[wr.res])
            P.dma("pool", identb[:], Dm["ident"][:, :], writes=[identb.res])
            P.op("pool", lambda E: E.memset(ones4[:], 1.0), writes=[ones4.res])
            xos = [_sb(nc, ph, f"c_xo{j}", [128, 8, 512], BF16) for j in range(2)]
            ymTs = [_sb(nc, ph, f"c_ymT{j}", [128, 8, 512], BF16) for j in range(2)]
            yaT = _sb(nc, ph, "c_yaT", [128, 4, 512], BF16)
            yab = [_sb(nc, ph, f"c_yab{j}", [128, 512], BF16) for j in range(2)]
            mT = _sb(nc, ph, "c_mT", [128, 8, 512], BF16)
            sa = [_sb(nc, ph, f"c_sa{j}", [128, 512], F32) for j in range(2)]
            sm = [_sb(nc, ph, f"c_sm{j}", [128, 512], F32) for j in range(2)]
            m1 = [_sb(nc, ph, f"c_m1{j}", [128, 512], F32) for j in range(2)]
            m2 = [_sb(nc, ph, f"c_m2{j}", [128, 512], F32) for j in range(2)]
            xb_2 = [_sb(nc, ph, f"c_xb{j}", [128, D], F32) for j in range(2)]
            z_2 = [_sb(nc, ph, f"c_z{j}", [128, D], F32) for j in range(2)]
            x1_2 = [_sb(nc, ph, f"c_x1{j}", [128, D], F32) for j in range(2)]
            junk = _sb(nc, ph, "c_junk", [128, D], F32)
            x1Tf_2 = [_sb(nc, ph, f"c_x1Tf{j}", [128, 8, 128], F32) for j in range(2)]
            x1Tb_2 = [_sb(nc, ph, f"c_x1Tb{j}", [128, 8, 128], BF16) for j in range(2)]
            st_2 = [_sb(nc, ph, f"c_st{j}", [128, 8], F32) for j in range(2)]
            rl_2 = [_sb(nc, ph, f"c_rl{j}", [128, 20], F32) for j in range(2)]
            rs_2 = [_sb(nc, ph, f"c_rs{j}", [128, 16], F32) for j in range(2)]
            gm_2 = [_sb(nc, ph, f"c_gm{j}", [128, 16], F32) for j in range(2)]
            elm_2 = [_sb(nc, ph, f"c_elm{j}", [128, 16], F32) for j in range(2)]
            oh1_2 = [_sb(nc, ph, f"c_oh1{j}", [128, 16], F32) for j in range(2)]
            oh2_2 = [_sb(nc, ph, f"c_oh2{j}", [128, 16], F32) for j in range(2)]
            goh_2 = [_sb(nc, ph, f"c_goh{j}", [128, 4], F32) for j in range(2)]
            gex_2 = [_sb(nc, ph, f"c_gex{j}", [128, 4], F32) for j in range(2)]
            psA = _ps(nc, ph, "c_psA", [128, 512], F32)
            psB = _ps(nc, ph, "c_psB", [128, 512], F32)
            psC = _ps(nc, ph, "c_psC", [128, 512], F32)
            psD = _ps(nc, ph, "c_psD", [128, 512], F32)
            psO = _ps(nc, ph, "c_psO", [128, 512], F32)
            psT = _ps(nc, ph, "c_psT", [128, 1024], BF16)
            psX = _ps(nc, ph, "c_psX", [128, 512], F32)
            psR = _ps(nc, ph, "c_psR", [128, 512], F32)
            xTo = Dm["xT_own"].rearrange("(c p) t -> p c t", p=128)
            BIGM = 1.0e4
            def c1_block(g, blk, xb, z, x1, x1Tf, x1Tb, st, rl, rs, gm, elm, oh1, oh2, goh, gex):
                i = g * 4 + blk
                tsl = slice(blk * 128, (blk + 1) * 128)
                rows = slice(i * 128, (i + 1) * 128)
                P.dma("sp", xb[:], Dm["x_own"][rows, :], writes=[xb.res])
                for half in range(2):
                    hs = slice(half * 512, (half + 1) * 512)
                    for c in range(8):
                        P.op("pe", lambda E, c=c, tsl=tsl, hs=hs: E.matmul(psO[:], mT[:, c, tsl], wout[:, c, hs], start=(c == 0), stop=(c == 7)),
                             reads=[mT.res, wout.res], writes=[psO.res], inc=(c == 7))
                    P.op("dve", lambda E, hs=hs: E.scalar_tensor_tensor(
                        out=z[:, hs], in0=xb[:, hs], scalar=ALPHA, in1=psO[:], op0=ALU.mult, op1=ALU.add),
                        reads=[xb.res, psO.res], writes=[z.res])
                _layer_norm(P, z, x1, junk, g1, b1, st, LN_EPS)
                P.dma("sp", x1_s[rows, :], x1[:], reads=[x1.res])
                if C1SUB == 'c':
                    return
                for rnd in range(2):
                    for c4 in range(4):
                        c = rnd * 4 + c4
                        P.op("pe", lambda E, c=c, c4=c4: E.transpose(
                            out=psX[:, c4 * 128:(c4 + 1) * 128], in_=x1[:, c * 128:(c + 1) * 128], identity=identf[:]),
                            reads=[x1.res, identf.res], writes=[psX.res])
                    P.op("act", lambda E, rnd=rnd: E.activation(
                        out=x1Tf[:, rnd * 4:(rnd + 1) * 4, :], in_=psX[:].rearrange("p (c t) -> p c t", c=4), func=AF.Identity),
                        reads=[psX.res], writes=[x1Tf.res])
                    P.op("dve", lambda E, rnd=rnd: E.tensor_copy(
                        out=x1Tb[:, rnd * 4:(rnd + 1) * 4, :], in_=psX[:].rearrange("p (c t) -> p c t", c=4)),
                        reads=[psX.res], writes=[x1Tb.res])
                P.dma("sp", x1T_s[:, :, rows], x1Tb[:], reads=[x1Tb.res])
                if C1SUB == 'd':
                    return
                for c in range(8):
                    P.op("pe", lambda E, c=c: E.matmul(psR[:, 0:20], x1Tf[:, c, :], wr[:, c, :], start=(c == 0), stop=(c == 7)),
                         reads=[x1Tf.res, wr.res], writes=[psR.res], inc=(c == 7))
                P.op("dve", lambda E: E.tensor_tensor(out=rl[:], in0=psR[:, 0:20], in1=brb[:], op=ALU.add),
                     reads=[psR.res, brb.res], writes=[rl.res])
                P.op("dve", lambda E: E.tensor_reduce(out=rs[:, 0:1], in_=rl[:, 0:4], axis=AX.X, op=ALU.max),
                     reads=[rl.res], writes=[rs.res])
                P.op("dve", lambda E: E.tensor_scalar(out=rs[:, 1:2], in0=rs[:, 0:1], scalar1=-1.0, scalar2=None, op0=ALU.mult),
                     reads=[rs.res], writes=[rs.res])
                P.op("act", lambda E: E.activation(out=gex[:], in_=rl[:, 0:4], func=AF.Exp, bias=rs[:, 1:2], accum_out=rs[:, 2:3]),
                     reads=[rl.res, rs.res], writes=[gex.res, rs.res])
                P.op("dve", lambda E: E.reciprocal(out=rs[:, 3:4], in_=rs[:, 2:3]), reads=[rs.res], writes=[rs.res])
                P.op("dve", lambda E: E.tensor_scalar(out=goh[:], in0=rl[:, 0:4], scalar1=rs[:, 0:1], scalar2=None, op0=ALU.is_ge),
                     reads=[rl.res, rs.res], writes=[goh.res])
                for j in range(4):
                    P.op("dve", lambda E, j=j: E.tensor_scalar(
                        out=gm[:, j * 4:(j + 1) * 4], in0=ones4[:], scalar1=goh[:, j:j + 1], scalar2=None, op0=ALU.mult),
                        reads=[ones4.res, goh.res], writes=[gm.res])
                P.op("dve", lambda E: E.tensor_scalar(out=gm[:], in0=gm[:], scalar1=-1.0, scalar2=BIGM, op0=ALU.add, op1=ALU.mult),
                     reads=[gm.res], writes=[gm.res])
                P.op("dve", lambda E: E.tensor_tensor(out=elm[:], in0=gm[:], in1=rl[:, 4:20], op=ALU.add),
                     reads=[gm.res, rl.res], writes=[elm.res])
                P.op("dve", lambda E: E.tensor_reduce(out=rs[:, 4:5], in_=elm[:], axis=AX.X, op=ALU.max),
                     reads=[elm.res], writes=[rs.res])
                P.op("dve", lambda E: E.tensor_scalar(out=oh1[:], in0=elm[:], scalar1=rs[:, 4:5], scalar2=None, op0=ALU.is_ge),
                     reads=[elm.res, rs.res], writes=[oh1.res])
                P.op("dve", lambda E: E.scalar_tensor_tensor(out=elm[:], in0=oh1[:], scalar=-BIGM, in1=elm[:], op0=ALU.mult, op1=ALU.add),
                     reads=[oh1.res, elm.res], writes=[elm.res])
                P.op("dve", lambda E: E.tensor_reduce(out=rs[:, 5:6], in_=elm[:], axis=AX.X, op=ALU.max),
                     reads=[elm.res], writes=[rs.res])
                P.op("dve", lambda E: E.tensor_scalar(out=oh2[:], in0=elm[:], scalar1=rs[:, 5:6], scalar2=None, op0=ALU.is_ge),
                     reads=[elm.res, rs.res], writes=[oh2.res])
                P.op("dve", lambda E: E.tensor_tensor(out=rs[:, 6:7], in0=rs[:, 5:6], in1=rs[:, 4:5], op=ALU.subtract),
                     reads=[rs.res], writes=[rs.res])
                P.op("act", lambda E: E.activation(out=rs[:, 7:8], in_=rs[:, 6:7], func=AF.Exp),
                     reads=[rs.res], writes=[rs.res])
                P.op("dve", lambda E: E.tensor_scalar(out=rs[:, 8:9], in0=rs[:, 7:8], scalar1=1.0, scalar2=None, op0=ALU.add),
                     reads=[rs.res], writes=[rs.res])
                P.op("dve", lambda E: E.reciprocal(out=rs[:, 8:9], in_=rs[:, 8:9]), reads=[rs.res], writes=[rs.res])
                P.op("dve", lambda E: E.tensor_tensor(out=rs[:, 9:10], in0=rs[:, 7:8], in1=rs[:, 8:9], op=ALU.mult),
                     reads=[rs.res], writes=[rs.res])
                P.op("dve", lambda E: E.tensor_scalar(out=oh1[:], in0=oh1[:], scalar1=rs[:, 8:9], scalar2=None, op0=ALU.mult),
                     reads=[oh1.res, rs.res], writes=[oh1.res])
                P.op("dve", lambda E: E.scalar_tensor_tensor(out=oh2[:], in0=oh2[:], scalar=rs[:, 9:10], in1=oh1[:], op0=ALU.mult, op1=ALU.add),
                     reads=[oh2.res, rs.res, oh1.res], writes=[oh2.res])
                P.op("dve", lambda E, i=i: E.tensor_scalar(out=comb[:, i, :], in0=oh2[:], scalar1=rs[:, 3:4], scalar2=None, op0=ALU.mult),
                     reads=[oh2.res, rs.res], writes=[comb.res])
            def c1_group(g, xo, ymT):
                sl = slice(g * 512, (g + 1) * 512)
                P.dma("pool", xo[:], xTo[:, :, sl], writes=[xo.res])
                P.dma("sp", ymT[:], ymT_s[:, :, sl], writes=[ymT.res])
                for blk in range(4):
                    Yb = yab[blk % 2]
                    P.dma("sp", Yb[:], yat_s[g * 512 + blk * 128:g * 512 + (blk + 1) * 128, :], writes=[Yb.res])
                    for c in range(4):
                        P.op("pe", lambda E, Yb=Yb, c=c: E.transpose(
                            out=psT[:, c * 128:(c + 1) * 128], in_=Yb[:, c * 128:(c + 1) * 128], identity=identb[:]),
                            reads=[Yb.res, identb.res], writes=[psT.res])
                    P.op("act", lambda E, blk=blk: E.activation(
                        out=yaT[:, :, blk * 128:(blk + 1) * 128], in_=psT[:, 0:512].rearrange("p (c t) -> p c t", c=4),
                        func=AF.Identity),
                        reads=[psT.res], writes=[yaT.res])
                if C1SUB == 'a':
                    return
                for mc in range(8):
                    msl = slice(mc * 128, (mc + 1) * 128)
                    SA, SM, M1, M2 = sa[mc % 2], sm[mc % 2], m1[mc % 2], m2[mc % 2]
                    for c in range(4):
                        P.op("pe", lambda E, c=c, msl=msl: E.matmul(psA[:], wba[:, c, msl], yaT[:, c, :], start=(c == 0), stop=(c == 3)),
                             reads=[wba.res, yaT.res], writes=[psA.res], inc=(c == 3))
                    for c in range(8):
                        P.op("pe", lambda E, c=c, msl=msl: E.matmul(psB[:], wbm[:, c, msl], ymT[:, c, :], start=(c == 0), stop=(c == 7)),
                             reads=[wbm.res, ymT.res], writes=[psB.res], inc=(c == 7))
                    for c in range(8):
                        P.op("pe", lambda E, c=c, msl=msl: E.matmul(psC[:], wga[:, c, msl], xo[:, c, :], start=(c == 0), stop=(c == 7)),
                             reads=[wga.res, xo.res], writes=[psC.res], inc=(c == 7))
                    for c in range(8):
                        P.op("pe", lambda E, c=c, msl=msl: E.matmul(psD[:], wgm[:, c, msl], xo[:, c, :], start=(c == 0), stop=(c == 7)),
                             reads=[wgm.res, xo.res], writes=[psD.res], inc=(c == 7))
                    P.op("act", lambda E, SA=SA, mc=mc: E.activation(out=SA[:], in_=psC[:], func=AF.Sigmoid, bias=bga[:, mc:mc + 1]),
                         reads=[psC.res, bga.res], writes=[SA.res])
                    P.op("act", lambda E, SM=SM, mc=mc: E.activation(out=SM[:], in_=psD[:], func=AF.Sigmoid, bias=bgm[:, mc:mc + 1]),
                         reads=[psD.res, bgm.res], writes=[SM.res])
                    P.op("dve", lambda E, M1=M1, SA=SA: E.tensor_tensor(out=M1[:], in0=psA[:], in1=SA[:], op=ALU.mult),
                         reads=[psA.res, SA.res], writes=[M1.res])
                    P.op("dve", lambda E, M2=M2, SM=SM: E.tensor_tensor(out=M2[:], in0=psB[:], in1=SM[:], op=ALU.mult),
                         reads=[psB.res, SM.res], writes=[M2.res])
                    P.op("pool", lambda E, M1=M1, M2=M2, mc=mc: E.tensor_tensor(out=mT[:, mc, :], in0=M1[:], in1=M2[:], op=ALU.add),
                         reads=[M1.res, M2.res], writes=[mT.res])
                if C1SUB == 'b':
                    return
                for blk in range(4):
                    j2 = (g * 4 + blk) % 2
                    c1_block(g, blk, xb_2[j2], z_2[j2], x1_2[j2], x1Tf_2[j2], x1Tb_2[j2], st_2[j2], rl_2[j2], rs_2[j2], gm_2[j2], elm_2[j2], oh1_2[j2], oh2_2[j2], goh_2[j2], gex_2[j2])
            for g in range(NBLK // 4):
                c1_group(g, xos[g % 2], ymTs[g % 2])
          P.barrier()

        if stop not in ("q", "kv", "A1", "A2", "A3", "C1", "M", "A"):
          with ExitStack() as ph:
            NBH = min(NBLK, 16)
            NH = NBH * 128
            acc = _sb(nc, ph, "e_acc", [128, NBH, D], F32)
            x1T = _sb(nc, ph, "e_x1T", [128, 8, NH], BF16)
            wg = [_sb(nc, ph, f"e_wg{j}", [128, 8, 512], BF16) for j in range(2)]
            wu = [_sb(nc, ph, f"e_wu{j}", [128, 8, 512], BF16) for j in range(2)]
            wd = [_sb(nc, ph, f"e_wd{j}", [128, 4, D], BF16) for j in range(2)]
            sg = [_sb(nc, ph, f"e_sg{j}", [128, 512], F32) for j in range(2)]
            hdn = [_sb(nc, ph, f"e_hdn{j}", [128, 4, 512], BF16) for j in range(2)]
            g2 = _sb(nc, ph, "e_g2", [128, D], F32)
            b2 = _sb(nc, ph, "e_b2", [128, D], F32)
            junk = _sb(nc, ph, "e_junk", [128, D], F32)
            ot = [_sb(nc, ph, f"e_ot{j}", [128, D], F32) for j in range(2)]
            zt = _sb(nc, ph, "e_zt", [128, D], F32)
            st = _sb(nc, ph, "e_st", [128, 8], F32)
            P.dma("sp", g2[:], Dm["ln2g_bc"][:, :], writes=[g2.res])
            P.dma("sp", b2[:], Dm["ln2b_bc"][:, :], writes=[b2.res])
            psG = [_ps(nc, ph, f"e_psG{j}", [128, 512], F32) for j in range(2)]
            psU = [_ps(nc, ph, f"e_psU{j}", [128, 512], F32) for j in range(2)]
            psDn = [_ps(nc, ph, f"e_psDn{j}", [128, 512], F32) for j in range(4)]
            accr = [Res(f"acc{b}") for b in range(NBH)]
            for hf in range(2 if NBLK == 32 else 1):
                tok = slice(hf * NH, (hf + 1) * NH)
                P.dma("sp", x1T[:], x1T_s[:, :, tok], writes=[x1T.res])
                for b in range(NBH):
                    rows = slice(hf * NH + b * 128, hf * NH + (b + 1) * 128)
                    P.dma("sp", acc[:, b, :], x1_s[rows, :], writes=[accr[b]])
                    P.op("act", lambda E, b=b: E.activation(out=acc[:, b, :], in_=acc[:, b, :], func=AF.Identity, scale=ALPHA),
                         reads=[accr[b]], writes=[accr[b]])
                for e in range(16):
                    WG, WU, WD = wg[e % 2], wu[e % 2], wd[e % 2]
                    P.dma("pool", WG[:], _wview(Dm["weg"][e]), writes=[WG.res])
                    P.dma("pool", WU[:], _wview(Dm["weu"][e]), writes=[WU.res])
                    P.dma("pool", WD[:], _wview(Dm["wed"][e]), writes=[WD.res])
                    for tg in range(NH // 512):
                        tsl = slice(tg * 512, (tg + 1) * 512)
                        H = hdn[tg % 2]
                        for fc in range(4):
                            fsl = slice(fc * 128, (fc + 1) * 128)
                            G, U, SG = psG[fc % 2], psU[fc % 2], sg[fc % 2]
                            for k in range(8):
                                P.op("pe", lambda E, G=G, WG=WG, k=k, fsl=fsl, tsl=tsl: E.matmul(
                                    G[:], WG[:, k, fsl], x1T[:, k, tsl], start=(k == 0), stop=(k == 7)),
                                    reads=[WG.res, x1T.res], writes=[G.res], inc=(k == 7))
                            for k in range(8):
                                P.op("pe", lambda E, U=U, WU=WU, k=k, fsl=fsl, tsl=tsl: E.matmul(
                                    U[:], WU[:, k, fsl], x1T[:, k, tsl], start=(k == 0), stop=(k == 7)),
                                    reads=[WU.res, x1T.res], writes=[U.res], inc=(k == 7))
                            P.op("act", lambda E, SG=SG, G=G: E.activation(out=SG[:], in_=G[:], func=AF.Silu),
                                 reads=[G.res], writes=[SG.res])
                            P.op("dve", lambda E, H=H, fc=fc, SG=SG, U=U: E.tensor_tensor(out=H[:, fc, :], in0=U[:], in1=SG[:], op=ALU.mult),
                                 reads=[U.res, SG.res], writes=[H.res])
                        for blk in range(4):
                            b = tg * 4 + blk
                            ib = hf * NBH + b
                            bsl = slice(blk * 128, (blk + 1) * 128)
                            for half in range(2):
                                hs = slice(half * 512, (half + 1) * 512)
                                Dp = psDn[(blk * 2 + half) % 4]
                                for fc in range(4):
                                    P.op("pe", lambda E, Dp=Dp, H=H, fc=fc, bsl=bsl, WD=WD, hs=hs: E.matmul(
                                        Dp[:], H[:, fc, bsl], WD[:, fc, hs], start=(fc == 0), stop=(fc == 3)),
                                        reads=[H.res, WD.res], writes=[Dp.res], inc=(fc == 3))
                                P.op("dve", lambda E, Dp=Dp, b=b, hs=hs, ib=ib, e=e: E.scalar_tensor_tensor(
                                    out=acc[:, b, hs], in0=Dp[:], scalar=comb[:, ib, e:e + 1], in1=acc[:, b, hs],
                                    op0=ALU.mult, op1=ALU.add),
                                    reads=[Dp.res, comb.res, accr[b]], writes=[accr[b]])
                for b in range(NBH):
                    rows = slice(hf * NH + b * 128, hf * NH + (b + 1) * 128)
                    O = ot[b % 2]
                    zb = Tl(acc.t[:, b, :], f"accv{b}")
                    zb.res = accr[b]
                    _layer_norm(P, zb, O, junk, g2, b2, st, LN_EPS)
                    P.dma("sp", out[rows, :], O[:], reads=[O.res])
          P.barrier()
        P.finish()
        nc._prog = P
        with nc.Block() as block:
            P.emit(block)
    return nc


IN_SPLITS = (512, 512, 512, 256, 64, 4, 512, 512, 1024, 8, 8, 1024, 1024, 1024)
OFF = np.concatenate([[0], np.cumsum(IN_SPLITS)]).astype(int)


def _swap_perm(nheads):
    idx = []
    for h in range(nheads):
        idx += list(range(h * 64 + 32, h * 64 + 64)) + list(range(h * 64, h * 64 + 32))
    return np.array(idx)


def _fm_bias(b):
    return np.ascontiguousarray(b.reshape(-1, 128).T)


def _rope_tabs(pos, scale=1.0):
    inv = (np.float32(10000.0) ** (-np.arange(0, 64, 2, dtype=np.float32) / np.float32(64))).astype(np.float32)
    ang = pos.astype(np.float32)[None, :] * inv[:, None]
    c = np.cos(ang).astype(np.float32)
    s = np.sin(ang).astype(np.float32)
    cos = np.concatenate([c, c, c, c], 0)
    sin = np.concatenate([-s, s, -s, s], 0)
    return np.ascontiguousarray(cos * np.float32(scale)), np.ascontiguousarray(sin * np.float32(scale))


def _shared_inputs(w_in, b_in, inputs):
    sh = {}
    f = lambda k: np.asarray(inputs[k], np.float32)
    bc = lambda v, n: np.broadcast_to(v[None, :], (128, n))
    sh["wba"], sh["wbm"], sh["wout"] = f("w_branch_attn"), f("w_branch_mlstm"), f("w_out")
    sh["ln1g_bc"], sh["ln1b_bc"] = bc(f("ln1_gain"), D), bc(f("ln1_bias"), D)
    sh["ln2g_bc"], sh["ln2b_bc"] = bc(f("ln2_gain"), D), bc(f("ln2_bias"), D)
    sh["wr"] = np.concatenate([f("w_router_group"), f("w_router_expert")], 1)
    sh["br_bc"] = bc(np.concatenate([f("b_router_group"), f("b_router_expert")]), 20)
    sh["weg"], sh["weu"], sh["wed"] = f("w_exp_gate"), f("w_exp_up"), f("w_exp_down")
    sw8, sw4, sw1 = _swap_perm(8), _swap_perm(4), _swap_perm(1)
    aq, ak, av = w_in[:, OFF[0]:OFF[1]], w_in[:, OFF[1]:OFF[2]], w_in[:, OFF[2]:OFF[3]]
    iq, ik, iw = w_in[:, OFF[3]:OFF[4]], w_in[:, OFF[4]:OFF[5]], w_in[:, OFF[5]:OFF[6]]
    baq, bak, bav = b_in[OFF[0]:OFF[1]], b_in[OFF[1]:OFF[2]], b_in[OFF[2]:OFF[3]]
    biq, bik, biw = b_in[OFF[3]:OFF[4]], b_in[OFF[4]:OFF[5]], b_in[OFF[5]:OFF[6]]
    sh["wq"], sh["wq_s"] = aq, aq[:, sw8]
    sh["wk"], sh["wk_s"] = ak, ak[:, sw8]
    sh["wv"] = av
    sh["wiq"], sh["wiq_s"] = iq, iq[:, sw4]
    sh["wik2"] = np.concatenate([ik, ik], 1)
    sh["wik2_s"] = np.concatenate([ik[:, sw1], ik[:, sw1]], 1)
    sh["wiw"] = iw
    sh["bq"], sh["bq_s"] = _fm_bias(baq), _fm_bias(baq[sw8])
    sh["bk"], sh["bk_s"] = _fm_bias(bak), _fm_bias(bak[sw8])
    sh["biq"], sh["biq_s"] = _fm_bias(biq), _fm_bias(biq[sw4])
    sh["bik2"] = _fm_bias(np.concatenate([bik, bik]))
    sh["bik2_s"] = _fm_bias(np.concatenate([bik[sw1], bik[sw1]]))
    sh["bv_bc"] = np.broadcast_to(bav[None, :], (128, 512))
    sh["biw_bc"] = np.broadcast_to(biw[None, :], (128, 4))
    sh["wga"], sh["wgm"] = w_in[:, OFF[12]:OFF[13]], w_in[:, OFF[13]:OFF[14]]
    sh["bga"], sh["bgm"] = _fm_bias(b_in[OFF[12]:OFF[13]]), _fm_bias(b_in[OFF[13]:OFF[14]])
    sh["wmq"], sh["wmk"] = w_in[:, OFF[6]:OFF[7]], w_in[:, OFF[7]:OFF[8]]
    sh["bmq"], sh["bmk"] = _fm_bias(b_in[OFF[6]:OFF[7]]), _fm_bias(b_in[OFF[7]:OFF[8]])
    cm = f("conv_m")
    sh["cwq"] = cm[:, 0:512].T.reshape(4, 128, 4).transpose(1, 0, 2).reshape(128, 16)
    sh["cwk"] = cm[:, 512:1024].T.reshape(4, 128, 4).transpose(1, 0, 2).reshape(128, 16)
    sh["wmv"], sh["bmv_bc"] = w_in[:, OFF[8]:OFF[9]], bc(b_in[OFF[8]:OFF[9]], D)
    sh["wmif"], sh["bif_bc"] = w_in[:, OFF[9]:OFF[11]], bc(b_in[OFF[9]:OFF[11]], 16)
    sh["wmo"], sh["bmo_bc"] = w_in[:, OFF[11]:OFF[12]], bc(b_in[OFF[11]:OFF[12]], D)
    sh["gn_bc"] = bc(f("gn_m_gain"), D)
    qq = np.arange(128)
    sh["tri"] = (qq[:, None] <= qq[None, :]).astype(np.float32)
    sh["ones"] = np.ones((128, 128), np.float32)
    sh["pow2"] = np.broadcast_to((2.0 ** -(np.arange(32) + 1.0)).astype(np.float32)[None, :], (128, 32))
    cmask = np.where(qq[None, :] <= qq[:, None], 0.0, NEG).astype(np.float32)
    sh["cmask_rep"] = np.tile(cmask, (1, 8))
    sh["cmaskT_rep"] = np.tile(cmask.T, (1, 8))
    eye = np.eye(128, dtype=np.float32)
    sh["ident"] = eye
    sh["irep"] = np.concatenate([eye] * 4, 1)
    q = np.arange(128)
    sh["negm_diag"] = np.where(q[None, :] <= q[:, None], 0.0, NEG).astype(np.float32)
    return {k: np.ascontiguousarray(v, dtype=np.float32) for k, v in sh.items()}


def _core_inputs(x, c):
    b, p = c // 2, c % 2
    xb = x[b]
    if p == 1:
        xprog = xb
        pos = np.arange(S)
    else:
        xprog = np.concatenate([np.zeros((128, D), np.float32), xb[:S - 128]], 0)
        pos = np.concatenate([np.zeros(128), np.arange(S - 128)])
    own = xprog.reshape(64, 128, D)[1::2].reshape(NOWN, D)
    pos_own = pos.reshape(64, 128)[1::2].reshape(NOWN)
    m = {"xT": np.ascontiguousarray(xprog.T), "xT_own": np.ascontiguousarray(own.T),
         "x_own": np.ascontiguousarray(own)}
    m["cosk"], m["sink"] = _rope_tabs(pos)
    m["cosq"], m["sinq"] = _rope_tabs(pos_own, 0.125)
    m["cosi"], m["sini"] = _rope_tabs(pos_own)
    m["negm_b0"] = np.full((128, 128), NEG if p == 0 else 0.0, np.float32)
    m["vflag"] = np.full((128, 1), 0.0 if p == 0 else 1.0, np.float32)
    m["ineg"] = np.full((128, 1), NEG if p == 0 else 0.0, np.float32)
    return m


def make_in_maps(inputs):
    x = np.asarray(inputs["x"], np.float32)
    sh = _shared_inputs(np.asarray(inputs["w_in"], np.float32), np.asarray(inputs["b_in"], np.float32), inputs)
    maps = []
    for c in range(8):
        m = dict(sh)
        m.update(_core_inputs(x, c))
        maps.append({k: m[k] for k in IN_SPECS})
    return maps


def kernel(**inputs):
    nc = build_nc()
    in_maps = make_in_maps(inputs)
    res = run_bass_kernel_spmd(nc, in_maps, core_ids=list(range(8)))
    B = 4
    out = np.zeros((B, S, D), np.float32)
    for c in range(8):
        b, p = c // 2, c % 2
        o = np.asarray(res.results[c]["out"]).reshape(NPAIR, 128, D)
        out[b].reshape(64, 128, D)[p::2] = o
    return out
```
